# Optimizing a Trainium2 kernel written in Bass

```python
import jax
import jax.numpy as jnp
from jax import lax
import numpy as np

D_MODEL = 1024
BATCH = 4
SEQ = 8192
DEPTH = 1
DEC_BATCH = 128
DEC_SEQ = 8
PAST_LEN = 8192
PAGE_SIZE = 128

HEAD_DIM = 64
MIX_WIDTH = D_MODEL
RW_WIDTH = MIX_WIDTH // 2
AT_WIDTH = MIX_WIDTH - RW_WIDTH
RW_HEADS = RW_WIDTH // HEAD_DIM
AT_HEADS = AT_WIDTH // HEAD_DIM
DECAY_LORA = 32
AAA_LORA = 32
GATE_LORA = 64
RW_COLS = 3 * RW_WIDTH + DECAY_LORA + AAA_LORA + GATE_LORA
IN_COLS = RW_COLS + 3 * AT_WIDTH
GN_EPS = 64e-5
RMS_EPS = 1e-6
WINDOWS = (128, 512, 2048)
DILATIONS = (1, 4, 16)
MAX_WINDOW = max(WINDOWS)
ATT_BLOCK = 128
N_GROUPS = 4
EXPERTS_PER_GROUP = 8
N_EXPERTS = N_GROUPS * EXPERTS_PER_GROUP
TOP_K = 2
D_EXPERT = 256
PLE_DIM = 256

kernel_name = 'hymba_rwkv7_dilated_alibi_hmoe_step'


def rmsnorm(x, g):
    xf = x.astype(jnp.float32)
    y = xf * lax.rsqrt(jnp.mean(xf * xf, axis=-1, keepdims=True) + RMS_EPS)
    return (y * g.astype(jnp.float32)).astype(x.dtype)


def alibi_slopes():
    return jnp.exp2(-8.0 * jnp.arange(1, AT_HEADS + 1, dtype=jnp.float32) / AT_HEADS)


def wkv_scan(s0, r, decay, k, v, kk, a):
    def step(s, inp):
        r_t, w_t, k_t, v_t, kk_t, a_t = inp
        s_kk = jnp.einsum('bhvk,bhk->bhv', s, kk_t)
        s = (s * w_t[:, :, None, :]
             - s_kk[..., None] * (kk_t * a_t)[:, :, None, :]
             + v_t[..., None] * k_t[:, :, None, :])
        return s, jnp.einsum('bhvk,bhk->bhv', s, r_t)
    xs = tuple(jnp.swapaxes(z.astype(jnp.float32), 0, 1) for z in (r, decay, k, v, kk, a))
    s_final, ys = lax.scan(step, s0.astype(jnp.float32), xs)
    return jnp.swapaxes(ys, 0, 1), s_final


def rwkv7_group(pr, pr_prev, wkv0, prm):
    B, T, _ = pr.shape
    xm = pr + (pr_prev - pr) * prm['mu']
    c = [int(e) for e in np.cumsum([0, RW_WIDTH, RW_WIDTH, RW_WIDTH, DECAY_LORA, AAA_LORA, GATE_LORA])]
    xr, xk, xv, xw, xa, xg = [xm[..., c[i]:c[i + 1]] for i in range(6)]
    heads = lambda z: z.reshape(B, T, RW_HEADS, HEAD_DIM)
    w = -jax.nn.softplus(-(prm['w0'] + jnp.tanh(xw) @ prm['w2'])) - 0.5
    decay = jnp.exp(-jnp.exp(w.astype(jnp.float32)))
    a = jax.nn.sigmoid(prm['a0'] + xa @ prm['a2'])
    g = jax.nn.sigmoid(xg) @ prm['g2']
    kk = heads(xk * prm['k_k']).astype(jnp.float32)
    kk = kk / jnp.maximum(jnp.sqrt(jnp.sum(kk * kk, axis=-1, keepdims=True)), 1e-12)
    k = heads(xk * (1.0 + (a - 1.0) * prm['k_a']))
    r, v, a = heads(xr), heads(xv), heads(a)
    y, wkv_new = wkv_scan(wkv0, r, heads(decay), k, v, kk, a)
    mean = jnp.mean(y, axis=-1, keepdims=True)
    var = jnp.mean(jnp.square(y - mean), axis=-1, keepdims=True)
    yn = ((y - mean) * lax.rsqrt(var + GN_EPS)).reshape(B, T, RW_WIDTH) * prm['ln_x_w'] + prm['ln_x_b']
    bonus = (jnp.sum(r * k * prm['r_k'], axis=-1, keepdims=True) * v).reshape(B, T, RW_WIDTH)
    return (yn + bonus) * g, wkv_new


def dilated_branch_prompt(q, k, v, dil, n_steps, slopes):
    B, S, H, Dh = q.shape
    span = ATT_BLOCK * dil
    s_pad = -(-S // span) * span
    n_blk = s_pad // span

    def to_blocks(z):
        z = jnp.pad(z.astype(jnp.float32), ((0, 0), (0, s_pad - S), (0, 0), (0, 0)))
        z = jnp.swapaxes(z.reshape(B, s_pad // dil, dil, H, Dh), 1, 2)
        return z.reshape(B, dil, n_blk, ATT_BLOCK, H, Dh)

    def with_prev(z):
        prev = jnp.pad(z, ((0, 0), (0, 0), (1, 0), (0, 0), (0, 0), (0, 0)))[:, :, :-1]
        return jnp.concatenate([prev, z], axis=3)

    def from_blocks(z):
        rest = z.shape[4:]
        z = z.reshape((B, dil, s_pad // dil) + rest)
        return jnp.swapaxes(z, 1, 2).reshape((B, s_pad) + rest)[:, :S]

    qb = to_blocks(q)
    kb, vb = with_prev(to_blocks(k)), with_prev(to_blocks(v))
    scores = jnp.einsum('brnqhd,brnkhd->brnhqk', qb, kb) * (HEAD_DIM ** -0.5)
    qi = jnp.arange(ATT_BLOCK)[:, None]
    ki = jnp.arange(2 * ATT_BLOCK)[None, :]
    steps = ATT_BLOCK + qi - ki
    blk = jnp.arange(n_blk)[:, None, None]
    valid = (steps >= 0) & (steps <= n_steps) & ((blk > 0) | (ki >= ATT_BLOCK))
    bias = -slopes[:, None, None] * (steps * dil).astype(jnp.float32)
    scores = jnp.where(valid[:, None], scores + bias, -jnp.inf)
    m = jnp.max(scores, axis=-1, keepdims=True)
    e = jnp.exp(scores - m)
    den = jnp.sum(e, axis=-1)
    o = jnp.einsum('brnhqk,brnkhd->brnqhd', e, vb) / jnp.swapaxes(den, -1, -2)[..., None]
    lse = jnp.swapaxes(m[..., 0] + jnp.log(den), -1, -2)
    return from_blocks(o), from_blocks(lse)


def dilated_branch_sample(q, k_all, v_all, dil, n_steps, slopes):
    B, T, H, Dh = q.shape
    n_prev = k_all.shape[1] - T
    steps = jnp.arange(n_steps + 1)
    idx = n_prev + jnp.arange(T)[:, None] - steps[None, :] * dil
    valid = idx >= 0
    idx = jnp.maximum(idx, 0)
    kg = k_all[:, idx].astype(jnp.float32)
    vg = v_all[:, idx].astype(jnp.float32)
    scores = (jnp.einsum('bthd,btkhd->bthk', q.astype(jnp.float32), kg) * (HEAD_DIM ** -0.5)
              - slopes[:, None] * (steps * dil).astype(jnp.float32))
    scores = jnp.where(valid[None, :, None, :], scores, -jnp.inf)
    m = jnp.max(scores, axis=-1, keepdims=True)
    e = jnp.exp(scores - m)
    den = jnp.sum(e, axis=-1)
    o = jnp.einsum('bthk,btkhd->bthd', e, vg) / den[..., None]
    return o, m[..., 0] + jnp.log(den)


def mix_branches(outs, lses):
    wts = jax.nn.softmax(jnp.stack(lses), axis=0)
    return jnp.sum(wts[..., None] * jnp.stack(outs), axis=0)


def hier_moe(h, prm):
    B, T, D = h.shape
    hf = h.reshape(B * T, D)
    n = hf.shape[0]
    glog = (hf @ prm['w_group'] + prm['b_group']).astype(jnp.float32)
    gsel = jnp.argmax(glog, axis=-1)
    gw = jnp.take_along_axis(jax.nn.softmax(glog, axis=-1), gsel[:, None], axis=-1)
    elog = (hf @ prm['w_expert_router'] + prm['b_expert_router']).astype(jnp.float32)
    elog = elog.reshape(n, N_GROUPS, EXPERTS_PER_GROUP)[jnp.arange(n), gsel]
    tv, ti = lax.top_k(elog, TOP_K)
    tw = jax.nn.softmax(tv, axis=-1) * gw
    eid = gsel[:, None] * EXPERTS_PER_GROUP + ti
    comb = jnp.einsum('nk,nke->ne', tw, jax.nn.one_hot(eid, N_EXPERTS, dtype=jnp.float32))

    def expert_step(acc, ws):
        wg, wu, wd, c = ws
        out = (jax.nn.silu(hf @ wg) * (hf @ wu)) @ wd
        return acc + c[:, None] * out.astype(jnp.float32), None

    acc, _ = lax.scan(expert_step, jnp.zeros((n, D), jnp.float32),
                      (prm['w_gate'], prm['w_up'], prm['w_down'], comb.T))
    return acc.reshape(B, T, D).astype(h.dtype)


def decoder_layer(x, p, shift0, wkv0, attend, prm):
    B, T, _ = x.shape
    h = rmsnorm(x, prm['norm_mix'])
    proj = h @ prm['w_in']
    pr, qkv = proj[..., :RW_COLS], proj[..., RW_COLS:]
    pr_prev = jnp.concatenate([shift0[:, None].astype(pr.dtype), pr[:, :-1]], axis=1)
    rw_out, wkv_new = rwkv7_group(pr, pr_prev, wkv0, prm)
    q, k, v = [qkv[..., i * AT_WIDTH:(i + 1) * AT_WIDTH].reshape(B, T, AT_HEADS, HEAD_DIM) for i in range(3)]
    at_out = rmsnorm(attend(q, k, v).reshape(B, T, AT_WIDTH), prm['attn_gain'])
    mixed = jnp.concatenate([rw_out.astype(x.dtype), at_out.astype(x.dtype)], axis=-1)
    x = x + mixed @ prm['w_out']
    x = x + hier_moe(rmsnorm(x, prm['norm_ffn']), prm)
    gate = jax.nn.sigmoid(rmsnorm(x, prm['norm_ple']) @ prm['w_ple_gate'])
    x = x + (p @ prm['w_ple']) * gate
    return x, pr[:, -1], wkv_new, k, v


def setup_inputs(seed: int = 0) -> dict:
    key = jax.random.key(seed)
    keys = iter(jax.random.split(key, 64))
    f32 = jnp.float32
    L = DEPTH
    win_buf = min(MAX_WINDOW, PAST_LEN)

    def nrm(shape, scale):
        return scale * jax.random.normal(next(keys), shape, f32)

    def gain(shape):
        return 1.0 + nrm(shape, 0.05)

    return {
        'x_prompt': nrm((BATCH, SEQ, D_MODEL), 1.0),
        'x_sample': nrm((DEC_BATCH, DEC_SEQ, D_MODEL), 1.0),
        'state_wkv': nrm((L, DEC_BATCH, RW_HEADS, HEAD_DIM, HEAD_DIM), 0.3),
        'state_shift': nrm((L, DEC_BATCH, RW_COLS), 1.0),
        'cache_k_win': nrm((L, DEC_BATCH, win_buf, AT_HEADS, HEAD_DIM), 1.0),
        'cache_v_win': nrm((L, DEC_BATCH, win_buf, AT_HEADS, HEAD_DIM), 1.0),
        'p_prompt': nrm((L, BATCH, SEQ, PLE_DIM), 1.0),
        'p_sample': nrm((L, DEC_BATCH, DEC_SEQ, PLE_DIM), 1.0),
        'norm_mix': gain((L, D_MODEL)),
        'w_in': nrm((L, D_MODEL, IN_COLS), D_MODEL ** -0.5),
        'mu': jax.random.uniform(next(keys), (L, RW_COLS), f32),
        'w0': jax.random.uniform(next(keys), (L, RW_WIDTH), f32, -6.0, 1.0),
        'w2': nrm((L, DECAY_LORA, RW_WIDTH), 0.1),
        'a0': nrm((L, RW_WIDTH), 0.5),
        'a2': nrm((L, AAA_LORA, RW_WIDTH), 0.1),
        'g2': nrm((L, GATE_LORA, RW_WIDTH), GATE_LORA ** -0.5),
        'k_k': 0.85 + nrm((L, RW_WIDTH), 0.05),
        'k_a': gain((L, RW_WIDTH)),
        'r_k': nrm((L, RW_HEADS, HEAD_DIM), 0.1),
        'ln_x_w': gain((L, RW_WIDTH)),
        'ln_x_b': nrm((L, RW_WIDTH), 0.02),
        'attn_gain': gain((L, AT_WIDTH)),
        'w_out': nrm((L, MIX_WIDTH, D_MODEL), MIX_WIDTH ** -0.5),
        'norm_ffn': gain((L, D_MODEL)),
        'w_group': nrm((L, D_MODEL, N_GROUPS), D_MODEL ** -0.5),
        'b_group': nrm((L, N_GROUPS), 0.01),
        'w_expert_router': nrm((L, D_MODEL, N_EXPERTS), D_MODEL ** -0.5),
        'b_expert_router': nrm((L, N_EXPERTS), 0.01),
        'w_gate': nrm((L, N_EXPERTS, D_MODEL, D_EXPERT), D_MODEL ** -0.5),
        'w_up': nrm((L, N_EXPERTS, D_MODEL, D_EXPERT), D_MODEL ** -0.5),
        'w_down': nrm((L, N_EXPERTS, D_EXPERT, D_MODEL), D_EXPERT ** -0.5),
        'norm_ple': gain((L, D_MODEL)),
        'w_ple': nrm((L, PLE_DIM, D_MODEL), PLE_DIM ** -0.5),
        'w_ple_gate': nrm((L, D_MODEL, D_MODEL), D_MODEL ** -0.5),
        'norm_final': gain((D_MODEL,)),
    }


def reference(x_prompt, x_sample, state_wkv, state_shift, cache_k_win, cache_v_win, p_prompt, p_sample,
              norm_mix, w_in, mu, w0, w2, a0, a2, g2, k_k, k_a, r_k, ln_x_w, ln_x_b, attn_gain, w_out,
              norm_ffn, w_group, b_group, w_expert_router, b_expert_router, w_gate, w_up, w_down,
              norm_ple, w_ple, w_ple_gate, norm_final):
    slopes = alibi_slopes()
    branches = tuple((d, w // d) for w, d in zip(WINDOWS, DILATIONS))
    n_prompt = x_prompt.shape[0]

    def attend_prompt(q, k, v):
        res = [dilated_branch_prompt(q, k, v, d, n, slopes) for d, n in branches]
        return mix_branches([r[0] for r in res], [r[1] for r in res])

    yp, ys = x_prompt, x_sample
    wkv_p, shift_p, kwin_p, vwin_p = [], [], [], []
    wkv_s, shift_s, kwin_s, vwin_s = [], [], [], []
    for i in range(DEPTH):
        prm = {
            'norm_mix': norm_mix[i], 'w_in': w_in[i], 'mu': mu[i], 'w0': w0[i], 'w2': w2[i],
            'a0': a0[i], 'a2': a2[i], 'g2': g2[i], 'k_k': k_k[i], 'k_a': k_a[i], 'r_k': r_k[i],
            'ln_x_w': ln_x_w[i], 'ln_x_b': ln_x_b[i], 'attn_gain': attn_gain[i], 'w_out': w_out[i],
            'norm_ffn': norm_ffn[i], 'w_group': w_group[i], 'b_group': b_group[i],
            'w_expert_router': w_expert_router[i], 'b_expert_router': b_expert_router[i],
            'w_gate': w_gate[i], 'w_up': w_up[i], 'w_down': w_down[i],
            'norm_ple': norm_ple[i], 'w_ple': w_ple[i], 'w_ple_gate': w_ple_gate[i],
        }
        shift0 = jnp.zeros((n_prompt, RW_COLS), x_prompt.dtype)
        wkv0 = jnp.zeros((n_prompt, RW_HEADS, HEAD_DIM, HEAD_DIM), jnp.float32)
        yp, sh, wkv, kp, vp = decoder_layer(yp, p_prompt[i], shift0, wkv0, attend_prompt, prm)
        keep_p = min(MAX_WINDOW, kp.shape[1])
        wkv_p.append(wkv)
        shift_p.append(sh)
        kwin_p.append(kp[:, kp.shape[1] - keep_p:])
        vwin_p.append(vp[:, vp.shape[1] - keep_p:])

        kc, vc = cache_k_win[i], cache_v_win[i]

        def attend_sample(q, k, v, kc=kc, vc=vc):
            k_all = jnp.concatenate([kc.astype(k.dtype), k], axis=1)
            v_all = jnp.concatenate([vc.astype(v.dtype), v], axis=1)
            res = [dilated_branch_sample(q, k_all, v_all, d, n, slopes) for d, n in branches]
            return mix_branches([r[0] for r in res], [r[1] for r in res])

        ys, sh_s, wkv_n, ks, vs = decoder_layer(ys, p_sample[i], state_shift[i], state_wkv[i], attend_sample, prm)
        keep_s = kc.shape[1]
        k_all = jnp.concatenate([kc.astype(ks.dtype), ks], axis=1)
        v_all = jnp.concatenate([vc.astype(vs.dtype), vs], axis=1)
        wkv_s.append(wkv_n)
        shift_s.append(sh_s)
        kwin_s.append(k_all[:, k_all.shape[1] - keep_s:])
        vwin_s.append(v_all[:, v_all.shape[1] - keep_s:])

    y_prompt = rmsnorm(yp, norm_final)
    y_sample = rmsnorm(ys, norm_final)
    return (y_prompt, y_sample,
            jnp.stack(wkv_p), jnp.stack(shift_p), jnp.stack(kwin_p), jnp.stack(vwin_p),
            jnp.stack(wkv_s), jnp.stack(shift_s), jnp.stack(kwin_s), jnp.stack(vwin_s))
```

```python
import numpy as np
from contextlib import ExitStack
import concourse.bass as bass
import concourse.mybir as mybir
from concourse.bass_utils import run_bass_kernel_spmd

F32 = mybir.dt.float32
BF16 = mybir.dt.bfloat16
I32 = mybir.dt.int32
AF = mybir.ActivationFunctionType
ALU = mybir.AluOpType
AX = mybir.AxisListType


class Buf:
    def __init__(self, t, name, psum=False):
        self.t = t
        self.name = name
        self.psum = psum
        self.st = {None: [{}, {}]}


def _merge(dst, src):
    for k, v in src.items():
        if dst.get(k, 0) < v:
            dst[k] = v


class FW:
    ENGS = ('pe', 'act', 'dve', 'pool', 'sp')

    def __init__(self, nc, st, n_dma_sems=24):
        self.nc = nc
        self.stk = st
        self.ops = {e: [] for e in self.ENGS}
        self.cnt = {e: 0 for e in self.ENGS}
        self.sem = {}
        self.semobj = {}
        for e in self.ENGS:
            s = st.enter_context(nc.semaphore("c_" + e))
            self.sem[e] = id(s)
            self.semobj[id(s)] = s
        self.waited = {e: {} for e in self.ENGS}
        self.dsems = {'hw': [], 'sw': []}
        for kind in ('hw', 'sw'):
            for i in range(n_dma_sems):
                s = st.enter_context(nc.semaphore("d%s%d" % (kind, i)))
                self.semobj[id(s)] = s
                self.dsems[kind].append([s, 0])
        self.dnext = {'hw': 0, 'sw': 0}
        self.out_tokens = {}
        self.n_inst = 0
        self.extra_dsems = []

    def sb(self, name, shape, dtype):
        t = self.stk.enter_context(self.nc.sbuf_tensor("s_" + name, list(shape), dtype))
        return Buf(t, name)

    def ps(self, name, shape, dtype):
        t = self.stk.enter_context(self.nc.psum_tensor("p_" + name, list(shape), dtype))
        return Buf(t, name, psum=True)

    @staticmethod
    def _norm(x):
        if isinstance(x, tuple):
            if x[0].psum:
                return (x[0], None)
            return x
        return (x, None)

    def _deps(self, reads, writes):
        deps = {}
        for b, k in map(self._norm, reads):
            if k is None:
                for kk, (w, r) in b.st.items():
                    _merge(deps, w)
                    if b.psum:
                        _merge(deps, r)
            else:
                _merge(deps, b.st[None][0])
                if k in b.st:
                    _merge(deps, b.st[k][0])
        for b, k in map(self._norm, writes):
            if k is None:
                for kk, (w, r) in b.st.items():
                    _merge(deps, w)
                    _merge(deps, r)
            else:
                _merge(deps, b.st[None][0])
                _merge(deps, b.st[None][1])
                if k in b.st:
                    _merge(deps, b.st[k][0])
                    _merge(deps, b.st[k][1])
        return deps

    def _update(self, reads, writes, tok):
        sid, val = tok
        for b, k in map(self._norm, reads):
            s = b.st.setdefault(k, [{}, {}])
            if s[1].get(sid, 0) < val:
                s[1][sid] = val
        for b, k in map(self._norm, writes):
            if k is None:
                b.st = {None: [{sid: val}, {}]}
            else:
                b.st[k] = [{sid: val}, {}]

    def _emit_waits(self, eng, deps, skip_self):
        lst = []
        wd = self.waited[eng]
        for sid, val in deps.items():
            if skip_self and sid == self.sem[eng]:
                continue
            if wd.get(sid, 0) >= val:
                continue
            wd[sid] = val
            lst.append((self.semobj[sid], val))
        return lst

    def op(self, eng, fn, reads=(), writes=()):
        deps = self._deps(reads, writes)
        waits = self._emit_waits(eng, deps, skip_self=(eng == 'pe'))
        self.cnt[eng] += 1
        idx = self.cnt[eng]
        semo = self.semobj[self.sem[eng]]

        def run(e, fn=fn, waits=waits, semo=semo):
            for s, v in waits:
                e.wait_ge(s, v)
            fn(e).then_inc(semo, 1)
        self.ops[eng].append(run)
        self.waited[eng][self.sem[eng]] = max(self.waited[eng].get(self.sem[eng], 0), 0)
        self._update(reads, writes, (self.sem[eng], idx))
        self.n_inst += 1
        return (self.sem[eng], idx)

    def dma(self, eng, out, in_, reads=(), writes=(), out_dram=False, nc_ok=False, n=1, fn=None, ent=None):
        deps = self._deps(reads, writes)
        kind = 'sw' if eng == 'pool' else 'hw'
        if ent is None:
            ent = self.dsems[kind][self.dnext[kind]]
            self.dnext[kind] = (self.dnext[kind] + 1) % len(self.dsems[kind])
            if ent[1] > 0:
                deps[id(ent[0])] = max(deps.get(id(ent[0]), 0), ent[1])
        dsem = ent[0]
        waits = self._emit_waits(eng, deps, skip_self=False)
        ent[1] += 16 * n
        val = ent[1]

        def run(e, waits=waits, dsem=dsem):
            for s, v in waits:
                e.wait_ge(s, v)
            if fn is not None:
                for ins in fn(e):
                    ins.then_inc(dsem, 16)
            else:
                kw = {}
                if nc_ok:
                    kw['allow_slow_non_contiguous'] = True
                e.dma_start(out=out, in_=in_, **kw).then_inc(dsem, 16)
        self.ops[eng].append(run)
        tok = (id(dsem), val)
        self._update(reads, writes, tok)
        if out_dram:
            self.out_tokens[id(dsem)] = val
        self.n_inst += 1
        return tok

    def new_dsem(self, name):
        s = self.stk.enter_context(self.nc.semaphore(name))
        self.semobj[id(s)] = s
        ent = [s, 0]
        self.extra_dsems.append(ent)
        return ent

    def barrier(self):
        for eng in self.ENGS:
            waits = []
            wd = self.waited[eng]
            for e2 in self.ENGS:
                sid = self.sem[e2]
                if e2 != eng and self.cnt[e2] > wd.get(sid, 0):
                    waits.append((self.semobj[sid], self.cnt[e2]))
                    wd[sid] = self.cnt[e2]
            for kind in ('hw', 'sw'):
                for sm, v in self.dsems[kind]:
                    if v > wd.get(id(sm), 0):
                        waits.append((sm, v))
                        wd[id(sm)] = v

            def run(e, waits=waits):
                for sm, v in waits:
                    e.wait_ge(sm, v)
            self.ops[eng].append(run)

    def make_identity(self, ident, dtype_f32_tmp=None):
        def f1(e):
            return e.memset(ident.t[:, :], 1.0)
        self.op('pool', f1, writes=[ident])
        def f2(e):
            return e.affine_select(ident.t[:, :], ident.t[:, :], [[-1, 128]], ALU.is_equal, 0.0,
                                   base=0, channel_multiplier=1)
        self.op('pool', f2, reads=[ident], writes=[ident])

    def finish(self):
        finals = [(self.semobj[sid], v) for sid, v in self.out_tokens.items()]
        nc = self.nc
        with nc.Block() as block:
            @block.tensor
            def _(e):
                for f in self.ops['pe']:
                    f(e)

            @block.scalar
            def _(e):
                for f in self.ops['act']:
                    f(e)

            @block.vector
            def _(e):
                for f in self.ops['dve']:
                    f(e)

            @block.gpsimd
            def _(e):
                for f in self.ops['pool']:
                    f(e)

            @block.sync
            def _(e):
                for f in self.ops['sp']:
                    f(e)
                for s, v in finals:
                    e.wait_ge(s, v)


D = 1024
RW = 512
RW_COLS = 1664
IN_COLS = 3200
QOFF, KOFF, VOFF = 1664, 1664 + 512, 1664 + 1024
W = 512
RING = 24
NKB = 17
RMS_EPS = 1e-6
GN_EPS = 64e-5
DECAY_C = float(np.exp(-0.5))
MAXWIN = 2048
NSEQ = 16


def _host_consts():
    c = {}
    s = np.arange(64)[:, None]
    t = np.arange(64)[None, :]
    c["cI"] = (-DECAY_C * (s <= t)).astype(np.float32)
    c["cS"] = (-DECAY_C * (s < t)).astype(np.float32)
    c["cR"] = (-DECAY_C * (s > t)).astype(np.float32)
    up_s = (s < t).astype(np.float32)
    up_i = (s <= t).astype(np.float32)
    lo_s = (t < s).astype(np.float32)
    c["mk"] = np.concatenate([up_s, lo_s, up_s, up_i, up_i], axis=1)
    bo = np.zeros((128, 128), np.float32)
    bo[:64, :64] = 1
    bo[64:, 64:] = 1
    c["bones"] = bo
    slopes = 2.0 ** (-8.0 * np.arange(1, 9) / 8)
    ki = np.arange(128)[:, None]
    qi = np.arange(128)[None, :]
    em = np.zeros((8, 128, NKB, 128), np.float32)
    for j in range(NKB):
        dl = 128 * j + qi - ki
        mult = np.zeros_like(dl, dtype=np.float64)
        for wd, dil in ((128, 1), (512, 4), (2048, 16)):
            mult += ((dl >= 0) & (dl % dil == 0) & (dl <= wd))
        for h in range(8):
            em[h, :, j, :] = mult * np.exp(-slopes[h] * np.maximum(dl, 0))
    c["em"] = em
    def fac(dl):
        mult = np.zeros(dl.shape, np.float64)
        for wd, dil in ((128, 1), (512, 4), (2048, 16)):
            mult += ((dl >= 0) & (dl % dil == 0) & (dl <= wd))
        return mult[None] * np.exp(-slopes[:, None, None, None] * np.maximum(dl, 0)[None])
    p_ = np.arange(128)[:, None, None]
    blk_ = np.arange(16)[None, :, None]
    t_ = np.arange(8)[None, None, :]
    ems = fac(MAXWIN + t_ - (blk_ * 128 + p_))
    c["ems"] = np.ascontiguousarray(ems.transpose(1, 2, 0, 3)).reshape(128, 1024).astype(np.float32)
    bq_ = np.arange(NSEQ)[None, :, None]
    emn = fac(t_ - (p_ % 8) + 0 * bq_) * ((p_ // 8) == bq_)[None]
    c["emn"] = np.ascontiguousarray(emn.transpose(1, 2, 0, 3)).reshape(128, 1024).astype(np.float32)
    return c


CONST_SHAPES = {"cI": [64, 64], "cS": [64, 64], "cR": [64, 64], "mk": [64, 320],
                "bones": [128, 128], "em": [8, 128, NKB, 128], "ems": [128, 1024], "emn": [128, 1024]}

WEIGHT_SHAPES = {
    "norm_mix": [D], "w_in": [D, IN_COLS], "mu": [RW_COLS], "w0": [RW], "w2": [32, RW],
    "a0": [RW], "a2": [32, RW], "g2": [64, RW], "k_k": [RW], "k_a": [RW], "r_k": [RW],
    "ln_x_w": [RW], "ln_x_b": [RW], "attn_gain": [RW], "w_out": [D, D], "norm_ffn": [D],
    "w_group": [D, 4], "b_group": [4], "w_expert_router": [D, 32], "b_expert_router": [32],
    "w_gate": [32, D, 256], "w_up": [32, D, 256], "w_down": [32, 256, D],
    "norm_ple": [D], "w_ple": [256, D], "w_ple_gate": [D, D], "norm_final": [D],
}


import os as _os
SKIP = set(_os.environ.get('KSKIP', '').split(','))
VC_ENG = _os.environ.get('KVCENG', 'pool')


class StopBuild(Exception):
    pass


class Builder:
    def __init__(self, seq, stages=99, dbg=()):
        self.seq = seq
        self.nw = seq // W
        self.stages = stages
        self.dbgnames = set(dbg)
        self.nc = bass.Bass("TRN2", target_bir_lowering=False)
        self.dr = {}
        self.dbg_out = {}

    def din(self, name, shape):
        self.dr[name] = self.nc.dram_tensor(name, list(shape), F32, kind="ExternalInput").ap()
        return self.dr[name]

    def dout(self, name, shape):
        self.dr[name] = self.nc.dram_tensor(name, list(shape), F32, kind="ExternalOutput").ap()
        return self.dr[name]

    def ck(self, x):
        if self.stages < x:
            raise StopBuild()

    def bank(self):
        for _ in range(8):
            b = self.PS[self.pnext]
            self.pnext = (self.pnext + 1) % 8
            if b not in self.held:
                return b
        raise RuntimeError("no free bank")

    def mm(self, out, lhsT, rhs, start, stop, r, w):
        self.fw.op('pe', lambda e: e.matmul(out, lhsT, rhs, start=start, stop=stop), reads=r, writes=w)

    def tr(self, out, in_, ident, r, w):
        self.fw.op('pe', lambda e: e.transpose(out, in_, ident), reads=r, writes=w)

    def act(self, out, in_, func, r, w, bias=None, scale=None, accum=None):
        kw = {}
        if bias is not None:
            kw['bias'] = bias
        if scale is not None:
            kw['scale'] = scale
        if accum is not None:
            kw['accum_out'] = accum
        self.fw.op('act', lambda e: e.activation(out, in_, func, **kw), reads=r, writes=w)

    def tt(self, out, a, b, op, r, w, eng='dve'):
        self.fw.op(eng, lambda e: e.tensor_tensor(out, a, b, op), reads=r, writes=w)

    def ts(self, out, a, s1, s2, op0, op1, r, w, eng='dve'):
        if s2 is None:
            self.fw.op(eng, lambda e: e.tensor_scalar(out, a, s1, None, op0), reads=r, writes=w)
        else:
            self.fw.op(eng, lambda e: e.tensor_scalar(out, a, s1, s2, op0, op1), reads=r, writes=w)

    def stt(self, out, a, sc, b, op0, op1, r, w, eng='dve'):
        self.fw.op(eng, lambda e: e.scalar_tensor_tensor(out, a, sc, b, op0, op1), reads=r, writes=w)

    def cp(self, out, in_, r, w, eng='dve'):
        if eng == 'act':
            self.act(out, in_, AF.Copy, r, w)
        else:
            self.fw.op(eng, lambda e: e.tensor_copy(out, in_), reads=r, writes=w)

    def dbg(self, name, ap, shape, reads):
        if name not in self.dbgnames:
            return
        d = self.dout("dbg_" + name, shape)
        self.fw.dma('sp', d, ap, reads=reads, out_dram=True)

    def wload(self, src_ap, rows_k, cols, eng='sp', reads=()):
        t = self.WP[self.wnext]
        self.wnext = (self.wnext + 1) % len(self.WP)
        self.fw.dma(eng, t.t[:, 0:rows_k, 0:cols], src_ap, reads=list(reads), writes=[t], nc_ok=True)
        return t

    def vec_load(self, name, nk, dst=None):
        t = self.fw.sb("v_" + name, [128, nk], F32) if dst is None else dst
        self.fw.dma('sp', t.t[:, :], self.dr[name].rearrange("(k p) -> p k", p=128), writes=[t], nc_ok=True)
        return t

    def norm_T(self, Xs, gain, hT, rows=128):
        fw = self.fw
        for blk, Xb in enumerate(Xs):
            ss = self.ss
            self.act(self.junk.t[0:rows, :, :].rearrange("p a b -> p (a b)"), Xb.t[0:rows, :], AF.Square, [Xb], [self.junk, ss], accum=ss.t[0:rows, :])
            self.ts(ss.t[0:rows, :], ss.t[0:rows, :], 1.0 / D, RMS_EPS, ALU.mult, ALU.add, [ss], [ss])
            self.act(ss.t[0:rows, :], ss.t[0:rows, :], AF.Sqrt, [ss], [ss])
            fw.op('dve', lambda e, ss=ss: e.reciprocal(ss.t[0:rows, :], ss.t[0:rows, :]), reads=[ss], writes=[ss])
            xn = self.xn
            self.ts(xn.t[0:rows, :], Xb.t[0:rows, :], ss.t[0:rows, 0:1], None, ALU.mult, None, [Xb, ss], [xn])
            for half in range(2):
                pb = self.bank()
                for kk in range(4):
                    k = half * 4 + kk
                    self.tr(pb.t[:, kk * 128:kk * 128 + rows], xn.t[0:rows, k * 128:(k + 1) * 128],
                            self.identf.t[0:rows, 0:rows], [xn, self.identf], [pb])
                src = pb.t[:, :].rearrange("p (k t) -> p k t", k=4)[:, :, 0:rows]
                gb = gain.t[:, half * 4:half * 4 + 4].unsqueeze(2).to_broadcast([128, 4, rows])
                self.tt(hT.t[:, half * 4:half * 4 + 4, blk * 128:blk * 128 + rows], src, gb, ALU.mult,
                        [pb, gain], [(hT, ('b', blk, half))])

    def build(self):
        nc = self.nc
        seq, nw = self.seq, self.nw
        keep = min(MAXWIN, seq)
        xp = self.din("xp", [seq, D])
        pp = self.din("pp", [seq, 256])
        for k, shp in WEIGHT_SHAPES.items():
            self.din(k, shp)
        for k, shp in CONST_SHAPES.items():
            self.din(k, shp)
        yp = self.dout("yp", [seq, D])
        wkvp = self.dout("wkvp", [8, 64, 64])
        shp_o = self.dout("shp", [RW_COLS])
        kwp = self.dout("kwp", [keep, 512])
        vwp = self.dout("vwp", [keep, 512])
        xs = self.din("xs", [128, D])
        pps = self.din("pps", [128, 256])
        swkv = self.din("swkv", [128, 4096])
        sshift = self.din("sshift", [NSEQ, RW_COLS])
        kc = self.din("kc", [NSEQ, MAXWIN, 512])
        vc = self.din("vc", [NSEQ, MAXWIN, 512])
        ys = self.dout("ys", [128, D])
        wkvs = self.dout("wkvs", [128, 4096])
        shs = self.dout("shs", [NSEQ, RW_COLS])
        kws = self.dout("kws", [NSEQ, MAXWIN, 512])
        vws = self.dout("vws", [NSEQ, MAXWIN, 512])
        scrA = self.dout("scrA", [128, 6 * 512])
        scrB = self.dout("scrB", [128, 512])
        dr = self.dr
        with ExitStack() as st:
            fw = self.fw = FW(nc, st, n_dma_sems=16)
            sb = fw.sb
            self.PS = [fw.ps("ps%d" % i, [128, 512], F32) for i in range(8)]
            self.pnext = 0
            self.held = set()
            self.WP = [sb("wp%d" % i, [128, 8, 512], BF16) for i in range(2)]
            self.wnext = 0
            self.identf = identf = sb("identf", [128, 128], F32)
            fw.make_identity(identf)
            g_mix, g_ffn, g_ple = self.vec_load("norm_mix", 8), self.vec_load("norm_ffn", 8), self.vec_load("norm_ple", 8)
            gfin = sb("gfin", [128, D], F32)
            fw.dma('sp', gfin.t[:, :], dr["norm_final"].partition_broadcast(128), writes=[gfin])
            again = sb("again", [128, RW], F32)
            fw.dma('sp', again.t[:, :], dr["attn_gain"].partition_broadcast(128), writes=[again])
            w0rep = sb("w0rep", [128, RW], F32)
            fw.dma('sp', w0rep.t[:, :], dr["w0"].partition_broadcast(128), writes=[w0rep])
            LW = sb("LW", [128, RW], BF16)
            fw.dma('pool', LW.t[0:32, :], dr["w2"], writes=[(LW, 0)])
            fw.dma('pool', LW.t[32:64, :], dr["a2"], writes=[(LW, 1)])
            fw.dma('pool', LW.t[64:128, :], dr["g2"], writes=[(LW, 2)])
            Wrt = sb("Wrt", [128, 8, 36], BF16)
            fw.dma('pool', Wrt.t[:, :, 0:4], dr["w_group"].rearrange("(k p) c -> p k c", p=128), writes=[(Wrt, 0)])
            fw.dma('pool', Wrt.t[:, :, 4:36], dr["w_expert_router"].rearrange("(k p) c -> p k c", p=128), writes=[(Wrt, 1)])
            brep = sb("brep", [128, 36], F32)
            fw.dma('sp', brep.t[:, 0:4], dr["b_group"].partition_broadcast(128), writes=[(brep, 0)])
            fw.dma('sp', brep.t[:, 4:36], dr["b_expert_router"].partition_broadcast(128), writes=[(brep, 1)])
            X = [sb("X%d" % i, [128, D], F32) for i in range(4)]
            self.xn = sb("xn", [128, D], F32)
            self.ss = sb("ss", [128, 1], F32)
            hT = sb("hT", [128, 8, W], BF16)
            rwT = sb("rwT", [128, 4, W], BF16)
            atT = sb("atT", [128, 4, W], BF16)
            Lt = sb("Lt", [128, W], BF16)
            T = {n: sb("t_" + n, [128, W], F32) for n in ("a", "kkr", "kk", "e1", "e2", "tmp")}
            lg = sb("lg", [128, 36], F32)
            comb = sb("comb", [128, 4, 32], F32)
            rt = {n: sb("r_" + n, [128, 1], F32) for n in ("gmax", "ngmax", "sumg", "gw", "nm1", "e2", "den", "w1", "w2")}
            oh = sb("oh", [128, 4], F32)
            eg = sb("eg", [128, 4], F32)
            elm = sb("elm", [128, 32], F32)
            top8 = sb("top8", [128, 8], F32)
            c1 = sb("c1", [128, 32], F32)
            sgt = T["tmp"]
            Aff = sb("Aff", [128, 2, W], BF16)
            self.junk = Aff
            Wd = [sb("Wd%d" % i, [128, 2, D], BF16) for i in range(1)]
            pT = sb("pT", [128, 2, W], BF16)
            pin = sb("pin", [128, 256], F32)
            yst = self.xn
            P_ = dict(identf=identf, g_mix=g_mix, g_ffn=g_ffn, g_ple=g_ple, gfin=gfin, again=again, w0rep=w0rep, LW=LW,
                      X=X, hT=hT, rwT=rwT, atT=atT, Lt=Lt, T=T)
            self.P_ = P_

            roll_ent = fw.new_dsem("d_roll")
            nrows = MAXWIN - 8
            piece = nrows // 4
            for j in range(NSEQ if 'roll' not in SKIP else 0):
                for src, dst in ((dr["kc"], dr["kws"]), (dr["vc"], dr["vws"])):
                    def mkroll(e, j=j, src=src, dst=dst):
                        return [e.dma_start(out=dst[j, a:a + piece, :], in_=src[j, a + 8:a + piece + 8, :])
                                for a in range(0, nrows, piece)]
                    fw.dma('act', None, None, fn=mkroll, n=4, out_dram=True, ent=roll_ent)

            pstk = ExitStack()
            fw.stk = pstk
            cI, cS, cR = sb("cI", [64, 64], F32), sb("cS", [64, 64], F32), sb("cR", [64, 64], F32)
            mk = sb("mk", [64, 320], F32)
            bones = sb("bones", [128, 128], BF16)
            for t_, n_ in ((cI, "cI"), (cS, "cS"), (cR, "cR"), (mk, "mk")):
                fw.dma('sp', t_.t[:, :], dr[n_], writes=[t_])
            fw.dma('pool', bones.t[:, :], dr["bones"], writes=[bones])
            identb = sb("identb", [64, 64], BF16)
            self.cp(identb.t[:, :], identf.t[0:64, 0:64], [identf], [identb])
            mu = self.vec_load("mu", 13)
            w0, a0, k_k, k_a = self.vec_load("w0", 4), self.vec_load("a0", 4), self.vec_load("k_k", 4), self.vec_load("k_a", 4)
            r_k, ln_w, ln_b = self.vec_load("r_k", 4), self.vec_load("ln_x_w", 4), self.vec_load("ln_x_b", 4)
            omka = sb("omka", [128, 4], F32)
            self.ts(omka.t[:, :], k_a.t[:, :], -1.0, 1.0, ALU.mult, ALU.add, [k_a], [omka])
            carry = sb("carry", [128, 13], F32)
            fw.op('pool', lambda e: e.memset(carry.t[:, :], 0.0), writes=[carry])
            ST = sb("ST", [128, 4, 64], F32)
            STb = sb("STb", [128, 4, 128], BF16)
            fw.op('pool', lambda e: e.memset(ST.t[:, :, :], 0.0), writes=[ST])
            fw.op('pool', lambda e: e.memset(STb.t[:, :, :], 0.0), writes=[STb])
            KT = sb("KT", [128, 4, RING * 128], BF16)
            V1 = sb("V1", [128, RING, 8, 65], BF16)
            fw.op('pool', lambda e: e.memset(KT.t[:, :, :], 0.0), writes=[KT])
            fw.op('pool', lambda e: e.memset(V1.t[:, :, :, :], 0.0), writes=[V1])
            QT = sb("QT", [128, 4, W], BF16)
            EM2 = [sb("EMh%d" % i, [128, NKB, 128], BF16) for i in range(1)]
            Pk = [sb("Pk%d" % i, [128, W + 1], F32) for i in range(2)]
            dlt = sb("dlt", [128, W], F32)
            xm = [sb("xm%d" % i, [128, W], F32) for i in range(3)]
            xml = xm[0]
            T = dict(T)
            T["kf"] = T["kkr"]
            T["bb"] = T["tmp"]
            T["e3"] = dlt
            sqb = sb("sqb", [128, W], BF16)
            t3b = sb("t3b", [128, W], BF16)
            bonus = sb("bonus", [128, W], F32)
            gg = sb("gg", [128, W], F32)
            ATt, RTt, KTt, BTt = (sb(n, [128, W], BF16) for n in ("ATt", "RTt", "KTt", "BTt"))
            Pend = sb("Pend", [128, 8], F32)
            sgwT = sb("sgwT", [64, 8, 128], F32)
            eR = sb("eR", [64, 8, 128], F32)
            Vtok, Ktok, Btok = (sb(n, [64, 8, 128], BF16) for n in ("Vtok", "Ktok", "Btok"))
            Ytok = sb("Ytok", [64, 8, 128], F32)
            Ach = [[sb("Ach%d_%d" % (i, h), [64, 320], BF16) for h in range(2)] for i in range(2)]
            Xi = [[sb("Xi%d_%d" % (i, p), [64, 2, 192], BF16) for p in range(2)] for i in range(2)]
            Zb = sb("Zb", [64, 128], BF16)
            Ub = sb("Ub", [64, 128], BF16)
            gs1, gs2 = sb("gs1", [64, 16], F32), sb("gs2", [64, 16], F32)
            Otok = [sb("Otok%d" % i, [128, RW], F32) for i in range(4)]
            orec = sb("orec", [128, 1], F32)
            Eb = sb("Eb", [128, 512], BF16)
            Pm = sb("Pm", [128, 512], BF16)
            stK = sb("stK", [128, 512], F32)
            stV = stK

            def wscr(name, shape):
                return nc.dram_tensor(name, list(shape), BF16, kind="Internal").ap(), Buf(None, name)
            kp = lambda ap: ap.rearrange("(k p) c -> p k c", p=128)
            win_bf, win_b = wscr("win_bf", [128, 8, IN_COLS])
            em_bf, em_b = wscr("em_bf", [8, 128, NKB, 128])
            wo_bf, wo_b = wscr("wo_bf", [128, 8, D])
            wgu_bf, wgu_b = wscr("wgu_bf", [32, 128, 8, 512])
            wd_bf, wd_b = wscr("wd_bf", [32, 128, 2, D])
            wpg_bf, wpg_b = wscr("wpg_bf", [128, 8, D])
            wpl_bf, wpl_b = wscr("wpl_bf", [128, 2, D])
            for c0 in range(0, IN_COLS, 640):
                fw.dma('pool', win_bf[:, :, c0:c0 + 640], kp(dr["w_in"])[:, :, c0:c0 + 640], writes=[(win_b, c0)], nc_ok=True)
            for h in range(8):
                fw.dma('pool', em_bf[h], dr["em"][h], writes=[(em_b, h)])
            for hf in range(2):
                fw.dma('pool', wo_bf[:, :, hf * 512:(hf + 1) * 512], kp(dr["w_out"])[:, :, hf * 512:(hf + 1) * 512], writes=[(wo_b, hf)], nc_ok=True)
            for ex in range(32):
                fw.dma('pool', wgu_bf[ex][:, :, 0:256], kp(dr["w_gate"][ex]), writes=[(wgu_b, (ex, 0))], nc_ok=True)
                fw.dma('pool', wgu_bf[ex][:, :, 256:512], kp(dr["w_up"][ex]), writes=[(wgu_b, (ex, 1))], nc_ok=True)
                fw.dma('pool', wd_bf[ex], kp(dr["w_down"][ex]), writes=[(wd_b, ex)], nc_ok=True)
            for hf in range(2):
                fw.dma('pool', wpg_bf[:, :, hf * 512:(hf + 1) * 512], kp(dr["w_ple_gate"])[:, :, hf * 512:(hf + 1) * 512], writes=[(wpg_b, hf)], nc_ok=True)
            fw.dma('pool', wpl_bf, kp(dr["w_ple"]), writes=[wpl_b], nc_ok=True)
            win_v = win_bf

            def tail(nblk, pp_ap, y_ap):
                nt = nblk * 128
                wo = [self.wload(wo_bf[:, :, hf * 512:(hf + 1) * 512], 8, 512, reads=[(wo_b, hf)]) for hf in range(2)]
                for blk in range(nblk):
                    bs = slice(blk * 128, (blk + 1) * 128)
                    for hf in range(2):
                        pb = self.bank()
                        for k in range(8):
                            lhs = rwT.t[:, k, bs] if k < 4 else atT.t[:, k - 4, bs]
                            self.mm(pb.t[:, :], lhs, wo[hf].t[:, k, :], k == 0, k == 7, [rwT, atT, wo[hf]], [pb])
                        self.tt(X[blk].t[:, hf * 512:(hf + 1) * 512], X[blk].t[:, hf * 512:(hf + 1) * 512], pb.t[:, :], ALU.add,
                                [pb, X[blk]], [X[blk]])

                self.norm_T(X[0:nblk], g_ffn, hT)
                for blk in range(nblk):
                    bs = slice(blk * 128, (blk + 1) * 128)
                    pb = self.bank()
                    for k in range(8):
                        self.mm(pb.t[:, 0:36], hT.t[:, k, bs], Wrt.t[:, k, :], k == 0, k == 7, [hT, Wrt], [pb])
                    self.tt(lg.t[:, :], pb.t[:, 0:36], brep.t[:, :], ALU.add, [pb, brep], [lg])
                    fw.op('dve', lambda e: e.reduce_max(rt["gmax"].t[:, :], lg.t[:, 0:4], AX.X), reads=[lg], writes=[rt["gmax"]])
                    self.ts(oh.t[:, :], lg.t[:, 0:4], rt["gmax"].t[:, 0:1], None, ALU.is_equal, None, [lg, rt["gmax"]], [oh])
                    self.ts(rt["ngmax"].t[:, :], rt["gmax"].t[:, :], -1.0, None, ALU.mult, None, [rt["gmax"]], [rt["ngmax"]])
                    self.act(eg.t[:, :], lg.t[:, 0:4], AF.Exp, [lg, rt["ngmax"]], [eg, rt["sumg"]], bias=rt["ngmax"].t[:, 0:1],
                             accum=rt["sumg"].t[:, :])
                    fw.op('dve', lambda e: e.reciprocal(rt["gw"].t[:, :], rt["sumg"].t[:, :]), reads=[rt["sumg"]], writes=[rt["gw"]])
                    self.ts(oh.t[:, :], oh.t[:, :], 1e30, -1e30, ALU.mult, ALU.add, [oh], [oh])
                    self.tt(elm.t[:, :].rearrange("p (g e) -> p g e", g=4), lg.t[:, 4:36].rearrange("p (g e) -> p g e", g=4),
                            oh.t[:, :].unsqueeze(2).to_broadcast([128, 4, 8]), ALU.add, [lg, oh], [elm])
                    fw.op('dve', lambda e: e.max(top8.t[:, :], elm.t[:, :]), reads=[elm], writes=[top8])
                    self.ts(rt["nm1"].t[:, :], top8.t[:, 0:1], -1.0, None, ALU.mult, None, [top8], [rt["nm1"]])
                    self.act(rt["e2"].t[:, :], top8.t[:, 1:2], AF.Exp, [top8, rt["nm1"]], [rt["e2"]], bias=rt["nm1"].t[:, 0:1])
                    self.ts(rt["den"].t[:, :], rt["e2"].t[:, :], 1.0, None, ALU.add, None, [rt["e2"]], [rt["den"]])
                    fw.op('dve', lambda e: e.reciprocal(rt["den"].t[:, :], rt["den"].t[:, :]), reads=[rt["den"]], writes=[rt["den"]])
                    self.tt(rt["w1"].t[:, :], rt["den"].t[:, :], rt["gw"].t[:, :], ALU.mult, [rt["den"], rt["gw"]], [rt["w1"]])
                    self.tt(rt["w2"].t[:, :], rt["w1"].t[:, :], rt["e2"].t[:, :], ALU.mult, [rt["w1"], rt["e2"]], [rt["w2"]])
                    self.ts(c1.t[:, :], elm.t[:, :], top8.t[:, 0:1], rt["w1"].t[:, 0:1], ALU.is_equal, ALU.mult, [elm, top8, rt["w1"]], [c1])
                    self.ts(comb.t[:, blk, :], elm.t[:, :], top8.t[:, 1:2], rt["w2"].t[:, 0:1], ALU.is_equal, ALU.mult,
                            [elm, top8, rt["w2"]], [(comb, blk)])
                    self.tt(comb.t[:, blk, :], comb.t[:, blk, :], c1.t[:, :], ALU.add, [(comb, blk), c1], [(comb, blk)])
                for ex in range(32):
                    wgu = self.WP[self.wnext]
                    self.wnext = (self.wnext + 1) % len(self.WP)
                    fw.dma('sp', wgu.t[:, :, :], wgu_bf[ex], reads=[(wgu_b, (ex, 0)), (wgu_b, (ex, 1))], writes=[wgu])
                    wd = Wd[ex % len(Wd)]
                    fw.dma('sp', wd.t[:, :, :], wd_bf[ex], reads=[(wd_b, ex)], writes=[wd])
                    for fb in range(2):
                        gbk, ubk = self.bank(), self.bank()
                        for k in range(8):
                            self.mm(gbk.t[:, 0:nt], wgu.t[:, k, fb * 128:(fb + 1) * 128], hT.t[:, k, 0:nt], k == 0, k == 7, [wgu, hT], [gbk])
                        for k in range(8):
                            self.mm(ubk.t[:, 0:nt], wgu.t[:, k, 256 + fb * 128:256 + (fb + 1) * 128], hT.t[:, k, 0:nt], k == 0, k == 7, [wgu, hT], [ubk])
                        self.act(sgt.t[:, 0:nt], gbk.t[:, 0:nt], AF.Silu, [gbk], [sgt])
                        self.tt(Aff.t[:, fb, 0:nt], sgt.t[:, 0:nt], ubk.t[:, 0:nt], ALU.mult, [sgt, ubk], [(Aff, fb)])
                    for blk in range(nblk):
                        bs = slice(blk * 128, (blk + 1) * 128)
                        for hf in range(2):
                            pb = self.bank()
                            for fb in range(2):
                                self.mm(pb.t[:, :], Aff.t[:, fb, bs], wd.t[:, fb, hf * 512:(hf + 1) * 512], fb == 0, fb == 1, [Aff, wd], [pb])
                            xs_ = X[blk].t[:, hf * 512:(hf + 1) * 512]
                            self.stt(xs_, pb.t[:, :], comb.t[:, blk, ex:ex + 1], xs_, ALU.mult, ALU.add, [pb, (comb, blk), X[blk]], [X[blk]])

                self.norm_T(X[0:nblk], g_ple, hT)
                for blk in range(nblk):
                    fw.dma('sp', pin.t[:, :], pp_ap[blk * 128:(blk + 1) * 128, :], writes=[pin])
                    pb = self.bank()
                    for k2 in range(2):
                        self.tr(pb.t[:, k2 * 128:(k2 + 1) * 128], pin.t[:, k2 * 128:(k2 + 1) * 128], identf.t[:, :], [pin, identf], [pb])
                    self.cp(pT.t[:, :, blk * 128:(blk + 1) * 128], pb.t[:, 0:256].rearrange("p (k t) -> p k t", k=2), [pb], [(pT, blk)])
                for hf in range(2):
                    wpg = self.wload(wpg_bf[:, :, hf * 512:(hf + 1) * 512], 8, 512, reads=[(wpg_b, hf)])
                    wpl = self.wload(wpl_bf[:, :, hf * 512:(hf + 1) * 512], 2, 512, reads=[wpl_b])
                    for blk in range(nblk):
                        bs = slice(blk * 128, (blk + 1) * 128)
                        gbk, pbk = self.bank(), self.bank()
                        for k in range(8):
                            self.mm(gbk.t[:, :], hT.t[:, k, bs], wpg.t[:, k, :], k == 0, k == 7, [hT, wpg], [gbk])
                        for k2 in range(2):
                            self.mm(pbk.t[:, :], pT.t[:, k2, bs], wpl.t[:, k2, :], k2 == 0, k2 == 1, [pT, wpl], [pbk])
                        self.act(sgt.t[:, :], gbk.t[:, :], AF.Sigmoid, [gbk], [sgt])
                        self.tt(sgt.t[:, :], sgt.t[:, :], pbk.t[:, :], ALU.mult, [sgt, pbk], [sgt])
                        xs_ = X[blk].t[:, hf * 512:(hf + 1) * 512]
                        self.tt(xs_, xs_, sgt.t[:, :], ALU.add, [X[blk], sgt], [X[blk]])
                for blk in range(nblk):
                    ss = self.ss
                    Xb = X[blk]
                    self.act(self.junk.t[:, :, :].rearrange("p a b -> p (a b)"), Xb.t[:, :], AF.Square, [Xb], [self.junk, ss], accum=ss.t[:, :])
                    self.ts(ss.t[:, :], ss.t[:, :], 1.0 / D, RMS_EPS, ALU.mult, ALU.add, [ss], [ss])
                    self.act(ss.t[:, :], ss.t[:, :], AF.Sqrt, [ss], [ss])
                    fw.op('dve', lambda e: e.reciprocal(self.ss.t[:, :], self.ss.t[:, :]), reads=[ss], writes=[ss])
                    self.stt(yst.t[:, :], Xb.t[:, :], ss.t[:, 0:1], gfin.t[:, :], ALU.mult, ALU.mult, [Xb, ss, gfin], [yst])
                    fw.dma('sp', y_ap[blk * 128:(blk + 1) * 128, :], yst.t[:, :], reads=[yst], out_dram=True)

            for w in range(nw if self.stages >= 0.05 else 0):
              try:
                  t0 = w * W
                  for blk in range(4):
                      fw.dma('sp', X[blk].t[:, :], xp[t0 + blk * 128:t0 + (blk + 1) * 128, :], writes=[X[blk]])
                  self.ck(0.1)
                  self.norm_T(X, g_mix, hT)
                  self.ck(0.2)
                  self.dbg("hT", hT.t[:, :, :], None, [hT]) if False else None

                  def rw_block(ci, col0, dst, wt, wc0):
                      pb = self.bank()
                      for k in range(8):
                          self.mm(pb.t[:, :], wt.t[:, k, wc0:wc0 + 128], hT.t[:, k, :], k == 0, k == 7, [wt, hT], [pb])
                      P = Pk[ci % 2]
                      self.cp(P.t[:, 1:W + 1], pb.t[:, :], [pb], [(P, 1)], eng='act')
                      self.cp(P.t[:, 0:1], carry.t[:, ci:ci + 1], [(carry, ci)], [(P, 0)])
                      self.cp(carry.t[:, ci:ci + 1], P.t[:, W:W + 1], [(P, 1)], [(carry, ci)])
                      self.tt(dlt.t[:, :], P.t[:, 0:W], P.t[:, 1:W + 1], ALU.subtract, [P], [dlt])
                      self.stt(dst.t[:, :], dlt.t[:, :], mu.t[:, ci:ci + 1], P.t[:, 1:W + 1], ALU.mult, ALU.add,
                               [dlt, mu, P], [dst])

                  wl = self.wload(win_v[:, :, 1536:1664], 8, 128, reads=[win_b])
                  rw_block(12, 1536, xml, wl, 0)
                  self.act(Lt.t[0:32, :], xml.t[0:32, :], AF.Tanh, [xml], [(Lt, 0)])
                  self.cp(Lt.t[32:64, :], xml.t[32:64, :], [xml], [(Lt, 1)])
                  self.act(Lt.t[64:128, :], xml.t[64:128, :], AF.Sigmoid, [xml], [(Lt, 2)])

                  self.ck(0.3)
                  for hp in range(4):
                      wt3 = self.WP[self.wnext]
                      self.wnext = (self.wnext + 1) % len(self.WP)
                      for i3 in range(3):
                          c0 = i3 * 512 + hp * 128
                          fw.dma('sp', wt3.t[:, :, i3 * 128:(i3 + 1) * 128], win_v[:, :, c0:c0 + 128], reads=[win_b], writes=[(wt3, i3)], nc_ok=True)
                      rw_block(hp, hp * 128, xm[0], wt3, 0)
                      rw_block(4 + hp, 512 + hp * 128, xm[1], wt3, 128)
                      rw_block(8 + hp, 1024 + hp * 128, xm[2], wt3, 256)
                      self.ck(0.4)
                      xr, xk, xv = xm
                      hc = slice(hp * 128, (hp + 1) * 128)
                      pb = self.bank()
                      self.mm(pb.t[:, :], LW.t[32:64, hc], Lt.t[32:64, :], True, True, [LW, Lt], [pb])
                      self.act(T["a"].t[:, :], pb.t[:, :], AF.Sigmoid, [pb, a0], [T["a"]], bias=a0.t[:, hp:hp + 1])
                      pb = self.bank()
                      self.mm(pb.t[:, :], LW.t[64:128, hc], Lt.t[64:128, :], True, True, [LW, Lt], [pb])
                      self.cp(gg.t[:, :], pb.t[:, :], [pb], [gg], eng='act')
                      for half in range(2):
                          pb = self.bank()
                          for cc in range(4):
                              c = half * 4 + cc
                              self.mm(pb.t[0:64, cc * 128:(cc + 1) * 128], Lt.t[0:32, c * 64:(c + 1) * 64], LW.t[0:32, hc],
                                      True, True, [LW, Lt], [pb])
                          self.tt(sgwT.t[:, half * 4:half * 4 + 4, :], pb.t[0:64, :].rearrange("p (c f) -> p c f", c=4),
                                  w0rep.t[0:64, hc].unsqueeze(1).to_broadcast([64, 4, 128]), ALU.add,
                                  [pb, w0rep], [(sgwT, half)])
                          self.act(sgwT.t[:, half * 4:half * 4 + 4, :], sgwT.t[:, half * 4:half * 4 + 4, :], AF.Sigmoid,
                                   [(sgwT, half)], [(sgwT, half)])
                      self.ck(0.5)
                      pinc, pexc = self.bank(), self.bank()
                      for c in range(8):
                          self.mm(pinc.t[:, c * 64:(c + 1) * 64], sgwT.t[:, c, :], cI.t[:, :], True, True, [sgwT, cI], [pinc])
                      for c in range(8):
                          self.mm(pexc.t[:, c * 64:(c + 1) * 64], sgwT.t[:, c, :], cS.t[:, :], True, True, [sgwT, cS], [pexc])
                      self.act(T["e1"].t[:, :], pinc.t[:, :], AF.Exp, [pinc], [T["e1"]])
                      self.act(T["e2"].t[:, :], pinc.t[:, :], AF.Exp, [pinc], [T["e2"]], scale=-1.0)
                      self.act(T["e3"].t[:, :], pexc.t[:, :], AF.Exp, [pexc], [T["e3"]])
                      for half in range(2):
                          pb = self.bank()
                          for cc in range(4):
                              c = half * 4 + cc
                              self.mm(pb.t[0:64, cc * 128:(cc + 1) * 128], cR.t[:, :], sgwT.t[:, c, :], True, True, [sgwT, cR], [pb])
                          self.act(eR.t[:, half * 4:half * 4 + 4, :], pb.t[0:64, :].rearrange("p (c f) -> p c f", c=4), AF.Exp,
                                   [pb], [(eR, half)])
                      self.cp(Pend.t[:, :], T["e1"].t[:, 63::64], [T["e1"]], [Pend])
                      self.ck(0.6)
                      self.ts(T["kkr"].t[:, :], xk.t[:, :], k_k.t[:, hp:hp + 1], None, ALU.mult, None, [xk, k_k], [T["kkr"]])
                      self.act(sqb.t[:, :], T["kkr"].t[:, :], AF.Square, [T["kkr"]], [sqb])
                      pb = self.bank()
                      self.mm(pb.t[:, :], bones.t[:, :], sqb.t[:, :], True, True, [bones, sqb], [pb])
                      self.act(T["tmp"].t[:, :], pb.t[:, :], AF.Sqrt, [pb], [T["tmp"]])
                      self.ts(T["tmp"].t[:, :], T["tmp"].t[:, :], 1e-12, None, ALU.max, None, [T["tmp"]], [T["tmp"]])
                      fw.op('dve', lambda e: e.reciprocal(T["tmp"].t[:, :], T["tmp"].t[:, :]), reads=[T["tmp"]], writes=[T["tmp"]])
                      self.tt(T["kk"].t[:, :], T["kkr"].t[:, :], T["tmp"].t[:, :], ALU.mult, [T["kkr"], T["tmp"]], [T["kk"]])
                      self.ts(T["tmp"].t[:, :], T["a"].t[:, :], k_a.t[:, hp:hp + 1], omka.t[:, hp:hp + 1], ALU.mult, ALU.add,
                              [T["a"], k_a, omka], [T["tmp"]])
                      self.tt(T["kf"].t[:, :], xk.t[:, :], T["tmp"].t[:, :], ALU.mult, [xk, T["tmp"]], [T["kf"]])
                      self.tt(T["bb"].t[:, :], T["kk"].t[:, :], T["a"].t[:, :], ALU.mult, [T["kk"], T["a"]], [T["bb"]])
                      self.stt(ATt.t[:, :], T["kk"].t[:, :], -1.0, T["e3"].t[:, :], ALU.mult, ALU.mult, [T["kk"], T["e3"]], [ATt])
                      self.tt(RTt.t[:, :], xr.t[:, :], T["e1"].t[:, :], ALU.mult, [xr, T["e1"]], [RTt])
                      self.tt(KTt.t[:, :], T["kf"].t[:, :], T["e2"].t[:, :], ALU.mult, [T["kf"], T["e2"]], [KTt])
                      self.tt(BTt.t[:, :], T["bb"].t[:, :], T["e2"].t[:, :], ALU.mult, [T["bb"], T["e2"]], [BTt])
                      self.stt(t3b.t[:, :], xr.t[:, :], r_k.t[:, hp:hp + 1], T["kf"].t[:, :], ALU.mult, ALU.mult,
                               [xr, r_k, T["kf"]], [t3b])
                      pb = self.bank()
                      self.mm(pb.t[:, :], bones.t[:, :], t3b.t[:, :], True, True, [bones, t3b], [pb])
                      self.tt(bonus.t[:, :], pb.t[:, :], xv.t[:, :], ALU.mult, [pb, xv], [bonus])
                      self.ck(0.7)
                      for half in range(2):
                          for (srcT, dstT, mode) in ((T["kf"], Ktok, 1), (T["bb"], Btok, 1), (xv, Vtok, 0)):
                              pb = self.bank()
                              for cc in range(4):
                                  c = half * 4 + cc
                                  self.tr(pb.t[0:64, cc * 128:(cc + 1) * 128], srcT.t[:, c * 64:(c + 1) * 64], identf.t[:, :],
                                          [srcT, identf], [pb])
                              pv = pb.t[0:64, :].rearrange("p (c f) -> p c f", c=4)
                              if mode:
                                  self.tt(dstT.t[:, half * 4:half * 4 + 4, :], pv, eR.t[:, half * 4:half * 4 + 4, :], ALU.mult,
                                          [pb, (eR, half)], [(dstT, half)])
                              else:
                                  self.cp(dstT.t[:, half * 4:half * 4 + 4, :], pv, [pb], [(dstT, half)], eng='act')

                      self.ck(0.8)
                      def chunk_prep(c):
                          cc = slice(c * 64, (c + 1) * 64)
                          A = Ach[c % 2]
                          Xc = Xi[c % 2]
                          for h in range(2):
                              hr = slice(h * 64, (h + 1) * 64)
                              pb = self.bank()
                              ops = ((BTt, ATt), (ATt, BTt), (KTt, ATt), (BTt, RTt), (KTt, RTt))
                              for i, (l_, r_) in enumerate(ops):
                                  self.mm(pb.t[0:64, i * 64:(i + 1) * 64], l_.t[hr, cc], r_.t[hr, cc], True, True, [l_, r_], [pb])
                              self.tt(A[h].t[:, :], pb.t[0:64, 0:320], mk.t[:, :], ALU.mult, [pb, mk], [A[h]])
                              self.ck(0.81)
                              self.cp(Xc[0].t[:, h, 0:128], A[h].t[:, 0:128], [A[h]], [(Xc[0], (h, 'n'))], eng='act')
                              self.tt(Xc[0].t[:, h, 128:192], A[h].t[:, 0:64], identb.t[:, :], ALU.add, [A[h], identb], [(Xc[0], (h, 'm'))])
                          self.ck(0.82)
                          cur = 0
                          for ph in range(6):
                              self.ck(0.83 + 0.001 * ph)
                              pb = self.bank()
                              src, dstx = Xc[cur], Xc[1 - cur]
                              for h in range(2):
                                  N_, NT_, M_ = src.t[:, h, 0:64], src.t[:, h, 64:128], src.t[:, h, 128:192]
                                  o = h * 192
                                  if ph < 5 and 'sq' not in SKIP:
                                      self.mm(pb.t[0:64, o:o + 64], NT_, N_, True, True, [src], [pb])
                                      if 'sq2' not in SKIP:
                                          self.mm(pb.t[0:64, o + 64:o + 128], N_, NT_, True, True, [src], [pb])
                                  if ph >= 1 and 'mmM' not in SKIP:
                                      self.mm(pb.t[0:64, o + 128:o + 192], NT_, M_, True, True, [src], [pb])
                              pv = pb.t[0:64, 0:384].rearrange("p (h x) -> p h x", h=2)
                              if ph < 5 and 'actcp' not in SKIP:
                                  self.cp(dstx.t[:, :, 0:128], pv[:, :, 0:128], [pb], [(dstx, (0, 'n')), (dstx, (1, 'n'))], eng='act')
                              if ph >= 1 and 'ttM' in SKIP:
                                  pass
                              elif ph >= 1:
                                  self.tt(dstx.t[:, :, 128:192], pv[:, :, 128:192], src.t[:, :, 128:192], ALU.add, [pb, src], [(dstx, (0, 'm')), (dstx, (1, 'm'))])
                              elif 'dvecp' not in SKIP:
                                  self.cp(dstx.t[:, :, 128:192], src.t[:, :, 128:192], [src], [(dstx, (0, 'm')), (dstx, (1, 'm'))])
                              cur = 1 - cur
                          return A, Xc[cur]

                      prep = chunk_prep(0)
                      for c in range(8):
                          A, Xf = prep
                          if c + 1 < 8:
                              nxt = chunk_prep(c + 1)
                          cc = slice(c * 64, (c + 1) * 64)
                          self.ck(0.84)
                          zb, ub, yb, sbk = self.bank(), self.bank(), self.bank(), self.bank()
                          self.mm(zb.t[0:64, 0:128], ATt.t[:, cc], STb.t[:, hp, :], True, False, [ATt, (STb, hp)], [zb])
                          for h in range(2):
                              hr = slice(h * 64, (h + 1) * 64)
                              self.mm(zb.t[0:64, hr], A[h].t[:, 128:192], Vtok.t[:, c, hr], False, h == 1, [A[h], Vtok], [zb])
                          if 'zc' not in SKIP:
                              self.cp(Zb.t[:, :], zb.t[0:64, 0:128], [zb], [Zb], eng='act')
                          self.ck(0.85)
                          for h in range(2):
                              hr = slice(h * 64, (h + 1) * 64)
                              self.mm(ub.t[0:64, hr], Xf.t[:, h, 128:192], Zb.t[:, hr], True, True, [Xf, Zb], [ub])
                          self.cp(Ub.t[:, :], ub.t[0:64, 0:128], [ub], [Ub])
                          self.ck(0.86)
                          self.mm(yb.t[0:64, 0:128], RTt.t[:, cc], STb.t[:, hp, :], True, False, [RTt, (STb, hp)], [yb])
                          for h in range(2):
                              hr = slice(h * 64, (h + 1) * 64)
                              self.mm(yb.t[0:64, hr], A[h].t[:, 256:320], Vtok.t[:, c, hr], False, False, [A[h], Vtok], [yb])
                              self.mm(yb.t[0:64, hr], A[h].t[:, 192:256], Ub.t[:, hr], False, h == 1, [A[h], Ub], [yb])
                          self.cp(Ytok.t[:, c, :], yb.t[0:64, 0:128], [yb], [(Ytok, c)], eng='act')
                          self.ck(0.87)
                          self.mm(sbk.t[:, 0:128], Btok.t[:, c, :], Ub.t[:, :], True, False, [Btok, Ub], [sbk])
                          self.mm(sbk.t[:, 0:128], Ktok.t[:, c, :], Vtok.t[:, c, :], False, True, [Ktok, Vtok], [sbk])
                          for h in range(2):
                              hr = slice(h * 64, (h + 1) * 64)
                              self.stt(ST.t[hr, hp, :], ST.t[hr, hp, :], Pend.t[hr, c:c + 1], sbk.t[hr, hr], ALU.mult, ALU.add,
                                       [(ST, hp), Pend, sbk], [(ST, hp)])
                          for h in range(2):
                              hr = slice(h * 64, (h + 1) * 64)
                              self.cp(STb.t[hr, hp, hr], ST.t[hr, hp, :], [(ST, hp)], [(STb, hp)], eng='act')
                          if c + 1 < 8:
                              prep = nxt

                      self.ck(0.9)
                      yv = Ytok.t[:, :, :].rearrange("p c (h v) -> p (c h) v", h=2)
                      fw.op('dve', lambda e: e.reduce_sum(gs1.t[:, :], yv, AX.X), reads=[Ytok], writes=[gs1])
                      ysq = self.xn
                      ysq3 = ysq.t[0:64, :].rearrange("p (c f) -> p c f", c=8)
                      self.act(ysq3, Ytok.t[:, :, :], AF.Square, [Ytok], [ysq])
                      ysv = ysq.t[0:64, :].rearrange("p (c h v) -> p (c h) v", c=8, h=2)
                      fw.op('dve', lambda e: e.reduce_sum(gs2.t[:, :], ysv, AX.X), reads=[ysq], writes=[gs2])
                      self.ts(gs1.t[:, :], gs1.t[:, :], 1.0 / 64, None, ALU.mult, None, [gs1], [gs1])
                      self.tt(ysq.t[0:64, 0:16], gs1.t[:, :], gs1.t[:, :], ALU.mult, [gs1], [ysq])
                      self.stt(gs2.t[:, :], gs2.t[:, :], 1.0 / 64, ysq.t[0:64, 0:16], ALU.mult, ALU.subtract, [gs2, ysq], [gs2])
                      self.ts(gs2.t[:, :], gs2.t[:, :], GN_EPS, None, ALU.add, None, [gs2], [gs2])
                      self.act(gs2.t[:, :], gs2.t[:, :], AF.Sqrt, [gs2], [gs2])
                      fw.op('dve', lambda e: e.reciprocal(gs2.t[:, :], gs2.t[:, :]), reads=[gs2], writes=[gs2])
                      mb = gs1.t[:, :].unsqueeze(2).to_broadcast([64, 16, 64])
                      rb = gs2.t[:, :].unsqueeze(2).to_broadcast([64, 16, 64])
                      self.tt(yv, yv, mb, ALU.subtract, [Ytok, gs1], [Ytok])
                      self.tt(yv, yv, rb, ALU.mult, [Ytok, gs2], [Ytok])
                      pb = self.bank()
                      for c in range(8):
                          self.tr(pb.t[:, c * 64:(c + 1) * 64], Ytok.t[:, c, :], identf.t[0:64, 0:64], [Ytok, identf], [pb])
                      self.act(T["tmp"].t[:, :], pb.t[:, :], AF.Identity, [pb, ln_w, ln_b], [T["tmp"]],
                               bias=ln_b.t[:, hp:hp + 1], scale=ln_w.t[:, hp:hp + 1])
                      self.tt(T["tmp"].t[:, :], T["tmp"].t[:, :], bonus.t[:, :], ALU.add, [T["tmp"], bonus], [T["tmp"]])
                      self.tt(rwT.t[:, hp, :], T["tmp"].t[:, :], gg.t[:, :], ALU.mult, [T["tmp"], gg], [(rwT, hp)])

                  if self.stages < 2:
                      continue
                  wq_ = self.wload(win_v[:, :, QOFF:QOFF + 512], 8, 512, reads=[win_b])
                  for cb in range(4):
                      pb = self.bank()
                      for k in range(8):
                          self.mm(pb.t[:, :], wq_.t[:, k, cb * 128:(cb + 1) * 128], hT.t[:, k, :], k == 0, k == 7, [wq_, hT], [pb])
                      self.act(QT.t[:, cb, :], pb.t[:, :], AF.Copy, [pb], [(QT, cb)], scale=0.125)
                  wk2 = self.wload(win_v[:, :, KOFF:KOFF + 512], 8, 512, reads=[win_b])
                  slot0 = (w * 4) % RING
                  for cb in range(4):
                      pb = self.bank()
                      for k in range(8):
                          self.mm(pb.t[:, :], wk2.t[:, k, cb * 128:(cb + 1) * 128], hT.t[:, k, :], k == 0, k == 7, [wk2, hT], [pb])
                      self.cp(KT.t[:, cb, slot0 * 128:slot0 * 128 + W], pb.t[:, :], [pb], [KT], eng='act' if cb % 2 else 'dve')
                  wv2 = self.wload(win_v[:, :, VOFF:VOFF + 512], 8, 512, reads=[win_b])
                  for (wt, isv) in ((wk2, 0), (wv2, 1)):
                      for blk in range(4):
                          tok0 = t0 + blk * 128
                          need_out = tok0 >= seq - keep
                          if not isv and not need_out:
                              continue
                          pb = self.bank()
                          for k in range(8):
                              self.mm(pb.t[:, :], hT.t[:, k, blk * 128:(blk + 1) * 128], wt.t[:, k, :], k == 0, k == 7, [wt, hT], [pb])
                          if isv:
                              slot = (w * 4 + blk) % RING
                              self.cp(V1.t[:, slot, :, 1:65], pb.t[:, :].rearrange("p (h v) -> p h v", h=8), [pb], [V1], eng='act')
                              fw.op('pool', lambda e, slot=slot: e.memset(V1.t[:, slot, :, 0:1], 1.0), writes=[V1])
                          if need_out:
                              stg = stV if isv else stK
                              self.cp(stg.t[:, :], pb.t[:, :], [pb], [stg])
                              dst = (vwp if isv else kwp)[tok0 - (seq - keep):tok0 - (seq - keep) + 128, :]
                              fw.dma('sp', dst, stg.t[:, :], reads=[stg], out_dram=True)

                  for h in range(8):
                      hp, hr = h // 2, slice((h % 2) * 64, (h % 2) * 64 + 64)
                      EMh = EM2[0]
                      fw.dma('sp', EMh.t[:, :, :], em_bf[h], reads=[(em_b, h)], writes=[EMh])
                      for bq in range(4):
                          gbq = w * 4 + bq
                          ob = self.bank()
                          self.held.add(ob)
                          for g0 in range(0, NKB, 4):
                              n = min(4, NKB - g0)
                              sb_ = self.bank()
                              for jj in range(n):
                                  slot = (gbq - (g0 + jj)) % RING
                                  self.mm(sb_.t[:, jj * 128:(jj + 1) * 128], KT.t[hr, hp, slot * 128:(slot + 1) * 128],
                                          QT.t[hr, hp, bq * 128:(bq + 1) * 128], True, True, [KT, (QT, hp)], [sb_])
                              self.act(Eb.t[:, 0:n * 128], sb_.t[:, 0:n * 128], AF.Exp, [sb_], [Eb])
                              self.tt(Pm.t[:, 0:n * 128], Eb.t[:, 0:n * 128],
                                      EMh.t[:, g0:g0 + n, :].rearrange("p j q -> p (j q)"), ALU.mult, [Eb, EMh], [Pm])
                              for jj in range(n):
                                  j = g0 + jj
                                  slot = (gbq - j) % RING
                                  self.mm(ob.t[:, 0:65], Pm.t[:, jj * 128:(jj + 1) * 128], V1.t[:, slot, h, :],
                                          j == 0, j == NKB - 1, [Pm, V1], [ob])
                          self.held.discard(ob)
                          fw.op('dve', lambda e, ob=ob: e.reciprocal(orec.t[:, :], ob.t[:, 0:1]), reads=[ob], writes=[orec])
                          self.ts(Otok[bq].t[:, h * 64:(h + 1) * 64], ob.t[:, 1:65], orec.t[:, 0:1], None, ALU.mult, None,
                                  [ob, orec], [(Otok[bq], h)])
                  for bq in range(4):
                      Ot = Otok[bq]
                      ss = self.ss
                      self.act(self.junk.t[:, 0, :], Ot.t[:, :], AF.Square, [Ot], [self.junk, ss], accum=ss.t[:, :])
                      self.ts(ss.t[:, :], ss.t[:, :], 1.0 / RW, RMS_EPS, ALU.mult, ALU.add, [ss], [ss])
                      self.act(ss.t[:, :], ss.t[:, :], AF.Sqrt, [ss], [ss])
                      fw.op('dve', lambda e: e.reciprocal(self.ss.t[:, :], self.ss.t[:, :]), reads=[ss], writes=[ss])
                      self.stt(Ot.t[:, :], Ot.t[:, :], ss.t[:, 0:1], again.t[:, :], ALU.mult, ALU.mult, [Ot, ss, again], [Ot])
                      pb = self.bank()
                      for cb in range(4):
                          self.tr(pb.t[:, cb * 128:(cb + 1) * 128], Ot.t[:, cb * 128:(cb + 1) * 128], identf.t[:, :], [Ot, identf], [pb])
                      self.cp(atT.t[:, :, bq * 128:(bq + 1) * 128], pb.t[:, :].rearrange("p (c t) -> p c t", c=4), [pb], [(atT, bq)], eng='act')

                  if self.stages < 4:
                      continue
                  tail(4, pp[t0:t0 + W, :], yp[t0:t0 + W, :])
              except StopBuild:
                break

            fw.dma('sp', shp_o.rearrange("(k p) -> p k", p=128), carry.t[:, :], reads=[carry], out_dram=True, nc_ok=True)
            for hp in range(4):
                pb = self.bank()
                self.tr(pb.t[0:64, 0:128], ST.t[:, hp, :], identf.t[:, :], [ST, identf], [pb])
                self.cp(stK.t[0:64, 0:128], pb.t[0:64, 0:128], [pb], [stK])
                for h2 in range(2):
                    fw.dma('sp', wkvp[hp * 2 + h2], stK.t[0:64, h2 * 64:(h2 + 1) * 64], reads=[stK], out_dram=True)

            pstk.close()
            fw.barrier()
            sstk = ExitStack()
            fw.stk = sstk
            T = P_["T"]
            Xs = X[0]
            Ptok = sb("Ptok", [128, IN_COLS], F32)
            XM = sb("XM", [128, RW_COLS], F32)
            ATtok = sb("ATtok", [128, RW], F32)
            Gtok = sb("Gtok", [128, RW], F32)
            Btk = sb("Btk", [128, RW], F32)
            n8 = sb("n8", [128, 8], F32)
            m8 = sb("m8", [128, 8], F32)
            v8 = sb("v8", [128, 8], F32)
            QTs = sb("QTs", [128, 4, 128], BF16)
            KTn = sb("KTn", [128, 4, 128], BF16)
            Vn1 = sb("Vn1", [128, 8, 65], BF16)
            scrA_b = Buf(None, "scrA")
            scrB_b = Buf(None, "scrB")

            fw.dma('sp', Xs.t[:, :], xs, writes=[Xs])
            self.norm_T([Xs], g_mix, hT)
            for ci, c0 in enumerate(range(0, IN_COLS, 512)):
                cw = min(512, IN_COLS - c0)
                wl = self.wload(win_v[:, :, c0:c0 + cw], 8, cw, reads=[win_b])
                pb = self.bank()
                for k in range(8):
                    self.mm(pb.t[:, 0:cw], hT.t[:, k, 0:128], wl.t[:, k, 0:cw], k == 0, k == 7, [wl, hT], [pb])
                self.cp(Ptok.t[:, c0:c0 + cw], pb.t[:, 0:cw], [pb], [(Ptok, ci)], eng='act' if ci % 2 else 'dve')
            fw.dma('sp', shs, Ptok.t[7:128:8, 0:RW_COLS], reads=[Ptok], out_dram=True)
            for (off_, dst_) in ((KOFF, kws), (VOFF, vws)):
                def mknew(e, off_=off_, dst_=dst_):
                    return [e.dma_start(out=dst_[j, MAXWIN - 8:MAXWIN, :], in_=Ptok.t[8 * j:8 * j + 8, off_:off_ + 512])
                            for j in range(NSEQ)]
                fw.dma('sp', None, None, fn=mknew, n=NSEQ, reads=[Ptok], out_dram=True)
            for cb in range(4):
                pb = self.bank()
                self.tr(pb.t[:, 0:128], Ptok.t[:, QOFF + cb * 128:QOFF + (cb + 1) * 128], identf.t[:, :], [Ptok, identf], [pb])
                self.act(QTs.t[:, cb, :], pb.t[:, 0:128], AF.Copy, [pb], [(QTs, cb)], scale=0.125)
                pb = self.bank()
                self.tr(pb.t[:, 0:128], Ptok.t[:, KOFF + cb * 128:KOFF + (cb + 1) * 128], identf.t[:, :], [Ptok, identf], [pb])
                self.cp(KTn.t[:, cb, :], pb.t[:, 0:128], [pb], [(KTn, cb)])
            fw.op('pool', lambda e: e.memset(Vn1.t[:, :, 0:1], 1.0), writes=[Vn1])
            self.cp(Vn1.t[:, :, 1:65], Ptok.t[:, VOFF:VOFF + 512].rearrange("p (h v) -> p h v", h=8), [Ptok, Vn1], [Vn1])

            rstk = ExitStack()
            fw.stk = rstk
            S = sb("S", [128, 4096], F32)
            TM = [sb("TM%d" % i, [128, 4096], F32) for i in range(2)]
            RLh = sb("RLh", [128, 6, 8, 64], F32)
            Yrl = sb("Yrl", [128, 8, 64], F32)
            skk = sb("skk", [128, 64], F32)
            reps = {}
            for n_ in ("a0", "k_k", "k_a", "r_k", "ln_x_w", "ln_x_b"):
                reps[n_] = sb("rep_" + n_, [128, RW], F32)
                fw.dma('sp', reps[n_].t[:, :], dr[n_].partition_broadcast(128), writes=[reps[n_]])
            fw.dma('sp', S.t[:, :], swkv, writes=[S])
            mu_rep = TM[0]
            fw.dma('sp', mu_rep.t[:, 0:RW_COLS], dr["mu"].partition_broadcast(128), writes=[mu_rep])
            fw.dma('sp', XM.t[1:128, :], Ptok.t[0:127, 0:RW_COLS], reads=[Ptok], writes=[XM])
            fw.dma('sp', XM.t[0:128:8, :], sshift, writes=[XM])
            Pr = Ptok.t[:, 0:RW_COLS]
            self.tt(XM.t[:, :], XM.t[:, :], Pr, ALU.subtract, [XM, Ptok], [XM])
            self.tt(XM.t[:, :], XM.t[:, :], mu_rep.t[:, 0:RW_COLS], ALU.mult, [XM, mu_rep], [XM])
            self.tt(XM.t[:, :], XM.t[:, :], Pr, ALU.add, [XM, Ptok], [XM])
            xr, xk, xv = XM.t[:, 0:512], XM.t[:, 512:1024], XM.t[:, 1024:1536]
            RLb = [X[1], X[1], X[2], X[2], X[3], X[3]]
            RL = [RLb[j].t[:, (j % 2) * 512:(j % 2) * 512 + 512] for j in range(6)]
            h3 = lambda ap: ap.rearrange("p (h n) -> p h n", h=8)
            bc3 = lambda b8: b8.t[:, :].unsqueeze(2).to_broadcast([128, 8, 64])
            pb = self.bank()
            self.tr(pb.t[:, 0:128], XM.t[:, 1536:1664], identf.t[:, :], [XM, identf], [pb])
            self.act(Lt.t[0:32, 0:128], pb.t[0:32, 0:128], AF.Tanh, [pb], [(Lt, 0)])
            self.cp(Lt.t[32:64, 0:128], pb.t[32:64, 0:128], [pb], [(Lt, 1)])
            self.act(Lt.t[64:128, 0:128], pb.t[64:128, 0:128], AF.Sigmoid, [pb], [(Lt, 2)])
            tA, tB, Atok, kkr = T["e1"], T["e2"], T["a"], T["kkr"]
            pb = self.bank()
            self.mm(pb.t[:, :], Lt.t[0:32, 0:128], LW.t[0:32, :], True, True, [Lt, LW], [pb])
            self.tt(tA.t[:, :], pb.t[:, :], w0rep.t[:, :], ALU.add, [pb, w0rep], [tA])
            self.act(tA.t[:, :], tA.t[:, :], AF.Sigmoid, [tA], [tA])
            self.act(RL[1], tA.t[:, :], AF.Exp, [tA], [X[1]], scale=-DECAY_C)
            pb = self.bank()
            self.mm(pb.t[:, :], Lt.t[32:64, 0:128], LW.t[32:64, :], True, True, [Lt, LW], [pb])
            self.tt(Atok.t[:, :], pb.t[:, :], reps["a0"].t[:, :], ALU.add, [pb, reps["a0"]], [Atok])
            self.act(Atok.t[:, :], Atok.t[:, :], AF.Sigmoid, [Atok], [Atok])
            pb = self.bank()
            self.mm(pb.t[:, :], Lt.t[64:128, 0:128], LW.t[64:128, :], True, True, [Lt, LW], [pb])
            self.cp(Gtok.t[:, :], pb.t[:, :], [pb], [Gtok], eng='act')
            self.tt(kkr.t[:, :], xk, reps["k_k"].t[:, :], ALU.mult, [XM, reps["k_k"]], [kkr])
            self.act(tB.t[:, :], kkr.t[:, :], AF.Square, [kkr], [tB])
            fw.op('dve', lambda e: e.reduce_sum(n8.t[:, :], h3(tB.t[:, :]), AX.X), reads=[tB], writes=[n8])
            self.act(n8.t[:, :], n8.t[:, :], AF.Sqrt, [n8], [n8])
            self.ts(n8.t[:, :], n8.t[:, :], 1e-12, None, ALU.max, None, [n8], [n8])
            fw.op('dve', lambda e: e.reciprocal(n8.t[:, :], n8.t[:, :]), reads=[n8], writes=[n8])
            self.tt(h3(RL[4]), h3(kkr.t[:, :]), bc3(n8), ALU.mult, [kkr, n8], [X[3]])
            self.ts(tA.t[:, :], Atok.t[:, :], -1.0, None, ALU.add, None, [Atok], [tA])
            self.tt(tA.t[:, :], tA.t[:, :], reps["k_a"].t[:, :], ALU.mult, [tA, reps["k_a"]], [tA])
            self.ts(tA.t[:, :], tA.t[:, :], 1.0, None, ALU.add, None, [tA], [tA])
            self.tt(RL[2], xk, tA.t[:, :], ALU.mult, [XM, tA], [X[2]])
            self.tt(RL[5], RL[4], Atok.t[:, :], ALU.mult, [X[3], Atok], [X[3]])
            self.cp(RL[0], xr, [XM], [X[1]])
            self.cp(RL[3], xv, [XM], [X[2]], eng='act')
            self.tt(tA.t[:, :], xr, RL[2], ALU.mult, [XM, X[2]], [tA])
            self.tt(tA.t[:, :], tA.t[:, :], reps["r_k"].t[:, :], ALU.mult, [tA, reps["r_k"]], [tA])
            fw.op('dve', lambda e: e.reduce_sum(m8.t[:, :], h3(tA.t[:, :]), AX.X), reads=[tA], writes=[m8])
            self.tt(h3(Btk.t[:, :]), h3(xv), bc3(m8), ALU.mult, [XM, m8], [Btk])
            for i3 in range(3):
                fw.dma('sp', scrA[:, i3 * 1024:(i3 + 1) * 1024], X[1 + i3].t[:, :], reads=[X[1 + i3]], writes=[(scrA_b, i3)])
            scrA_v = scrA.rearrange("p (j h n) -> p j h n", j=6, h=8)
            for b in range(NSEQ):
                def mkrl(e, b=b):
                    return [e.dma_start(out=RLh.t[b * 8:(b + 1) * 8, j, :, :],
                                        in_=scrA_v[b * 8:(b + 1) * 8, j, :, :].rearrange("t h n -> h t n"),
                                        allow_slow_non_contiguous=True) for j in range(6)]
                fw.dma('sp' if b % 2 else 'act', None, None, fn=mkrl, n=6, reads=[scrA_b], writes=[(RLh, b)])
            S3 = S.t[:, :].rearrange("p (v k) -> p v k", v=64)
            TM3 = [t_.t[:, :].rearrange("p (v k) -> p v k", v=64) for t_ in TM]
            kbc = lambda j, t: RLh.t[:, j, t, :].unsqueeze(1).to_broadcast([128, 64, 64])
            vbc = lambda ap: ap.unsqueeze(2).to_broadcast([128, 64, 64])
            for t in range(8 if 'rec' not in SKIP else 0):
                self.tt(TM3[0], S3, kbc(4, t), ALU.mult, [S, RLh], [TM[0]])
                fw.op('dve', lambda e: e.reduce_sum(skk.t[:, :], TM3[0], AX.X), reads=[TM[0]], writes=[skk])
                self.tt(S3, S3, kbc(1, t), ALU.mult, [S, RLh], [S])
                self.tt(TM3[1], vbc(skk.t[:, :]), kbc(5, t), ALU.mult, [skk, RLh], [TM[1]], eng='pool')
                self.tt(S3, S3, TM3[1], ALU.subtract, [S, TM[1]], [S])
                self.tt(TM3[0], vbc(RLh.t[:, 3, t, :]), kbc(2, t), ALU.mult, [RLh], [TM[0]], eng='pool')
                self.tt(S3, S3, TM3[0], ALU.add, [S, TM[0]], [S])
                self.tt(TM3[1], S3, kbc(0, t), ALU.mult, [S, RLh], [TM[1]])
                fw.op('dve', lambda e, t=t: e.reduce_sum(Yrl.t[:, t, :], TM3[1], AX.X), reads=[TM[1]], writes=[(Yrl, t)])
            fw.dma('sp', wkvs, S.t[:, :], reads=[S], out_dram=True)
            fw.dma('sp', scrB, Yrl.t[:, :, :].rearrange("p t v -> p (t v)"), reads=[Yrl], writes=[scrB_b])
            Ytk = T["kk"]
            scrB_v = scrB.rearrange("p (t v) -> p t v", t=8)
            for b in range(NSEQ):
                fw.dma('sp' if b % 2 else 'act', Ytk.t[b * 8:(b + 1) * 8, :].rearrange("t (h v) -> t h v", h=8),
                       scrB_v[b * 8:(b + 1) * 8, :, :].rearrange("h t v -> t h v"),
                       reads=[scrB_b], writes=[(Ytk, b)], nc_ok=True)
            yvs = h3(Ytk.t[:, :])
            fw.op('dve', lambda e: e.reduce_sum(m8.t[:, :], yvs, AX.X), reads=[Ytk], writes=[m8])
            self.act(tB.t[:, :], Ytk.t[:, :], AF.Square, [Ytk], [tB])
            fw.op('dve', lambda e: e.reduce_sum(v8.t[:, :], h3(tB.t[:, :]), AX.X), reads=[tB], writes=[v8])
            self.ts(m8.t[:, :], m8.t[:, :], 1.0 / 64, None, ALU.mult, None, [m8], [m8])
            self.tt(n8.t[:, :], m8.t[:, :], m8.t[:, :], ALU.mult, [m8], [n8])
            self.stt(v8.t[:, :], v8.t[:, :], 1.0 / 64, n8.t[:, :], ALU.mult, ALU.subtract, [v8, n8], [v8])
            self.ts(v8.t[:, :], v8.t[:, :], GN_EPS, None, ALU.add, None, [v8], [v8])
            self.act(v8.t[:, :], v8.t[:, :], AF.Sqrt, [v8], [v8])
            fw.op('dve', lambda e: e.reciprocal(v8.t[:, :], v8.t[:, :]), reads=[v8], writes=[v8])
            self.tt(yvs, yvs, bc3(m8), ALU.subtract, [Ytk, m8], [Ytk])
            self.tt(yvs, yvs, bc3(v8), ALU.mult, [Ytk, v8], [Ytk])
            self.tt(Ytk.t[:, :], Ytk.t[:, :], reps["ln_x_w"].t[:, :], ALU.mult, [Ytk, reps["ln_x_w"]], [Ytk])
            self.tt(Ytk.t[:, :], Ytk.t[:, :], reps["ln_x_b"].t[:, :], ALU.add, [Ytk, reps["ln_x_b"]], [Ytk])
            self.tt(Ytk.t[:, :], Ytk.t[:, :], Btk.t[:, :], ALU.add, [Ytk, Btk], [Ytk])
            self.tt(Ytk.t[:, :], Ytk.t[:, :], Gtok.t[:, :], ALU.mult, [Ytk, Gtok], [Ytk])
            pb = self.bank()
            for cb in range(4):
                self.tr(pb.t[:, cb * 128:(cb + 1) * 128], Ytk.t[:, cb * 128:(cb + 1) * 128], identf.t[:, :], [Ytk, identf], [pb])
            self.cp(rwT.t[:, :, 0:128], pb.t[:, :].rearrange("p (c t) -> p c t", c=4), [pb], [rwT], eng='act')
            rstk.close()
            fw.barrier()

            astk = ExitStack()
            fw.stk = astk
            EMs = sb("EMs", [128, 1024], BF16)
            EMn = sb("EMn", [128, 1024], BF16)
            fw.dma('pool', EMs.t[:, :], dr["ems"], writes=[EMs])
            fw.dma('pool', EMn.t[:, :], dr["emn"], writes=[EMn])
            Kld = [sb("Kld%d" % i, [128, 4, 512], F32) for i in range(2)]
            Vld = [sb("Vld%d" % i, [128, 4, 512], F32) for i in range(2)]
            KTg = [sb("KTg%d" % i, [128, 4, 512], BF16) for i in range(2)]
            V1s = [sb("V1s%d" % i, [128, 8, 8, 65], BF16) for i in range(2)]
            for v1 in V1s:
                fw.op('pool', lambda e, v1=v1: e.memset(v1.t[:, :, :, 0:1], 1.0), writes=[v1])
            Ebs = sb("Ebs", [128, 512], BF16)
            Pms = [sb("Pms%d" % i, [128, 512], BF16) for i in range(2)]
            Pmn = sb("Pmn", [128, 64], BF16)
            ostg = [sb("ostg%d" % i, [8, 512], F32) for i in range(2)]
            orec8 = sb("orec8", [8, 8], F32)
            Qbd = sb("Qbd", [128, NSEQ, 4, 64], BF16)
            fw.op('pool', lambda e: e.memset(Qbd.t[:, :, :, :], 0.0), writes=[Qbd])
            for hp in range(4):
                for h2 in range(2):
                    hr = slice(h2 * 64, (h2 + 1) * 64)
                    c0 = (2 * hp + h2) * 8
                    self.cp(Qbd.t[hr, :, hp, c0:c0 + 8], QTs.t[hr, hp, :].rearrange("p (b t) -> p b t", b=NSEQ), [QTs, Qbd], [Qbd],
                            eng='act' if h2 else 'dve')
            zl = sb("zl", [128, 8], BF16)
            fw.op('pool', lambda e: e.memset(zl.t[:, :], 0.0), writes=[zl])
            gi = 0
            for b in range(NSEQ if 'attn' not in SKIP else 0):
                O2 = [self.bank(), self.bank()]
                self.held.update(O2)
                qs = slice(b * 8, (b + 1) * 8)
                for O in (O2 if 'a_pv' not in SKIP else []):
                    self.mm(O.t[0:8, 0:260], zl.t[:, :], EMs.t[:, 0:260], True, False, [zl, EMs], [O])
                for half in range(2):
                    sc = self.bank()
                    self.held.add(sc)
                    V1h = V1s[half]
                    for g2 in range(2):
                        g = half * 2 + g2
                        Kt, Vt, KTt = Kld[gi % 2], Vld[gi % 2], KTg[gi % 2]
                        gi += 1
                        fw.dma('sp', Kt.t[:, :, :], kc[b, g * 512:(g + 1) * 512, :].rearrange("(j p) c -> p j c", p=128), writes=[Kt])
                        fw.dma('sp', Vt.t[:, :, :], vc[b, g * 512:(g + 1) * 512, :].rearrange("(j p) c -> p j c", p=128), writes=[Vt])
                        for hp in range(4):
                            pb = self.bank()
                            for j in range(4):
                                self.tr(pb.t[:, j * 128:(j + 1) * 128], Kt.t[:, j, hp * 128:(hp + 1) * 128], identf.t[:, :], [Kt, identf], [pb])
                            self.cp(KTt.t[:, hp, :], pb.t[:, :], [pb], [(KTt, hp)], eng='act' if hp % 2 else 'dve')
                        if 'a_vc' not in SKIP:
                            self.cp(V1h.t[:, g2 * 4:(g2 + 1) * 4, :, 1:65], Vt.t[:, :, :].rearrange("p j (h v) -> p j h v", h=8),
                                    [Vt], [(V1h, g2)], eng=VC_ENG)
                        for j in range(4 if 'a_sc' not in SKIP else 0):
                            jj = g2 * 4 + j
                            for hp in range(4):
                                self.mm(sc.t[:, jj * 64:(jj + 1) * 64], KTt.t[:, hp, j * 128:(j + 1) * 128], Qbd.t[:, b, hp, :],
                                        hp == 0, hp == 3, [(KTt, hp), Qbd], [sc])
                    self.held.discard(sc)
                    if 'a_sc' in SKIP:
                        continue
                    self.act(Ebs.t[:, :], sc.t[:, :], AF.Exp, [sc], [Ebs])
                    Pm_ = Pms[half]
                    self.tt(Pm_.t[:, :], Ebs.t[:, :], EMs.t[:, half * 512:(half + 1) * 512], ALU.mult, [Ebs, EMs], [Pm_])
                    for jj in range(8 if 'a_pv' not in SKIP else 0):
                        for h in range(8):
                            O = O2[h // 4]
                            c0 = (jj * 8 + h) * 8
                            self.mm(O.t[0:8, (h % 4) * 65:(h % 4) * 65 + 65], Pm_.t[:, c0:c0 + 8], V1h.t[:, jj, h, :],
                                    False, False, [Pm_, V1h], [O])
                self.held.difference_update(O2)
                if 'a_pv' in SKIP:
                    continue
                self.held.update(O2)
                sc = self.bank()
                for hp in range(4):
                    self.mm(sc.t[:, 0:64], KTn.t[:, hp, :], Qbd.t[:, b, hp, :], hp == 0, hp == 3, [KTn, Qbd], [sc])
                self.act(Ebs.t[:, 0:64], sc.t[:, 0:64], AF.Exp, [sc], [Ebs])
                self.tt(Pmn.t[:, :], Ebs.t[:, 0:64], EMn.t[:, b * 64:(b + 1) * 64], ALU.mult, [Ebs, EMn], [Pmn])
                for h in range(8):
                    O = O2[h // 4]
                    self.mm(O.t[0:8, (h % 4) * 65:(h % 4) * 65 + 65], Pmn.t[:, h * 8:h * 8 + 8], Vn1.t[:, h, :], False, h % 4 == 3, [Pmn, Vn1], [O])
                self.held.difference_update(O2)
                og = ostg[b % 2]
                for oi, O in enumerate(O2):
                    ov = O.t[0:8, 0:260].rearrange("p (h c) -> p h c", h=4)
                    fw.op('dve', lambda e, ov=ov, oi=oi: e.reciprocal(orec8.t[:, oi * 4:(oi + 1) * 4].unsqueeze(2), ov[:, :, 0:1]),
                          reads=[O], writes=[(orec8, oi)])
                    self.tt(og.t[:, oi * 256:(oi + 1) * 256].rearrange("p (h v) -> p h v", h=4), ov[:, :, 1:65],
                            orec8.t[:, oi * 4:(oi + 1) * 4].unsqueeze(2).to_broadcast([8, 4, 64]), ALU.mult,
                            [O, (orec8, oi)], [(og, oi)])
                fw.dma('sp', ATtok.t[b * 8:(b + 1) * 8, :], og.t[:, :], reads=[og], writes=[(ATtok, b)])
            ss = self.ss
            self.act(self.junk.t[:, 0, :], ATtok.t[:, :], AF.Square, [ATtok], [self.junk, ss], accum=ss.t[:, :])
            self.ts(ss.t[:, :], ss.t[:, :], 1.0 / RW, RMS_EPS, ALU.mult, ALU.add, [ss], [ss])
            self.act(ss.t[:, :], ss.t[:, :], AF.Sqrt, [ss], [ss])
            fw.op('dve', lambda e: e.reciprocal(self.ss.t[:, :], self.ss.t[:, :]), reads=[ss], writes=[ss])
            self.stt(ATtok.t[:, :], ATtok.t[:, :], ss.t[:, 0:1], again.t[:, :], ALU.mult, ALU.mult, [ATtok, ss, again], [ATtok])
            pb = self.bank()
            for cb in range(4):
                self.tr(pb.t[:, cb * 128:(cb + 1) * 128], ATtok.t[:, cb * 128:(cb + 1) * 128], identf.t[:, :], [ATtok, identf], [pb])
            self.cp(atT.t[:, :, 0:128], pb.t[:, :].rearrange("p (c t) -> p c t", c=4), [pb], [atT], eng='act')
            astk.close()
            fw.barrier()
            tail(1, pps, ys)
            sstk.close()
            fw.stk = st
            fw.finish()
        return nc


def build_prompt_nc(seq, stages=99):
    b = Builder(seq, stages=stages)
    return b.build()


_NC_CACHE = {}


def kernel(**inputs):
    inputs = {k: np.asarray(v) for k, v in inputs.items()}
    B, S = inputs["x_prompt"].shape[0], inputs["x_prompt"].shape[1]
    if "nc" not in _NC_CACHE:
        _NC_CACHE["nc"] = build_prompt_nc(S)
    nc = _NC_CACHE["nc"]
    consts = _host_consts()
    f = np.ascontiguousarray
    wts = {}
    for k, shp in WEIGHT_SHAPES.items():
        v = inputs[k]
        v = v[0] if k != "norm_final" else v
        wts[k] = f(v.reshape(shp).astype(np.float32, copy=False))
    in_maps = []
    L = NSEQ
    for c in range(8):
        b = c % B
        sl = slice(c * L, (c + 1) * L)
        m = {"xp": f(inputs["x_prompt"][b]), "pp": f(inputs["p_prompt"][0, b]),
             "xs": f(inputs["x_sample"][sl].reshape(L * 8, D)),
             "pps": f(inputs["p_sample"][0, sl].reshape(L * 8, 256)),
             "swkv": f(inputs["state_wkv"][0, sl].reshape(L * 8, 4096)),
             "sshift": f(inputs["state_shift"][0, sl]),
             "kc": f(inputs["cache_k_win"][0, sl].reshape(L, MAXWIN, 512)),
             "vc": f(inputs["cache_v_win"][0, sl].reshape(L, MAXWIN, 512))}
        m.update(wts)
        m.update(consts)
        in_maps.append(m)
    res = run_bass_kernel_spmd(nc, in_maps, core_ids=list(range(8))).results
    f32 = np.float32
    keep = min(MAXWIN, S)
    y_prompt = np.stack([res[b]["yp"] for b in range(B)]).astype(f32, copy=False)
    wkv_p = np.stack([res[b]["wkvp"] for b in range(B)])[None].astype(f32, copy=False)
    shift_p = np.stack([res[b]["shp"] for b in range(B)])[None].astype(f32, copy=False)
    kwin_p = np.stack([res[b]["kwp"].reshape(keep, 8, 64) for b in range(B)])[None].astype(f32, copy=False)
    vwin_p = np.stack([res[b]["vwp"].reshape(keep, 8, 64) for b in range(B)])[None].astype(f32, copy=False)
    y_sample = np.concatenate([res[c]["ys"].reshape(L, 8, D) for c in range(8)]).astype(f32, copy=False)
    wkv_s = np.concatenate([res[c]["wkvs"].reshape(L, 8, 64, 64) for c in range(8)])[None].astype(f32, copy=False)
    shift_s = np.concatenate([res[c]["shs"] for c in range(8)])[None].astype(f32, copy=False)
    kwin_s = np.concatenate([res[c]["kws"].reshape(L, MAXWIN, 8, 64) for c in range(8)])[None].astype(f32, copy=False)
    vwin_s = np.concatenate([res[c]["vws"].reshape(L, MAXWIN, 8, 64) for c in range(8)])[None].astype(f32, copy=False)
    return (y_prompt, y_sample, wkv_p, shift_p, kwin_p, vwin_p, wkv_s, shift_s, kwin_s, vwin_s)
```

```python
import numpy as np
from contextlib import ExitStack
import concourse.bass as bass
import concourse.mybir as mybir
from concourse.bass_utils import run_bass_kernel_spmd

F32 = mybir.dt.float32
BF16 = mybir.dt.bfloat16
I32 = mybir.dt.int32
AF = mybir.ActivationFunctionType
ALU = mybir.AluOpType
AX = mybir.AxisListType


class Buf:
    def __init__(self, t, name, psum=False):
        self.t = t
        self.name = name
        self.psum = psum
        self.st = {None: [{}, {}]}


def _merge(dst, src):
    for k, v in src.items():
        if dst.get(k, 0) < v:
            dst[k] = v


class FW:
    ENGS = ('pe', 'act', 'dve', 'pool', 'sp')

    def __init__(self, nc, st, n_dma_sems=24):
        self.nc = nc
        self.stk = st
        self.ops = {e: [] for e in self.ENGS}
        self.cnt = {e: 0 for e in self.ENGS}
        self.sem = {}
        self.semobj = {}
        for e in self.ENGS:
            s = st.enter_context(nc.semaphore("c_" + e))
            self.sem[e] = id(s)
            self.semobj[id(s)] = s
        self.waited = {e: {} for e in self.ENGS}
        self.dsems = {'hw': [], 'sw': []}
        for kind in ('hw', 'sw'):
            for i in range(n_dma_sems):
                s = st.enter_context(nc.semaphore("d%s%d" % (kind, i)))
                self.semobj[id(s)] = s
                self.dsems[kind].append([s, 0])
        self.dnext = {'hw': 0, 'sw': 0}
        self.out_tokens = {}
        self.n_inst = 0
        self.extra_dsems = []

    def sb(self, name, shape, dtype):
        t = self.stk.enter_context(self.nc.sbuf_tensor("s_" + name, list(shape), dtype))
        return Buf(t, name)

    def ps(self, name, shape, dtype):
        t = self.stk.enter_context(self.nc.psum_tensor("p_" + name, list(shape), dtype))
        return Buf(t, name, psum=True)

    @staticmethod
    def _norm(x):
        if isinstance(x, tuple):
            if x[0].psum:
                return (x[0], None)
            return x
        return (x, None)

    def _deps(self, reads, writes):
        deps = {}
        for b, k in map(self._norm, reads):
            if k is None:
                for kk, (w, r) in b.st.items():
                    _merge(deps, w)
                    if b.psum:
                        _merge(deps, r)
            else:
                _merge(deps, b.st[None][0])
                if k in b.st:
                    _merge(deps, b.st[k][0])
        for b, k in map(self._norm, writes):
            if k is None:
                for kk, (w, r) in b.st.items():
                    _merge(deps, w)
                    _merge(deps, r)
            else:
                _merge(deps, b.st[None][0])
                _merge(deps, b.st[None][1])
                if k in b.st:
                    _merge(deps, b.st[k][0])
                    _merge(deps, b.st[k][1])
        return deps

    def _update(self, reads, writes, tok):
        sid, val = tok
        for b, k in map(self._norm, reads):
            s = b.st.setdefault(k, [{}, {}])
            if s[1].get(sid, 0) < val:
                s[1][sid] = val
        for b, k in map(self._norm, writes):
            if k is None:
                b.st = {None: [{sid: val}, {}]}
            else:
                b.st[k] = [{sid: val}, {}]

    def _emit_waits(self, eng, deps, skip_self):
        lst = []
        wd = self.waited[eng]
        for sid, val in deps.items():
            if skip_self and sid == self.sem[eng]:
                continue
            if wd.get(sid, 0) >= val:
                continue
            wd[sid] = val
            lst.append((self.semobj[sid], val))
        return lst

    def op(self, eng, fn, reads=(), writes=()):
        deps = self._deps(reads, writes)
        waits = self._emit_waits(eng, deps, skip_self=(eng == 'pe'))
        self.cnt[eng] += 1
        idx = self.cnt[eng]
        semo = self.semobj[self.sem[eng]]

        def run(e, fn=fn, waits=waits, semo=semo):
            for s, v in waits:
                e.wait_ge(s, v)
            fn(e).then_inc(semo, 1)
        self.ops[eng].append(run)
        self.waited[eng][self.sem[eng]] = max(self.waited[eng].get(self.sem[eng], 0), 0)
        self._update(reads, writes, (self.sem[eng], idx))
        self.n_inst += 1
        return (self.sem[eng], idx)

    def dma(self, eng, out, in_, reads=(), writes=(), out_dram=False, nc_ok=False, n=1, fn=None, ent=None):
        deps = self._deps(reads, writes)
        kind = 'sw' if eng == 'pool' else 'hw'
        if ent is None:
            ent = self.dsems[kind][self.dnext[kind]]
            self.dnext[kind] = (self.dnext[kind] + 1) % len(self.dsems[kind])
            if ent[1] > 0:
                deps[id(ent[0])] = max(deps.get(id(ent[0]), 0), ent[1])
        dsem = ent[0]
        waits = self._emit_waits(eng, deps, skip_self=False)
        ent[1] += 16 * n
        val = ent[1]

        def run(e, waits=waits, dsem=dsem):
            for s, v in waits:
                e.wait_ge(s, v)
            if fn is not None:
                for ins in fn(e):
                    ins.then_inc(dsem, 16)
            else:
                kw = {}
                if nc_ok:
                    kw['allow_slow_non_contiguous'] = True
                e.dma_start(out=out, in_=in_, **kw).then_inc(dsem, 16)
        self.ops[eng].append(run)
        tok = (id(dsem), val)
        self._update(reads, writes, tok)
        if out_dram:
            self.out_tokens[id(dsem)] = val
        self.n_inst += 1
        return tok

    def new_dsem(self, name):
        s = self.stk.enter_context(self.nc.semaphore(name))
        self.semobj[id(s)] = s
        ent = [s, 0]
        self.extra_dsems.append(ent)
        return ent

    def barrier(self):
        for eng in self.ENGS:
            waits = []
            wd = self.waited[eng]
            for e2 in self.ENGS:
                sid = self.sem[e2]
                if e2 != eng and self.cnt[e2] > wd.get(sid, 0):
                    waits.append((self.semobj[sid], self.cnt[e2]))
                    wd[sid] = self.cnt[e2]
            for kind in ('hw', 'sw'):
                for sm, v in self.dsems[kind]:
                    if v > wd.get(id(sm), 0):
                        waits.append((sm, v))
                        wd[id(sm)] = v

            def run(e, waits=waits):
                for sm, v in waits:
                    e.wait_ge(sm, v)
            self.ops[eng].append(run)

    def make_identity(self, ident, dtype_f32_tmp=None):
        def f1(e):
            return e.memset(ident.t[:, :], 1.0)
        self.op('pool', f1, writes=[ident])
        def f2(e):
            return e.affine_select(ident.t[:, :], ident.t[:, :], [[-1, 128]], ALU.is_equal, 0.0,
                                   base=0, channel_multiplier=1)
        self.op('pool', f2, reads=[ident], writes=[ident])

    def finish(self):
        finals = [(self.semobj[sid], v) for sid, v in self.out_tokens.items()]
        nc = self.nc
        with nc.Block() as block:
            @block.tensor
            def _(e):
                for f in self.ops['pe']:
                    f(e)

            @block.scalar
            def _(e):
                for f in self.ops['act']:
                    f(e)

            @block.vector
            def _(e):
                for f in self.ops['dve']:
                    f(e)

            @block.gpsimd
            def _(e):
                for f in self.ops['pool']:
                    f(e)

            @block.sync
            def _(e):
                for f in self.ops['sp']:
                    f(e)
                for s, v in finals:
                    e.wait_ge(s, v)


D = 1024
RW = 512
RW_COLS = 1664
IN_COLS = 3200
QOFF, KOFF, VOFF = 1664, 1664 + 512, 1664 + 1024
W = 512
RING = 24
NKB = 17
RMS_EPS = 1e-6
GN_EPS = 64e-5
DECAY_C = float(np.exp(-0.5))
MAXWIN = 2048
NSEQ = 16


def _host_consts():
    c = {}
    s = np.arange(64)[:, None]
    t = np.arange(64)[None, :]
    c["cI"] = (-DECAY_C * (s <= t)).astype(np.float32)
    c["cS"] = (-DECAY_C * (s < t)).astype(np.float32)
    c["cR"] = (-DECAY_C * (s > t)).astype(np.float32)
    up_s = (s < t).astype(np.float32)
    up_i = (s <= t).astype(np.float32)
    lo_s = (t < s).astype(np.float32)
    c["mk"] = np.concatenate([up_s, lo_s, up_s, up_i, up_i], axis=1)
    bo = np.zeros((128, 128), np.float32)
    bo[:64, :64] = 1
    bo[64:, 64:] = 1
    c["bones"] = bo
    slopes = 2.0 ** (-8.0 * np.arange(1, 9) / 8)
    ki = np.arange(128)[:, None]
    qi = np.arange(128)[None, :]
    em = np.zeros((8, 128, NKB, 128), np.float32)
    for j in range(NKB):
        dl = 128 * j + qi - ki
        mult = np.zeros_like(dl, dtype=np.float64)
        for wd, dil in ((128, 1), (512, 4), (2048, 16)):
            mult += ((dl >= 0) & (dl % dil == 0) & (dl <= wd))
        for h in range(8):
            em[h, :, j, :] = mult * np.exp(-slopes[h] * np.maximum(dl, 0))
    c["em"] = em
    def fac(dl):
        mult = np.zeros(dl.shape, np.float64)
        for wd, dil in ((128, 1), (512, 4), (2048, 16)):
            mult += ((dl >= 0) & (dl % dil == 0) & (dl <= wd))
        return mult[None] * np.exp(-slopes[:, None, None, None] * np.maximum(dl, 0)[None])
    p_ = np.arange(128)[:, None, None]
    blk_ = np.arange(16)[None, :, None]
    t_ = np.arange(8)[None, None, :]
    ems = fac(MAXWIN + t_ - (blk_ * 128 + p_))
    c["ems"] = np.ascontiguousarray(ems.transpose(1, 2, 0, 3)).reshape(128, 1024).astype(np.float32)
    bq_ = np.arange(NSEQ)[None, :, None]
    emn = fac(t_ - (p_ % 8) + 0 * bq_) * ((p_ // 8) == bq_)[None]
    c["emn"] = np.ascontiguousarray(emn.transpose(1, 2, 0, 3)).reshape(128, 1024).astype(np.float32)
    return c


CONST_SHAPES = {"cI": [64, 64], "cS": [64, 64], "cR": [64, 64], "mk": [64, 320],
                "bones": [128, 128], "em": [8, 128, NKB, 128], "ems": [128, 1024], "emn": [128, 1024]}

WEIGHT_SHAPES = {
    "norm_mix": [D], "w_in": [D, IN_COLS], "mu": [RW_COLS], "w0": [RW], "w2": [32, RW],
    "a0": [RW], "a2": [32, RW], "g2": [64, RW], "k_k": [RW], "k_a": [RW], "r_k": [RW],
    "ln_x_w": [RW], "ln_x_b": [RW], "attn_gain": [RW], "w_out": [D, D], "norm_ffn": [D],
    "w_group": [D, 4], "b_group": [4], "w_expert_router": [D, 32], "b_expert_router": [32],
    "w_gate": [32, D, 256], "w_up": [32, D, 256], "w_down": [32, 256, D],
    "norm_ple": [D], "w_ple": [256, D], "w_ple_gate": [D, D], "norm_final": [D],
}


import os as _os
SKIP = set(_os.environ.get('KSKIP', '').split(','))
VC_ENG = _os.environ.get('KVCENG', 'pool')


class StopBuild(Exception):
    pass


class Builder:
    def __init__(self, seq, stages=99, dbg=()):
        self.seq = seq
        self.nw = seq // W
        self.stages = stages
        self.dbgnames = set(dbg)
        self.nc = bass.Bass("TRN2", target_bir_lowering=False)
        self.dr = {}
        self.dbg_out = {}

    def din(self, name, shape):
        self.dr[name] = self.nc.dram_tensor(name, list(shape), F32, kind="ExternalInput").ap()
        return self.dr[name]

    def dout(self, name, shape):
        self.dr[name] = self.nc.dram_tensor(name, list(shape), F32, kind="ExternalOutput").ap()
        return self.dr[name]

    def ck(self, x):
        if self.stages < x:
            raise StopBuild()

    def bank(self):
        for _ in range(8):
            b = self.PS[self.pnext]
            self.pnext = (self.pnext + 1) % 8
            if b not in self.held:
                return b
        raise RuntimeError("no free bank")

    def mm(self, out, lhsT, rhs, start, stop, r, w):
        self.fw.op('pe', lambda e: e.matmul(out, lhsT, rhs, start=start, stop=stop), reads=r, writes=w)

    def tr(self, out, in_, ident, r, w):
        self.fw.op('pe', lambda e: e.transpose(out, in_, ident), reads=r, writes=w)

    def act(self, out, in_, func, r, w, bias=None, scale=None, accum=None):
        kw = {}
        if bias is not None:
            kw['bias'] = bias
        if scale is not None:
            kw['scale'] = scale
        if accum is not None:
            kw['accum_out'] = accum
        self.fw.op('act', lambda e: e.activation(out, in_, func, **kw), reads=r, writes=w)

    def tt(self, out, a, b, op, r, w, eng='dve'):
        self.fw.op(eng, lambda e: e.tensor_tensor(out, a, b, op), reads=r, writes=w)

    def ts(self, out, a, s1, s2, op0, op1, r, w, eng='dve'):
        if s2 is None:
            self.fw.op(eng, lambda e: e.tensor_scalar(out, a, s1, None, op0), reads=r, writes=w)
        else:
            self.fw.op(eng, lambda e: e.tensor_scalar(out, a, s1, s2, op0, op1), reads=r, writes=w)

    def stt(self, out, a, sc, b, op0, op1, r, w, eng='dve'):
        self.fw.op(eng, lambda e: e.scalar_tensor_tensor(out, a, sc, b, op0, op1), reads=r, writes=w)

    def cp(self, out, in_, r, w, eng='dve'):
        if eng == 'act':
            self.act(out, in_, AF.Copy, r, w)
        else:
            self.fw.op(eng, lambda e: e.tensor_copy(out, in_), reads=r, writes=w)

    def dbg(self, name, ap, shape, reads):
        if name not in self.dbgnames:
            return
        d = self.dout("dbg_" + name, shape)
        self.fw.dma('sp', d, ap, reads=reads, out_dram=True)

    def wload(self, src_ap, rows_k, cols, eng='sp', reads=()):
        t = self.WP[self.wnext]
        self.wnext = (self.wnext + 1) % len(self.WP)
        self.fw.dma(eng, t.t[:, 0:rows_k, 0:cols], src_ap, reads=list(reads), writes=[t], nc_ok=True)
        return t

    def vec_load(self, name, nk, dst=None):
        t = self.fw.sb("v_" + name, [128, nk], F32) if dst is None else dst
        self.fw.dma('sp', t.t[:, :], self.dr[name].rearrange("(k p) -> p k", p=128), writes=[t], nc_ok=True)
        return t

    def norm_T(self, Xs, gain, hT, rows=128):
        fw = self.fw
        for blk, Xb in enumerate(Xs):
            ss = self.ss
            self.act(self.junk.t[0:rows, :, :].rearrange("p a b -> p (a b)"), Xb.t[0:rows, :], AF.Square, [Xb], [self.junk, ss], accum=ss.t[0:rows, :])
            self.ts(ss.t[0:rows, :], ss.t[0:rows, :], 1.0 / D, RMS_EPS, ALU.mult, ALU.add, [ss], [ss])
            self.act(ss.t[0:rows, :], ss.t[0:rows, :], AF.Sqrt, [ss], [ss])
            fw.op('dve', lambda e, ss=ss: e.reciprocal(ss.t[0:rows, :], ss.t[0:rows, :]), reads=[ss], writes=[ss])
            xn = self.xn
            self.ts(xn.t[0:rows, :], Xb.t[0:rows, :], ss.t[0:rows, 0:1], None, ALU.mult, None, [Xb, ss], [xn])
            for half in range(2):
                pb = self.bank()
                for kk in range(4):
                    k = half * 4 + kk
                    self.tr(pb.t[:, kk * 128:kk * 128 + rows], xn.t[0:rows, k * 128:(k + 1) * 128],
                            self.identf.t[0:rows, 0:rows], [xn, self.identf], [pb])
                src = pb.t[:, :].rearrange("p (k t) -> p k t", k=4)[:, :, 0:rows]
                gb = gain.t[:, half * 4:half * 4 + 4].unsqueeze(2).to_broadcast([128, 4, rows])
                self.tt(hT.t[:, half * 4:half * 4 + 4, blk * 128:blk * 128 + rows], src, gb, ALU.mult,
                        [pb, gain], [(hT, ('b', blk, half))])

    def build(self):
        nc = self.nc
        seq, nw = self.seq, self.nw
        keep = min(MAXWIN, seq)
        xp = self.din("xp", [seq, D])
        pp = self.din("pp", [seq, 256])
        for k, shp in WEIGHT_SHAPES.items():
            self.din(k, shp)
        for k, shp in CONST_SHAPES.items():
            self.din(k, shp)
        yp = self.dout("yp", [seq, D])
        wkvp = self.dout("wkvp", [8, 64, 64])
        shp_o = self.dout("shp", [RW_COLS])
        kwp = self.dout("kwp", [keep, 512])
        vwp = self.dout("vwp", [keep, 512])
        xs = self.din("xs", [128, D])
        pps = self.din("pps", [128, 256])
        swkv = self.din("swkv", [128, 4096])
        sshift = self.din("sshift", [NSEQ, RW_COLS])
        kc = self.din("kc", [NSEQ, MAXWIN, 512])
        vc = self.din("vc", [NSEQ, MAXWIN, 512])
        ys = self.dout("ys", [128, D])
        wkvs = self.dout("wkvs", [128, 4096])
        shs = self.dout("shs", [NSEQ, RW_COLS])
        kws = self.dout("kws", [NSEQ, MAXWIN, 512])
        vws = self.dout("vws", [NSEQ, MAXWIN, 512])
        scrA = self.dout("scrA", [128, 6 * 512])
        scrB = self.dout("scrB", [128, 512])
        dr = self.dr
        with ExitStack() as st:
            fw = self.fw = FW(nc, st, n_dma_sems=16)
            sb = fw.sb
            self.PS = [fw.ps("ps%d" % i, [128, 512], F32) for i in range(8)]
            self.pnext = 0
            self.held = set()
            self.WP = [sb("wp%d" % i, [128, 8, 512], BF16) for i in range(2)]
            self.wnext = 0
            self.identf = identf = sb("identf", [128, 128], F32)
            fw.make_identity(identf)
            g_mix, g_ffn, g_ple = self.vec_load("norm_mix", 8), self.vec_load("norm_ffn", 8), self.vec_load("norm_ple", 8)
            gfin = sb("gfin", [128, D], F32)
            fw.dma('sp', gfin.t[:, :], dr["norm_final"].partition_broadcast(128), writes=[gfin])
            again = sb("again", [128, RW], F32)
            fw.dma('sp', again.t[:, :], dr["attn_gain"].partition_broadcast(128), writes=[again])
            w0rep = sb("w0rep", [128, RW], F32)
            fw.dma('sp', w0rep.t[:, :], dr["w0"].partition_broadcast(128), writes=[w0rep])
            LW = sb("LW", [128, RW], BF16)
            fw.dma('pool', LW.t[0:32, :], dr["w2"], writes=[(LW, 0)])
            fw.dma('pool', LW.t[32:64, :], dr["a2"], writes=[(LW, 1)])
            fw.dma('pool', LW.t[64:128, :], dr["g2"], writes=[(LW, 2)])
            Wrt = sb("Wrt", [128, 8, 36], BF16)
            fw.dma('pool', Wrt.t[:, :, 0:4], dr["w_group"].rearrange("(k p) c -> p k c", p=128), writes=[(Wrt, 0)])
            fw.dma('pool', Wrt.t[:, :, 4:36], dr["w_expert_router"].rearrange("(k p) c -> p k c", p=128), writes=[(Wrt, 1)])
            brep = sb("brep", [128, 36], F32)
            fw.dma('sp', brep.t[:, 0:4], dr["b_group"].partition_broadcast(128), writes=[(brep, 0)])
            fw.dma('sp', brep.t[:, 4:36], dr["b_expert_router"].partition_broadcast(128), writes=[(brep, 1)])
            X = [sb("X%d" % i, [128, D], F32) for i in range(4)]
            self.xn = sb("xn", [128, D], F32)
            self.ss = sb("ss", [128, 1], F32)
            hT = sb("hT", [128, 8, W], BF16)
            rwT = sb("rwT", [128, 4, W], BF16)
            atT = sb("atT", [128, 4, W], BF16)
            Lt = sb("Lt", [128, W], BF16)
            T = {n: sb("t_" + n, [128, W], F32) for n in ("a", "kkr", "kk", "e1", "e2", "tmp")}
            lg = sb("lg", [128, 36], F32)
            comb = sb("comb", [128, 4, 32], F32)
            rt = {n: sb("r_" + n, [128, 1], F32) for n in ("gmax", "ngmax", "sumg", "gw", "nm1", "e2", "den", "w1", "w2")}
            oh = sb("oh", [128, 4], F32)
            eg = sb("eg", [128, 4], F32)
            elm = sb("elm", [128, 32], F32)
            top8 = sb("top8", [128, 8], F32)
            c1 = sb("c1", [128, 32], F32)
            sgt = T["tmp"]
            Aff = sb("Aff", [128, 2, W], BF16)
            self.junk = Aff
            Wd = [sb("Wd%d" % i, [128, 2, D], BF16) for i in range(1)]
            pT = sb("pT", [128, 2, W], BF16)
            pin = sb("pin", [128, 256], F32)
            yst = self.xn
            P_ = dict(identf=identf, g_mix=g_mix, g_ffn=g_ffn, g_ple=g_ple, gfin=gfin, again=again, w0rep=w0rep, LW=LW,
                      X=X, hT=hT, rwT=rwT, atT=atT, Lt=Lt, T=T)
            self.P_ = P_

            roll_ent = fw.new_dsem("d_roll")
            nrows = MAXWIN - 8
            piece = nrows // 4
            for j in range(NSEQ if 'roll' not in SKIP else 0):
                for src, dst in ((dr["kc"], dr["kws"]), (dr["vc"], dr["vws"])):
                    def mkroll(e, j=j, src=src, dst=dst):
                        return [e.dma_start(out=dst[j, a:a + piece, :], in_=src[j, a + 8:a + piece + 8, :])
                                for a in range(0, nrows, piece)]
                    fw.dma('act', None, None, fn=mkroll, n=4, out_dram=True, ent=roll_ent)

            pstk = ExitStack()
            fw.stk = pstk
            cI, cS, cR = sb("cI", [64, 64], F32), sb("cS", [64, 64], F32), sb("cR", [64, 64], F32)
            mk = sb("mk", [64, 320], F32)
            bones = sb("bones", [128, 128], BF16)
            for t_, n_ in ((cI, "cI"), (cS, "cS"), (cR, "cR"), (mk, "mk")):
                fw.dma('sp', t_.t[:, :], dr[n_], writes=[t_])
            fw.dma('pool', bones.t[:, :], dr["bones"], writes=[bones])
            identb = sb("identb", [64, 64], BF16)
            self.cp(identb.t[:, :], identf.t[0:64, 0:64], [identf], [identb])
            mu = self.vec_load("mu", 13)
            w0, a0, k_k, k_a = self.vec_load("w0", 4), self.vec_load("a0", 4), self.vec_load("k_k", 4), self.vec_load("k_a", 4)
            r_k, ln_w, ln_b = self.vec_load("r_k", 4), self.vec_load("ln_x_w", 4), self.vec_load("ln_x_b", 4)
            omka = sb("omka", [128, 4], F32)
            self.ts(omka.t[:, :], k_a.t[:, :], -1.0, 1.0, ALU.mult, ALU.add, [k_a], [omka])
            carry = sb("carry", [128, 13], F32)
            fw.op('pool', lambda e: e.memset(carry.t[:, :], 0.0), writes=[carry])
            ST = sb("ST", [128, 4, 64], F32)
            STb = sb("STb", [128, 4, 128], BF16)
            fw.op('pool', lambda e: e.memset(ST.t[:, :, :], 0.0), writes=[ST])
            fw.op('pool', lambda e: e.memset(STb.t[:, :, :], 0.0), writes=[STb])
            KT = sb("KT", [128, 4, RING * 128], BF16)
            V1 = sb("V1", [128, RING, 8, 65], BF16)
            fw.op('pool', lambda e: e.memset(KT.t[:, :, :], 0.0), writes=[KT])
            fw.op('pool', lambda e: e.memset(V1.t[:, :, :, :], 0.0), writes=[V1])
            QT = sb("QT", [128, 4, W], BF16)
            EM2 = [sb("EMh%d" % i, [128, NKB, 128], BF16) for i in range(1)]
            Pk = [sb("Pk%d" % i, [128, W + 1], F32) for i in range(2)]
            dlt = sb("dlt", [128, W], F32)
            xm = [sb("xm%d" % i, [128, W], F32) for i in range(3)]
            xml = xm[0]
            T = dict(T)
            T["kf"] = T["kkr"]
            T["bb"] = T["tmp"]
            T["e3"] = dlt
            sqb = sb("sqb", [128, W], BF16)
            t3b = sb("t3b", [128, W], BF16)
            bonus = sb("bonus", [128, W], F32)
            gg = sb("gg", [128, W], F32)
            ATt, RTt, KTt, BTt = (sb(n, [128, W], BF16) for n in ("ATt", "RTt", "KTt", "BTt"))
            Pend = sb("Pend", [128, 8], F32)
            sgwT = sb("sgwT", [64, 8, 128], F32)
            eR = sb("eR", [64, 8, 128], F32)
            Vtok, Ktok, Btok = (sb(n, [64, 8, 128], BF16) for n in ("Vtok", "Ktok", "Btok"))
            Ytok = sb("Ytok", [64, 8, 128], F32)
            Ach = [[sb("Ach%d_%d" % (i, h), [64, 320], BF16) for h in range(2)] for i in range(2)]
            Xi = [[sb("Xi%d_%d" % (i, p), [64, 2, 192], BF16) for p in range(2)] for i in range(2)]
            Zb = sb("Zb", [64, 128], BF16)
            Ub = sb("Ub", [64, 128], BF16)
            gs1, gs2 = sb("gs1", [64, 16], F32), sb("gs2", [64, 16], F32)
            Otok = [sb("Otok%d" % i, [128, RW], F32) for i in range(4)]
            orec = sb("orec", [128, 1], F32)
            Eb = sb("Eb", [128, 512], BF16)
            Pm = sb("Pm", [128, 512], BF16)
            stK = sb("stK", [128, 512], F32)
            stV = stK

            def wscr(name, shape):
                return nc.dram_tensor(name, list(shape), BF16, kind="Internal").ap(), Buf(None, name)
            kp = lambda ap: ap.rearrange("(k p) c -> p k c", p=128)
            win_bf, win_b = wscr("win_bf", [128, 8, IN_COLS])
            em_bf, em_b = wscr("em_bf", [8, 128, NKB, 128])
            wo_bf, wo_b = wscr("wo_bf", [128, 8, D])
            wgu_bf, wgu_b = wscr("wgu_bf", [32, 128, 8, 512])
            wd_bf, wd_b = wscr("wd_bf", [32, 128, 2, D])
            wpg_bf, wpg_b = wscr("wpg_bf", [128, 8, D])
            wpl_bf, wpl_b = wscr("wpl_bf", [128, 2, D])
            for c0 in range(0, IN_COLS, 640):
                fw.dma('pool', win_bf[:, :, c0:c0 + 640], kp(dr["w_in"])[:, :, c0:c0 + 640], writes=[(win_b, c0)], nc_ok=True)
            for h in range(8):
                fw.dma('pool', em_bf[h], dr["em"][h], writes=[(em_b, h)])
            for hf in range(2):
                fw.dma('pool', wo_bf[:, :, hf * 512:(hf + 1) * 512], kp(dr["w_out"])[:, :, hf * 512:(hf + 1) * 512], writes=[(wo_b, hf)], nc_ok=True)
            for ex in range(32):
                fw.dma('pool', wgu_bf[ex][:, :, 0:256], kp(dr["w_gate"][ex]), writes=[(wgu_b, (ex, 0))], nc_ok=True)
                fw.dma('pool', wgu_bf[ex][:, :, 256:512], kp(dr["w_up"][ex]), writes=[(wgu_b, (ex, 1))], nc_ok=True)
                fw.dma('pool', wd_bf[ex], kp(dr["w_down"][ex]), writes=[(wd_b, ex)], nc_ok=True)
            for hf in range(2):
                fw.dma('pool', wpg_bf[:, :, hf * 512:(hf + 1) * 512], kp(dr["w_ple_gate"])[:, :, hf * 512:(hf + 1) * 512], writes=[(wpg_b, hf)], nc_ok=True)
            fw.dma('pool', wpl_bf, kp(dr["w_ple"]), writes=[wpl_b], nc_ok=True)
            win_v = win_bf

            def tail(nblk, pp_ap, y_ap):
                nt = nblk * 128
                wo = [self.wload(wo_bf[:, :, hf * 512:(hf + 1) * 512], 8, 512, reads=[(wo_b, hf)]) for hf in range(2)]
                for blk in range(nblk):
                    bs = slice(blk * 128, (blk + 1) * 128)
                    for hf in range(2):
                        pb = self.bank()
                        for k in range(8):
                            lhs = rwT.t[:, k, bs] if k < 4 else atT.t[:, k - 4, bs]
                            self.mm(pb.t[:, :], lhs, wo[hf].t[:, k, :], k == 0, k == 7, [rwT, atT, wo[hf]], [pb])
                        self.tt(X[blk].t[:, hf * 512:(hf + 1) * 512], X[blk].t[:, hf * 512:(hf + 1) * 512], pb.t[:, :], ALU.add,
                                [pb, X[blk]], [X[blk]])

                self.norm_T(X[0:nblk], g_ffn, hT)
                for blk in range(nblk):
                    bs = slice(blk * 128, (blk + 1) * 128)
                    pb = self.bank()
                    for k in range(8):
                        self.mm(pb.t[:, 0:36], hT.t[:, k, bs], Wrt.t[:, k, :], k == 0, k == 7, [hT, Wrt], [pb])
                    self.tt(lg.t[:, :], pb.t[:, 0:36], brep.t[:, :], ALU.add, [pb, brep], [lg])
                    fw.op('dve', lambda e: e.reduce_max(rt["gmax"].t[:, :], lg.t[:, 0:4], AX.X), reads=[lg], writes=[rt["gmax"]])
                    self.ts(oh.t[:, :], lg.t[:, 0:4], rt["gmax"].t[:, 0:1], None, ALU.is_equal, None, [lg, rt["gmax"]], [oh])
                    self.ts(rt["ngmax"].t[:, :], rt["gmax"].t[:, :], -1.0, None, ALU.mult, None, [rt["gmax"]], [rt["ngmax"]])
                    self.act(eg.t[:, :], lg.t[:, 0:4], AF.Exp, [lg, rt["ngmax"]], [eg, rt["sumg"]], bias=rt["ngmax"].t[:, 0:1],
                             accum=rt["sumg"].t[:, :])
                    fw.op('dve', lambda e: e.reciprocal(rt["gw"].t[:, :], rt["sumg"].t[:, :]), reads=[rt["sumg"]], writes=[rt["gw"]])
                    self.ts(oh.t[:, :], oh.t[:, :], 1e30, -1e30, ALU.mult, ALU.add, [oh], [oh])
                    self.tt(elm.t[:, :].rearrange("p (g e) -> p g e", g=4), lg.t[:, 4:36].rearrange("p (g e) -> p g e", g=4),
                            oh.t[:, :].unsqueeze(2).to_broadcast([128, 4, 8]), ALU.add, [lg, oh], [elm])
                    fw.op('dve', lambda e: e.max(top8.t[:, :], elm.t[:, :]), reads=[elm], writes=[top8])
                    self.ts(rt["nm1"].t[:, :], top8.t[:, 0:1], -1.0, None, ALU.mult, None, [top8], [rt["nm1"]])
                    self.act(rt["e2"].t[:, :], top8.t[:, 1:2], AF.Exp, [top8, rt["nm1"]], [rt["e2"]], bias=rt["nm1"].t[:, 0:1])
                    self.ts(rt["den"].t[:, :], rt["e2"].t[:, :], 1.0, None, ALU.add, None, [rt["e2"]], [rt["den"]])
                    fw.op('dve', lambda e: e.reciprocal(rt["den"].t[:, :], rt["den"].t[:, :]), reads=[rt["den"]], writes=[rt["den"]])
                    self.tt(rt["w1"].t[:, :], rt["den"].t[:, :], rt["gw"].t[:, :], ALU.mult, [rt["den"], rt["gw"]], [rt["w1"]])
                    self.tt(rt["w2"].t[:, :], rt["w1"].t[:, :], rt["e2"].t[:, :], ALU.mult, [rt["w1"], rt["e2"]], [rt["w2"]])
                    self.ts(c1.t[:, :], elm.t[:, :], top8.t[:, 0:1], rt["w1"].t[:, 0:1], ALU.is_equal, ALU.mult, [elm, top8, rt["w1"]], [c1])
                    self.ts(comb.t[:, blk, :], elm.t[:, :], top8.t[:, 1:2], rt["w2"].t[:, 0:1], ALU.is_equal, ALU.mult,
                            [elm, top8, rt["w2"]], [(comb, blk)])
                    self.tt(comb.t[:, blk, :], comb.t[:, blk, :], c1.t[:, :], ALU.add, [(comb, blk), c1], [(comb, blk)])
                for ex in range(32):
                    wgu = self.WP[self.wnext]
                    self.wnext = (self.wnext + 1) % len(self.WP)
                    fw.dma('sp', wgu.t[:, :, :], wgu_bf[ex], reads=[(wgu_b, (ex, 0)), (wgu_b, (ex, 1))], writes=[wgu])
                    wd = Wd[ex % len(Wd)]
                    fw.dma('sp', wd.t[:, :, :], wd_bf[ex], reads=[(wd_b, ex)], writes=[wd])
                    for fb in range(2):
                        gbk, ubk = self.bank(), self.bank()
                        for k in range(8):
                            self.mm(gbk.t[:, 0:nt], wgu.t[:, k, fb * 128:(fb + 1) * 128], hT.t[:, k, 0:nt], k == 0, k == 7, [wgu, hT], [gbk])
                        for k in range(8):
                            self.mm(ubk.t[:, 0:nt], wgu.t[:, k, 256 + fb * 128:256 + (fb + 1) * 128], hT.t[:, k, 0:nt], k == 0, k == 7, [wgu, hT], [ubk])
                        self.act(sgt.t[:, 0:nt], gbk.t[:, 0:nt], AF.Silu, [gbk], [sgt])
                        self.tt(Aff.t[:, fb, 0:nt], sgt.t[:, 0:nt], ubk.t[:, 0:nt], ALU.mult, [sgt, ubk], [(Aff, fb)])
                    for blk in range(nblk):
                        bs = slice(blk * 128, (blk + 1) * 128)
                        for hf in range(2):
                            pb = self.bank()
                            for fb in range(2):
                                self.mm(pb.t[:, :], Aff.t[:, fb, bs], wd.t[:, fb, hf * 512:(hf + 1) * 512], fb == 0, fb == 1, [Aff, wd], [pb])
                            xs_ = X[blk].t[:, hf * 512:(hf + 1) * 512]
                            self.stt(xs_, pb.t[:, :], comb.t[:, blk, ex:ex + 1], xs_, ALU.mult, ALU.add, [pb, (comb, blk), X[blk]], [X[blk]])

                self.norm_T(X[0:nblk], g_ple, hT)
                for blk in range(nblk):
                    fw.dma('sp', pin.t[:, :], pp_ap[blk * 128:(blk + 1) * 128, :], writes=[pin])
                    pb = self.bank()
                    for k2 in range(2):
                        self.tr(pb.t[:, k2 * 128:(k2 + 1) * 128], pin.t[:, k2 * 128:(k2 + 1) * 128], identf.t[:, :], [pin, identf], [pb])
                    self.cp(pT.t[:, :, blk * 128:(blk + 1) * 128], pb.t[:, 0:256].rearrange("p (k t) -> p k t", k=2), [pb], [(pT, blk)])
                for hf in range(2):
                    wpg = self.wload(wpg_bf[:, :, hf * 512:(hf + 1) * 512], 8, 512, reads=[(wpg_b, hf)])
                    wpl = self.wload(wpl_bf[:, :, hf * 512:(hf + 1) * 512], 2, 512, reads=[wpl_b])
                    for blk in range(nblk):
                        bs = slice(blk * 128, (blk + 1) * 128)
                        gbk, pbk = self.bank(), self.bank()
                        for k in range(8):
                            self.mm(gbk.t[:, :], hT.t[:, k, bs], wpg.t[:, k, :], k == 0, k == 7, [hT, wpg], [gbk])
                        for k2 in range(2):
                            self.mm(pbk.t[:, :], pT.t[:, k2, bs], wpl.t[:, k2, :], k2 == 0, k2 == 1, [pT, wpl], [pbk])
                        self.act(sgt.t[:, :], gbk.t[:, :], AF.Sigmoid, [gbk], [sgt])
                        self.tt(sgt.t[:, :], sgt.t[:, :], pbk.t[:, :], ALU.mult, [sgt, pbk], [sgt])
                        xs_ = X[blk].t[:, hf * 512:(hf + 1) * 512]
                        self.tt(xs_, xs_, sgt.t[:, :], ALU.add, [X[blk], sgt], [X[blk]])
                for blk in range(nblk):
                    ss = self.ss
                    Xb = X[blk]
                    self.act(self.junk.t[:, :, :].rearrange("p a b -> p (a b)"), Xb.t[:, :], AF.Square, [Xb], [self.junk, ss], accum=ss.t[:, :])
                    self.ts(ss.t[:, :], ss.t[:, :], 1.0 / D, RMS_EPS, ALU.mult, ALU.add, [ss], [ss])
                    self.act(ss.t[:, :], ss.t[:, :], AF.Sqrt, [ss], [ss])
                    fw.op('dve', lambda e: e.reciprocal(self.ss.t[:, :], self.ss.t[:, :]), reads=[ss], writes=[ss])
                    self.stt(yst.t[:, :], Xb.t[:, :], ss.t[:, 0:1], gfin.t[:, :], ALU.mult, ALU.mult, [Xb, ss, gfin], [yst])
                    fw.dma('sp', y_ap[blk * 128:(blk + 1) * 128, :], yst.t[:, :], reads=[yst], out_dram=True)

            for w in range(nw if self.stages >= 0.05 else 0):
              try:
                  t0 = w * W
                  for blk in range(4):
                      fw.dma('sp', X[blk].t[:, :], xp[t0 + blk * 128:t0 + (blk + 1) * 128, :], writes=[X[blk]])
                  self.ck(0.1)
                  self.norm_T(X, g_mix, hT)
                  self.ck(0.2)
                  self.dbg("hT", hT.t[:, :, :], None, [hT]) if False else None

                  def rw_block(ci, col0, dst, wt, wc0):
                      pb = self.bank()
                      for k in range(8):
                          self.mm(pb.t[:, :], wt.t[:, k, wc0:wc0 + 128], hT.t[:, k, :], k == 0, k == 7, [wt, hT], [pb])
                      P = Pk[ci % 2]
                      self.cp(P.t[:, 1:W + 1], pb.t[:, :], [pb], [(P, 1)], eng='act')
                      self.cp(P.t[:, 0:1], carry.t[:, ci:ci + 1], [(carry, ci)], [(P, 0)])
                      self.cp(carry.t[:, ci:ci + 1], P.t[:, W:W + 1], [(P, 1)], [(carry, ci)])
                      self.tt(dlt.t[:, :], P.t[:, 0:W], P.t[:, 1:W + 1], ALU.subtract, [P], [dlt])
                      self.stt(dst.t[:, :], dlt.t[:, :], mu.t[:, ci:ci + 1], P.t[:, 1:W + 1], ALU.mult, ALU.add,
                               [dlt, mu, P], [dst])

                  wl = self.wload(win_v[:, :, 1536:1664], 8, 128, reads=[win_b])
                  rw_block(12, 1536, xml, wl, 0)
                  self.act(Lt.t[0:32, :], xml.t[0:32, :], AF.Tanh, [xml], [(Lt, 0)])
                  self.cp(Lt.t[32:64, :], xml.t[32:64, :], [xml], [(Lt, 1)])
                  self.act(Lt.t[64:128, :], xml.t[64:128, :], AF.Sigmoid, [xml], [(Lt, 2)])

                  self.ck(0.3)
                  for hp in range(4):
                      wt3 = self.WP[self.wnext]
                      self.wnext = (self.wnext + 1) % len(self.WP)
                      for i3 in range(3):
                          c0 = i3 * 512 + hp * 128
                          fw.dma('sp', wt3.t[:, :, i3 * 128:(i3 + 1) * 128], win_v[:, :, c0:c0 + 128], reads=[win_b], writes=[(wt3, i3)], nc_ok=True)
                      rw_block(hp, hp * 128, xm[0], wt3, 0)
                      rw_block(4 + hp, 512 + hp * 128, xm[1], wt3, 128)
                      rw_block(8 + hp, 1024 + hp * 128, xm[2], wt3, 256)
                      self.ck(0.4)
                      xr, xk, xv = xm
                      hc = slice(hp * 128, (hp + 1) * 128)
                      pb = self.bank()
                      self.mm(pb.t[:, :], LW.t[32:64, hc], Lt.t[32:64, :], True, True, [LW, Lt], [pb])
                      self.act(T["a"].t[:, :], pb.t[:, :], AF.Sigmoid, [pb, a0], [T["a"]], bias=a0.t[:, hp:hp + 1])
                      pb = self.bank()
                      self.mm(pb.t[:, :], LW.t[64:128, hc], Lt.t[64:128, :], True, True, [LW, Lt], [pb])
                      self.cp(gg.t[:, :], pb.t[:, :], [pb], [gg], eng='act')
                      for half in range(2):
                          pb = self.bank()
                          for cc in range(4):
                              c = half * 4 + cc
                              self.mm(pb.t[0:64, cc * 128:(cc + 1) * 128], Lt.t[0:32, c * 64:(c + 1) * 64], LW.t[0:32, hc],
                                      True, True, [LW, Lt], [pb])
                          self.tt(sgwT.t[:, half * 4:half * 4 + 4, :], pb.t[0:64, :].rearrange("p (c f) -> p c f", c=4),
                                  w0rep.t[0:64, hc].unsqueeze(1).to_broadcast([64, 4, 128]), ALU.add,
                                  [pb, w0rep], [(sgwT, half)])
                          self.act(sgwT.t[:, half * 4:half * 4 + 4, :], sgwT.t[:, half * 4:half * 4 + 4, :], AF.Sigmoid,
                                   [(sgwT, half)], [(sgwT, half)])
                      self.ck(0.5)
                      pinc, pexc = self.bank(), self.bank()
                      for c in range(8):
                          self.mm(pinc.t[:, c * 64:(c + 1) * 64], sgwT.t[:, c, :], cI.t[:, :], True, True, [sgwT, cI], [pinc])
                      for c in range(8):
                          self.mm(pexc.t[:, c * 64:(c + 1) * 64], sgwT.t[:, c, :], cS.t[:, :], True, True, [sgwT, cS], [pexc])
                      self.act(T["e1"].t[:, :], pinc.t[:, :], AF.Exp, [pinc], [T["e1"]])
                      self.act(T["e2"].t[:, :], pinc.t[:, :], AF.Exp, [pinc], [T["e2"]], scale=-1.0)
                      self.act(T["e3"].t[:, :], pexc.t[:, :], AF.Exp, [pexc], [T["e3"]])
                      for half in range(2):
                          pb = self.bank()
                          for cc in range(4):
                              c = half * 4 + cc
                              self.mm(pb.t[0:64, cc * 128:(cc + 1) * 128], cR.t[:, :], sgwT.t[:, c, :], True, True, [sgwT, cR], [pb])
                          self.act(eR.t[:, half * 4:half * 4 + 4, :], pb.t[0:64, :].rearrange("p (c f) -> p c f", c=4), AF.Exp,
                                   [pb], [(eR, half)])
                      self.cp(Pend.t[:, :], T["e1"].t[:, 63::64], [T["e1"]], [Pend])
                      self.ck(0.6)
                      self.ts(T["kkr"].t[:, :], xk.t[:, :], k_k.t[:, hp:hp + 1], None, ALU.mult, None, [xk, k_k], [T["kkr"]])
                      self.act(sqb.t[:, :], T["kkr"].t[:, :], AF.Square, [T["kkr"]], [sqb])
                      pb = self.bank()
                      self.mm(pb.t[:, :], bones.t[:, :], sqb.t[:, :], True, True, [bones, sqb], [pb])
                      self.act(T["tmp"].t[:, :], pb.t[:, :], AF.Sqrt, [pb], [T["tmp"]])
                      self.ts(T["tmp"].t[:, :], T["tmp"].t[:, :], 1e-12, None, ALU.max, None, [T["tmp"]], [T["tmp"]])
                      fw.op('dve', lambda e: e.reciprocal(T["tmp"].t[:, :], T["tmp"].t[:, :]), reads=[T["tmp"]], writes=[T["tmp"]])
                      self.tt(T["kk"].t[:, :], T["kkr"].t[:, :], T["tmp"].t[:, :], ALU.mult, [T["kkr"], T["tmp"]], [T["kk"]])
                      self.ts(T["tmp"].t[:, :], T["a"].t[:, :], k_a.t[:, hp:hp + 1], omka.t[:, hp:hp + 1], ALU.mult, ALU.add,
                              [T["a"], k_a, omka], [T["tmp"]])
                      self.tt(T["kf"].t[:, :], xk.t[:, :], T["tmp"].t[:, :], ALU.mult, [xk, T["tmp"]], [T["kf"]])
                      self.tt(T["bb"].t[:, :], T["kk"].t[:, :], T["a"].t[:, :], ALU.mult, [T["kk"], T["a"]], [T["bb"]])
                      self.stt(ATt.t[:, :], T["kk"].t[:, :], -1.0, T["e3"].t[:, :], ALU.mult, ALU.mult, [T["kk"], T["e3"]], [ATt])
                      self.tt(RTt.t[:, :], xr.t[:, :], T["e1"].t[:, :], ALU.mult, [xr, T["e1"]], [RTt])
                      self.tt(KTt.t[:, :], T["kf"].t[:, :], T["e2"].t[:, :], ALU.mult, [T["kf"], T["e2"]], [KTt])
                      self.tt(BTt.t[:, :], T["bb"].t[:, :], T["e2"].t[:, :], ALU.mult, [T["bb"], T["e2"]], [BTt])
                      self.stt(t3b.t[:, :], xr.t[:, :], r_k.t[:, hp:hp + 1], T["kf"].t[:, :], ALU.mult, ALU.mult,
                               [xr, r_k, T["kf"]], [t3b])
                      pb = self.bank()
                      self.mm(pb.t[:, :], bones.t[:, :], t3b.t[:, :], True, True, [bones, t3b], [pb])
                      self.tt(bonus.t[:, :], pb.t[:, :], xv.t[:, :], ALU.mult, [pb, xv], [bonus])
                      self.ck(0.7)
                      for half in range(2):
                          for (srcT, dstT, mode) in ((T["kf"], Ktok, 1), (T["bb"], Btok, 1), (xv, Vtok, 0)):
                              pb = self.bank()
                              for cc in range(4):
                                  c = half * 4 + cc
                                  self.tr(pb.t[0:64, cc * 128:(cc + 1) * 128], srcT.t[:, c * 64:(c + 1) * 64], identf.t[:, :],
                                          [srcT, identf], [pb])
                              pv = pb.t[0:64, :].rearrange("p (c f) -> p c f", c=4)
                              if mode:
                                  self.tt(dstT.t[:, half * 4:half * 4 + 4, :], pv, eR.t[:, half * 4:half * 4 + 4, :], ALU.mult,
                                          [pb, (eR, half)], [(dstT, half)])
                              else:
                                  self.cp(dstT.t[:, half * 4:half * 4 + 4, :], pv, [pb], [(dstT, half)], eng='act')

                      self.ck(0.8)
                      def chunk_prep(c, res):
                          cc = slice(c * 64, (c + 1) * 64)
                          A = Ach[c % 2]
                          Xc = Xi[c % 2]
                          for h in range(2):
                              hr = slice(h * 64, (h + 1) * 64)
                              pb = self.bank()
                              ops = ((BTt, ATt), (ATt, BTt), (KTt, ATt), (BTt, RTt), (KTt, RTt))
                              for i, (l_, r_) in enumerate(ops):
                                  self.mm(pb.t[0:64, i * 64:(i + 1) * 64], l_.t[hr, cc], r_.t[hr, cc], True, True, [l_, r_], [pb])
                              self.tt(A[h].t[:, :], pb.t[0:64, 0:320], mk.t[:, :], ALU.mult, [pb, mk], [A[h]])
                              self.cp(Xc[0].t[:, h, 0:128], A[h].t[:, 0:128], [A[h]], [(Xc[0], (h, 'n'))], eng='act')
                              self.tt(Xc[0].t[:, h, 128:192], A[h].t[:, 0:64], identb.t[:, :], ALU.add, [A[h], identb], [(Xc[0], (h, 'm'))])
                          yield
                          cur = 0
                          for ph in range(6):
                              pb = self.bank()
                              src, dstx = Xc[cur], Xc[1 - cur]
                              for h in range(2):
                                  N_, NT_, M_ = src.t[:, h, 0:64], src.t[:, h, 64:128], src.t[:, h, 128:192]
                                  o = h * 192
                                  if ph < 5:
                                      self.mm(pb.t[0:64, o:o + 64], NT_, N_, True, True, [src], [pb])
                                      self.mm(pb.t[0:64, o + 64:o + 128], N_, NT_, True, True, [src], [pb])
                                  if ph >= 1:
                                      self.mm(pb.t[0:64, o + 128:o + 192], NT_, M_, True, True, [src], [pb])
                              pv = pb.t[0:64, 0:384].rearrange("p (h x) -> p h x", h=2)
                              if ph < 5:
                                  self.cp(dstx.t[:, :, 0:128], pv[:, :, 0:128], [pb], [(dstx, (0, 'n')), (dstx, (1, 'n'))], eng='act')
                              if ph >= 1:
                                  self.tt(dstx.t[:, :, 128:192], pv[:, :, 128:192], src.t[:, :, 128:192], ALU.add, [pb, src], [(dstx, (0, 'm')), (dstx, (1, 'm'))])
                              else:
                                  self.cp(dstx.t[:, :, 128:192], src.t[:, :, 128:192], [src], [(dstx, (0, 'm')), (dstx, (1, 'm'))])
                              cur = 1 - cur
                              yield
                          res[c] = (A, Xc[cur])

                      def chunk_chain(c, A, Xf):
                          cc = slice(c * 64, (c + 1) * 64)
                          zb, ub, yb, sbk = self.bank(), self.bank(), self.bank(), self.bank()
                          hold = [zb, ub, yb, sbk]
                          self.held.update(hold)
                          self.mm(zb.t[0:64, 0:128], ATt.t[:, cc], STb.t[:, hp, :], True, False, [ATt, (STb, hp)], [zb])
                          for h in range(2):
                              hr = slice(h * 64, (h + 1) * 64)
                              self.mm(zb.t[0:64, hr], A[h].t[:, 128:192], Vtok.t[:, c, hr], False, h == 1, [A[h], Vtok], [zb])
                          self.cp(Zb.t[:, :], zb.t[0:64, 0:128], [zb], [Zb], eng='act')
                          yield
                          for h in range(2):
                              hr = slice(h * 64, (h + 1) * 64)
                              self.mm(ub.t[0:64, hr], Xf.t[:, h, 128:192], Zb.t[:, hr], True, True, [Xf, Zb], [ub])
                          self.cp(Ub.t[:, :], ub.t[0:64, 0:128], [ub], [Ub])
                          yield
                          self.mm(sbk.t[:, 0:128], Btok.t[:, c, :], Ub.t[:, :], True, False, [Btok, Ub], [sbk])
                          self.mm(sbk.t[:, 0:128], Ktok.t[:, c, :], Vtok.t[:, c, :], False, True, [Ktok, Vtok], [sbk])
                          for h in range(2):
                              hr = slice(h * 64, (h + 1) * 64)
                              self.stt(ST.t[hr, hp, :], ST.t[hr, hp, :], Pend.t[hr, c:c + 1], sbk.t[hr, hr], ALU.mult, ALU.add,
                                       [(ST, hp), Pend, sbk], [(ST, hp)])
                          yield
                          self.mm(yb.t[0:64, 0:128], RTt.t[:, cc], STb.t[:, hp, :], True, False, [RTt, (STb, hp)], [yb])
                          for h in range(2):
                              hr = slice(h * 64, (h + 1) * 64)
                              self.mm(yb.t[0:64, hr], A[h].t[:, 256:320], Vtok.t[:, c, hr], False, False, [A[h], Vtok], [yb])
                              self.mm(yb.t[0:64, hr], A[h].t[:, 192:256], Ub.t[:, hr], False, h == 1, [A[h], Ub], [yb])
                          self.cp(Ytok.t[:, c, :], yb.t[0:64, 0:128], [yb], [(Ytok, c)], eng='act')
                          for h in range(2):
                              hr = slice(h * 64, (h + 1) * 64)
                              self.cp(STb.t[hr, hp, hr], ST.t[hr, hp, :], [(ST, hp)], [(STb, hp)], eng='act')
                          self.held.difference_update(hold)
                          yield

                      preps = {}
                      for _ in chunk_prep(0, preps):
                          pass
                      for c in range(8):
                          A, Xf = preps[c]
                          g1 = chunk_prep(c + 1, preps) if c + 1 < 8 else iter(())
                          g2 = chunk_chain(c, A, Xf)
                          while True:
                              d2 = next(g2, 0) == 0
                              d1 = next(g1, 0) == 0
                              if d1 and d2:
                                  break

                      self.ck(0.9)
                      yv = Ytok.t[:, :, :].rearrange("p c (h v) -> p (c h) v", h=2)
                      fw.op('dve', lambda e: e.reduce_sum(gs1.t[:, :], yv, AX.X), reads=[Ytok], writes=[gs1])
                      ysq = self.xn
                      ysq3 = ysq.t[0:64, :].rearrange("p (c f) -> p c f", c=8)
                      self.act(ysq3, Ytok.t[:, :, :], AF.Square, [Ytok], [ysq])
                      ysv = ysq.t[0:64, :].rearrange("p (c h v) -> p (c h) v", c=8, h=2)
                      fw.op('dve', lambda e: e.reduce_sum(gs2.t[:, :], ysv, AX.X), reads=[ysq], writes=[gs2])
                      self.ts(gs1.t[:, :], gs1.t[:, :], 1.0 / 64, None, ALU.mult, None, [gs1], [gs1])
                      self.tt(ysq.t[0:64, 0:16], gs1.t[:, :], gs1.t[:, :], ALU.mult, [gs1], [ysq])
                      self.stt(gs2.t[:, :], gs2.t[:, :], 1.0 / 64, ysq.t[0:64, 0:16], ALU.mult, ALU.subtract, [gs2, ysq], [gs2])
                      self.ts(gs2.t[:, :], gs2.t[:, :], GN_EPS, None, ALU.add, None, [gs2], [gs2])
                      self.act(gs2.t[:, :], gs2.t[:, :], AF.Sqrt, [gs2], [gs2])
                      fw.op('dve', lambda e: e.reciprocal(gs2.t[:, :], gs2.t[:, :]), reads=[gs2], writes=[gs2])
                      mb = gs1.t[:, :].unsqueeze(2).to_broadcast([64, 16, 64])
                      rb = gs2.t[:, :].unsqueeze(2).to_broadcast([64, 16, 64])
                      self.tt(yv, yv, mb, ALU.subtract, [Ytok, gs1], [Ytok])
                      self.tt(yv, yv, rb, ALU.mult, [Ytok, gs2], [Ytok])
                      pb = self.bank()
                      for c in range(8):
                          self.tr(pb.t[:, c * 64:(c + 1) * 64], Ytok.t[:, c, :], identf.t[0:64, 0:64], [Ytok, identf], [pb])
                      self.act(T["tmp"].t[:, :], pb.t[:, :], AF.Identity, [pb, ln_w, ln_b], [T["tmp"]],
                               bias=ln_b.t[:, hp:hp + 1], scale=ln_w.t[:, hp:hp + 1])
                      self.tt(T["tmp"].t[:, :], T["tmp"].t[:, :], bonus.t[:, :], ALU.add, [T["tmp"], bonus], [T["tmp"]])
                      self.tt(rwT.t[:, hp, :], T["tmp"].t[:, :], gg.t[:, :], ALU.mult, [T["tmp"], gg], [(rwT, hp)])

                  if self.stages < 2:
                      continue
                  wq_ = self.wload(win_v[:, :, QOFF:QOFF + 512], 8, 512, reads=[win_b])
                  for cb in range(4):
                      pb = self.bank()
                      for k in range(8):
                          self.mm(pb.t[:, :], wq_.t[:, k, cb * 128:(cb + 1) * 128], hT.t[:, k, :], k == 0, k == 7, [wq_, hT], [pb])
                      self.act(QT.t[:, cb, :], pb.t[:, :], AF.Copy, [pb], [(QT, cb)], scale=0.125)
                  wk2 = self.wload(win_v[:, :, KOFF:KOFF + 512], 8, 512, reads=[win_b])
                  slot0 = (w * 4) % RING
                  for cb in range(4):
                      pb = self.bank()
                      for k in range(8):
                          self.mm(pb.t[:, :], wk2.t[:, k, cb * 128:(cb + 1) * 128], hT.t[:, k, :], k == 0, k == 7, [wk2, hT], [pb])
                      self.cp(KT.t[:, cb, slot0 * 128:slot0 * 128 + W], pb.t[:, :], [pb], [KT], eng='act' if cb % 2 else 'dve')
                  wv2 = self.wload(win_v[:, :, VOFF:VOFF + 512], 8, 512, reads=[win_b])
                  for (wt, isv) in ((wk2, 0), (wv2, 1)):
                      for blk in range(4):
                          tok0 = t0 + blk * 128
                          need_out = tok0 >= seq - keep
                          if not isv and not need_out:
                              continue
                          pb = self.bank()
                          for k in range(8):
                              self.mm(pb.t[:, :], hT.t[:, k, blk * 128:(blk + 1) * 128], wt.t[:, k, :], k == 0, k == 7, [wt, hT], [pb])
                          if isv:
                              slot = (w * 4 + blk) % RING
                              self.cp(V1.t[:, slot, :, 1:65], pb.t[:, :].rearrange("p (h v) -> p h v", h=8), [pb], [V1], eng='act')
                              fw.op('pool', lambda e, slot=slot: e.memset(V1.t[:, slot, :, 0:1], 1.0), writes=[V1])
                          if need_out:
                              stg = stV if isv else stK
                              self.cp(stg.t[:, :], pb.t[:, :], [pb], [stg])
                              dst = (vwp if isv else kwp)[tok0 - (seq - keep):tok0 - (seq - keep) + 128, :]
                              fw.dma('sp', dst, stg.t[:, :], reads=[stg], out_dram=True)

                  for h in range(8):
                      hp, hr = h // 2, slice((h % 2) * 64, (h % 2) * 64 + 64)
                      EMh = EM2[0]
                      fw.dma('sp', EMh.t[:, :, :], em_bf[h], reads=[(em_b, h)], writes=[EMh])
                      for bq in range(4):
                          gbq = w * 4 + bq
                          ob = self.bank()
                          self.held.add(ob)
                          for g0 in range(0, NKB, 4):
                              n = min(4, NKB - g0)
                              sb_ = self.bank()
                              for jj in range(n):
                                  slot = (gbq - (g0 + jj)) % RING
                                  self.mm(sb_.t[:, jj * 128:(jj + 1) * 128], KT.t[hr, hp, slot * 128:(slot + 1) * 128],
                                          QT.t[hr, hp, bq * 128:(bq + 1) * 128], True, True, [KT, (QT, hp)], [sb_])
                              self.act(Eb.t[:, 0:n * 128], sb_.t[:, 0:n * 128], AF.Exp, [sb_], [Eb])
                              self.tt(Pm.t[:, 0:n * 128], Eb.t[:, 0:n * 128],
                                      EMh.t[:, g0:g0 + n, :].rearrange("p j q -> p (j q)"), ALU.mult, [Eb, EMh], [Pm])
                              for jj in range(n):
                                  j = g0 + jj
                                  slot = (gbq - j) % RING
                                  self.mm(ob.t[:, 0:65], Pm.t[:, jj * 128:(jj + 1) * 128], V1.t[:, slot, h, :],
                                          j == 0, j == NKB - 1, [Pm, V1], [ob])
                          self.held.discard(ob)
                          fw.op('dve', lambda e, ob=ob: e.reciprocal(orec.t[:, :], ob.t[:, 0:1]), reads=[ob], writes=[orec])
                          self.ts(Otok[bq].t[:, h * 64:(h + 1) * 64], ob.t[:, 1:65], orec.t[:, 0:1], None, ALU.mult, None,
                                  [ob, orec], [(Otok[bq], h)])
                  for bq in range(4):
                      Ot = Otok[bq]
                      ss = self.ss
                      self.act(self.junk.t[:, 0, :], Ot.t[:, :], AF.Square, [Ot], [self.junk, ss], accum=ss.t[:, :])
                      self.ts(ss.t[:, :], ss.t[:, :], 1.0 / RW, RMS_EPS, ALU.mult, ALU.add, [ss], [ss])
                      self.act(ss.t[:, :], ss.t[:, :], AF.Sqrt, [ss], [ss])
                      fw.op('dve', lambda e: e.reciprocal(self.ss.t[:, :], self.ss.t[:, :]), reads=[ss], writes=[ss])
                      self.stt(Ot.t[:, :], Ot.t[:, :], ss.t[:, 0:1], again.t[:, :], ALU.mult, ALU.mult, [Ot, ss, again], [Ot])
                      pb = self.bank()
                      for cb in range(4):
                          self.tr(pb.t[:, cb * 128:(cb + 1) * 128], Ot.t[:, cb * 128:(cb + 1) * 128], identf.t[:, :], [Ot, identf], [pb])
                      self.cp(atT.t[:, :, bq * 128:(bq + 1) * 128], pb.t[:, :].rearrange("p (c t) -> p c t", c=4), [pb], [(atT, bq)], eng='act')

                  if self.stages < 4:
                      continue
                  tail(4, pp[t0:t0 + W, :], yp[t0:t0 + W, :])
              except StopBuild:
                break

            fw.dma('sp', shp_o.rearrange("(k p) -> p k", p=128), carry.t[:, :], reads=[carry], out_dram=True, nc_ok=True)
            for hp in range(4):
                pb = self.bank()
                self.tr(pb.t[0:64, 0:128], ST.t[:, hp, :], identf.t[:, :], [ST, identf], [pb])
                self.cp(stK.t[0:64, 0:128], pb.t[0:64, 0:128], [pb], [stK])
                for h2 in range(2):
                    fw.dma('sp', wkvp[hp * 2 + h2], stK.t[0:64, h2 * 64:(h2 + 1) * 64], reads=[stK], out_dram=True)

            pstk.close()
            fw.barrier()
            if 'sample' in SKIP:
                fw.stk = st
                fw.finish()
                return nc
            sstk = ExitStack()
            fw.stk = sstk
            T = P_["T"]
            Xs = X[0]
            Ptok = sb("Ptok", [128, IN_COLS], F32)
            XM = sb("XM", [128, RW_COLS], F32)
            ATtok = sb("ATtok", [128, RW], F32)
            Gtok = sb("Gtok", [128, RW], F32)
            Btk = sb("Btk", [128, RW], F32)
            n8 = sb("n8", [128, 8], F32)
            m8 = sb("m8", [128, 8], F32)
            v8 = sb("v8", [128, 8], F32)
            QTs = sb("QTs", [128, 4, 128], BF16)
            KTn = sb("KTn", [128, 4, 128], BF16)
            Vn1 = sb("Vn1", [128, 8, 65], BF16)
            scrA_b = Buf(None, "scrA")
            scrB_b = Buf(None, "scrB")

            fw.dma('sp', Xs.t[:, :], xs, writes=[Xs])
            self.norm_T([Xs], g_mix, hT)
            for ci, c0 in enumerate(range(0, IN_COLS, 512)):
                cw = min(512, IN_COLS - c0)
                wl = self.wload(win_v[:, :, c0:c0 + cw], 8, cw, reads=[win_b])
                pb = self.bank()
                for k in range(8):
                    self.mm(pb.t[:, 0:cw], hT.t[:, k, 0:128], wl.t[:, k, 0:cw], k == 0, k == 7, [wl, hT], [pb])
                self.cp(Ptok.t[:, c0:c0 + cw], pb.t[:, 0:cw], [pb], [(Ptok, ci)], eng='act' if ci % 2 else 'dve')
            fw.dma('sp', shs, Ptok.t[7:128:8, 0:RW_COLS], reads=[Ptok], out_dram=True)
            for (off_, dst_) in ((KOFF, kws), (VOFF, vws)):
                def mknew(e, off_=off_, dst_=dst_):
                    return [e.dma_start(out=dst_[j, MAXWIN - 8:MAXWIN, :], in_=Ptok.t[8 * j:8 * j + 8, off_:off_ + 512])
                            for j in range(NSEQ)]
                fw.dma('sp', None, None, fn=mknew, n=NSEQ, reads=[Ptok], out_dram=True)
            for cb in range(4):
                pb = self.bank()
                self.tr(pb.t[:, 0:128], Ptok.t[:, QOFF + cb * 128:QOFF + (cb + 1) * 128], identf.t[:, :], [Ptok, identf], [pb])
                self.act(QTs.t[:, cb, :], pb.t[:, 0:128], AF.Copy, [pb], [(QTs, cb)], scale=0.125)
                pb = self.bank()
                self.tr(pb.t[:, 0:128], Ptok.t[:, KOFF + cb * 128:KOFF + (cb + 1) * 128], identf.t[:, :], [Ptok, identf], [pb])
                self.cp(KTn.t[:, cb, :], pb.t[:, 0:128], [pb], [(KTn, cb)])
            fw.op('pool', lambda e: e.memset(Vn1.t[:, :, 0:1], 1.0), writes=[Vn1])
            self.cp(Vn1.t[:, :, 1:65], Ptok.t[:, VOFF:VOFF + 512].rearrange("p (h v) -> p h v", h=8), [Ptok, Vn1], [Vn1])

            rstk = ExitStack()
            fw.stk = rstk
            S = sb("S", [128, 4096], F32)
            TM = [sb("TM%d" % i, [128, 4096], F32) for i in range(2)]
            RLh = sb("RLh", [128, 6, 8, 64], F32)
            Yrl = sb("Yrl", [128, 8, 64], F32)
            skk = sb("skk", [128, 64], F32)
            reps = {}
            for n_ in ("a0", "k_k", "k_a", "r_k", "ln_x_w", "ln_x_b"):
                reps[n_] = sb("rep_" + n_, [128, RW], F32)
                fw.dma('sp', reps[n_].t[:, :], dr[n_].partition_broadcast(128), writes=[reps[n_]])
            fw.dma('sp', S.t[:, :], swkv, writes=[S])
            mu_rep = TM[0]
            fw.dma('sp', mu_rep.t[:, 0:RW_COLS], dr["mu"].partition_broadcast(128), writes=[mu_rep])
            fw.dma('sp', XM.t[1:128, :], Ptok.t[0:127, 0:RW_COLS], reads=[Ptok], writes=[XM])
            fw.dma('sp', XM.t[0:128:8, :], sshift, writes=[XM])
            Pr = Ptok.t[:, 0:RW_COLS]
            self.tt(XM.t[:, :], XM.t[:, :], Pr, ALU.subtract, [XM, Ptok], [XM])
            self.tt(XM.t[:, :], XM.t[:, :], mu_rep.t[:, 0:RW_COLS], ALU.mult, [XM, mu_rep], [XM])
            self.tt(XM.t[:, :], XM.t[:, :], Pr, ALU.add, [XM, Ptok], [XM])
            xr, xk, xv = XM.t[:, 0:512], XM.t[:, 512:1024], XM.t[:, 1024:1536]
            RLb = [X[1], X[1], X[2], X[2], X[3], X[3]]
            RL = [RLb[j].t[:, (j % 2) * 512:(j % 2) * 512 + 512] for j in range(6)]
            h3 = lambda ap: ap.rearrange("p (h n) -> p h n", h=8)
            bc3 = lambda b8: b8.t[:, :].unsqueeze(2).to_broadcast([128, 8, 64])
            pb = self.bank()
            self.tr(pb.t[:, 0:128], XM.t[:, 1536:1664], identf.t[:, :], [XM, identf], [pb])
            self.act(Lt.t[0:32, 0:128], pb.t[0:32, 0:128], AF.Tanh, [pb], [(Lt, 0)])
            self.cp(Lt.t[32:64, 0:128], pb.t[32:64, 0:128], [pb], [(Lt, 1)])
            self.act(Lt.t[64:128, 0:128], pb.t[64:128, 0:128], AF.Sigmoid, [pb], [(Lt, 2)])
            tA, tB, Atok, kkr = T["e1"], T["e2"], T["a"], T["kkr"]
            pb = self.bank()
            self.mm(pb.t[:, :], Lt.t[0:32, 0:128], LW.t[0:32, :], True, True, [Lt, LW], [pb])
            self.tt(tA.t[:, :], pb.t[:, :], w0rep.t[:, :], ALU.add, [pb, w0rep], [tA])
            self.act(tA.t[:, :], tA.t[:, :], AF.Sigmoid, [tA], [tA])
            self.act(RL[1], tA.t[:, :], AF.Exp, [tA], [X[1]], scale=-DECAY_C)
            pb = self.bank()
            self.mm(pb.t[:, :], Lt.t[32:64, 0:128], LW.t[32:64, :], True, True, [Lt, LW], [pb])
            self.tt(Atok.t[:, :], pb.t[:, :], reps["a0"].t[:, :], ALU.add, [pb, reps["a0"]], [Atok])
            self.act(Atok.t[:, :], Atok.t[:, :], AF.Sigmoid, [Atok], [Atok])
            pb = self.bank()
            self.mm(pb.t[:, :], Lt.t[64:128, 0:128], LW.t[64:128, :], True, True, [Lt, LW], [pb])
            self.cp(Gtok.t[:, :], pb.t[:, :], [pb], [Gtok], eng='act')
            self.tt(kkr.t[:, :], xk, reps["k_k"].t[:, :], ALU.mult, [XM, reps["k_k"]], [kkr])
            self.act(tB.t[:, :], kkr.t[:, :], AF.Square, [kkr], [tB])
            fw.op('dve', lambda e: e.reduce_sum(n8.t[:, :], h3(tB.t[:, :]), AX.X), reads=[tB], writes=[n8])
            self.act(n8.t[:, :], n8.t[:, :], AF.Sqrt, [n8], [n8])
            self.ts(n8.t[:, :], n8.t[:, :], 1e-12, None, ALU.max, None, [n8], [n8])
            fw.op('dve', lambda e: e.reciprocal(n8.t[:, :], n8.t[:, :]), reads=[n8], writes=[n8])
            self.tt(h3(RL[4]), h3(kkr.t[:, :]), bc3(n8), ALU.mult, [kkr, n8], [X[3]])
            self.ts(tA.t[:, :], Atok.t[:, :], -1.0, None, ALU.add, None, [Atok], [tA])
            self.tt(tA.t[:, :], tA.t[:, :], reps["k_a"].t[:, :], ALU.mult, [tA, reps["k_a"]], [tA])
            self.ts(tA.t[:, :], tA.t[:, :], 1.0, None, ALU.add, None, [tA], [tA])
            self.tt(RL[2], xk, tA.t[:, :], ALU.mult, [XM, tA], [X[2]])
            self.tt(RL[5], RL[4], Atok.t[:, :], ALU.mult, [X[3], Atok], [X[3]])
            self.cp(RL[0], xr, [XM], [X[1]])
            self.cp(RL[3], xv, [XM], [X[2]], eng='act')
            self.tt(tA.t[:, :], xr, RL[2], ALU.mult, [XM, X[2]], [tA])
            self.tt(tA.t[:, :], tA.t[:, :], reps["r_k"].t[:, :], ALU.mult, [tA, reps["r_k"]], [tA])
            fw.op('dve', lambda e: e.reduce_sum(m8.t[:, :], h3(tA.t[:, :]), AX.X), reads=[tA], writes=[m8])
            self.tt(h3(Btk.t[:, :]), h3(xv), bc3(m8), ALU.mult, [XM, m8], [Btk])
            for i3 in range(3):
                fw.dma('sp', scrA[:, i3 * 1024:(i3 + 1) * 1024], X[1 + i3].t[:, :], reads=[X[1 + i3]], writes=[(scrA_b, i3)])
            scrA_v = scrA.rearrange("p (j h n) -> p j h n", j=6, h=8)
            for b in range(NSEQ):
                def mkrl(e, b=b):
                    return [e.dma_start(out=RLh.t[b * 8:(b + 1) * 8, j, :, :],
                                        in_=scrA_v[b * 8:(b + 1) * 8, j, :, :].rearrange("t h n -> h t n"),
                                        allow_slow_non_contiguous=True) for j in range(6)]
                fw.dma('sp' if b % 2 else 'act', None, None, fn=mkrl, n=6, reads=[scrA_b], writes=[(RLh, b)])
            S3 = S.t[:, :].rearrange("p (v k) -> p v k", v=64)
            TM3 = [t_.t[:, :].rearrange("p (v k) -> p v k", v=64) for t_ in TM]
            kbc = lambda j, t: RLh.t[:, j, t, :].unsqueeze(1).to_broadcast([128, 64, 64])
            vbc = lambda ap: ap.unsqueeze(2).to_broadcast([128, 64, 64])
            for t in range(8 if 'rec' not in SKIP else 0):
                self.tt(TM3[0], S3, kbc(4, t), ALU.mult, [S, RLh], [TM[0]])
                fw.op('dve', lambda e: e.reduce_sum(skk.t[:, :], TM3[0], AX.X), reads=[TM[0]], writes=[skk])
                self.tt(S3, S3, kbc(1, t), ALU.mult, [S, RLh], [S])
                self.tt(TM3[1], vbc(skk.t[:, :]), kbc(5, t), ALU.mult, [skk, RLh], [TM[1]], eng='pool')
                self.tt(S3, S3, TM3[1], ALU.subtract, [S, TM[1]], [S])
                self.tt(TM3[0], vbc(RLh.t[:, 3, t, :]), kbc(2, t), ALU.mult, [RLh], [TM[0]], eng='pool')
                self.tt(S3, S3, TM3[0], ALU.add, [S, TM[0]], [S])
                self.tt(TM3[1], S3, kbc(0, t), ALU.mult, [S, RLh], [TM[1]])
                fw.op('dve', lambda e, t=t: e.reduce_sum(Yrl.t[:, t, :], TM3[1], AX.X), reads=[TM[1]], writes=[(Yrl, t)])
            fw.dma('sp', wkvs, S.t[:, :], reads=[S], out_dram=True)
            fw.dma('sp', scrB, Yrl.t[:, :, :].rearrange("p t v -> p (t v)"), reads=[Yrl], writes=[scrB_b])
            Ytk = T["kk"]
            scrB_v = scrB.rearrange("p (t v) -> p t v", t=8)
            for b in range(NSEQ):
                fw.dma('sp' if b % 2 else 'act', Ytk.t[b * 8:(b + 1) * 8, :].rearrange("t (h v) -> t h v", h=8),
                       scrB_v[b * 8:(b + 1) * 8, :, :].rearrange("h t v -> t h v"),
                       reads=[scrB_b], writes=[(Ytk, b)], nc_ok=True)
            yvs = h3(Ytk.t[:, :])
            fw.op('dve', lambda e: e.reduce_sum(m8.t[:, :], yvs, AX.X), reads=[Ytk], writes=[m8])
            self.act(tB.t[:, :], Ytk.t[:, :], AF.Square, [Ytk], [tB])
            fw.op('dve', lambda e: e.reduce_sum(v8.t[:, :], h3(tB.t[:, :]), AX.X), reads=[tB], writes=[v8])
            self.ts(m8.t[:, :], m8.t[:, :], 1.0 / 64, None, ALU.mult, None, [m8], [m8])
            self.tt(n8.t[:, :], m8.t[:, :], m8.t[:, :], ALU.mult, [m8], [n8])
            self.stt(v8.t[:, :], v8.t[:, :], 1.0 / 64, n8.t[:, :], ALU.mult, ALU.subtract, [v8, n8], [v8])
            self.ts(v8.t[:, :], v8.t[:, :], GN_EPS, None, ALU.add, None, [v8], [v8])
            self.act(v8.t[:, :], v8.t[:, :], AF.Sqrt, [v8], [v8])
            fw.op('dve', lambda e: e.reciprocal(v8.t[:, :], v8.t[:, :]), reads=[v8], writes=[v8])
            self.tt(yvs, yvs, bc3(m8), ALU.subtract, [Ytk, m8], [Ytk])
            self.tt(yvs, yvs, bc3(v8), ALU.mult, [Ytk, v8], [Ytk])
            self.tt(Ytk.t[:, :], Ytk.t[:, :], reps["ln_x_w"].t[:, :], ALU.mult, [Ytk, reps["ln_x_w"]], [Ytk])
            self.tt(Ytk.t[:, :], Ytk.t[:, :], reps["ln_x_b"].t[:, :], ALU.add, [Ytk, reps["ln_x_b"]], [Ytk])
            self.tt(Ytk.t[:, :], Ytk.t[:, :], Btk.t[:, :], ALU.add, [Ytk, Btk], [Ytk])
            self.tt(Ytk.t[:, :], Ytk.t[:, :], Gtok.t[:, :], ALU.mult, [Ytk, Gtok], [Ytk])
            pb = self.bank()
            for cb in range(4):
                self.tr(pb.t[:, cb * 128:(cb + 1) * 128], Ytk.t[:, cb * 128:(cb + 1) * 128], identf.t[:, :], [Ytk, identf], [pb])
            self.cp(rwT.t[:, :, 0:128], pb.t[:, :].rearrange("p (c t) -> p c t", c=4), [pb], [rwT], eng='act')
            rstk.close()
            fw.barrier()

            astk = ExitStack()
            fw.stk = astk
            EMs = sb("EMs", [128, 1024], BF16)
            EMn = sb("EMn", [128, 1024], BF16)
            fw.dma('pool', EMs.t[:, :], dr["ems"], writes=[EMs])
            fw.dma('pool', EMn.t[:, :], dr["emn"], writes=[EMn])
            Kld = [sb("Kld%d" % i, [128, 4, 512], F32) for i in range(2)]
            Vld = [sb("Vld%d" % i, [128, 4, 512], F32) for i in range(2)]
            KTg = [sb("KTg%d" % i, [128, 4, 512], BF16) for i in range(2)]
            V1s = [sb("V1s%d" % i, [128, 8, 8, 65], BF16) for i in range(2)]
            for v1 in V1s:
                fw.op('pool', lambda e, v1=v1: e.memset(v1.t[:, :, :, 0:1], 1.0), writes=[v1])
            Ebs = sb("Ebs", [128, 512], BF16)
            Pms = [sb("Pms%d" % i, [128, 512], BF16) for i in range(2)]
            Pmn = sb("Pmn", [128, 64], BF16)
            ostg = [sb("ostg%d" % i, [8, 512], F32) for i in range(2)]
            orec8 = sb("orec8", [8, 8], F32)
            Qbd = sb("Qbd", [128, NSEQ, 4, 64], BF16)
            fw.op('pool', lambda e: e.memset(Qbd.t[:, :, :, :], 0.0), writes=[Qbd])
            for hp in range(4):
                for h2 in range(2):
                    hr = slice(h2 * 64, (h2 + 1) * 64)
                    c0 = (2 * hp + h2) * 8
                    self.cp(Qbd.t[hr, :, hp, c0:c0 + 8], QTs.t[hr, hp, :].rearrange("p (b t) -> p b t", b=NSEQ), [QTs, Qbd], [Qbd],
                            eng='act' if h2 else 'dve')
            zl = sb("zl", [128, 8], BF16)
            fw.op('pool', lambda e: e.memset(zl.t[:, :], 0.0), writes=[zl])
            gi = 0
            for b in range(NSEQ if 'attn' not in SKIP else 0):
                O2 = [self.bank(), self.bank()]
                self.held.update(O2)
                qs = slice(b * 8, (b + 1) * 8)
                for O in (O2 if 'a_pv' not in SKIP else []):
                    self.mm(O.t[0:8, 0:260], zl.t[:, :], EMs.t[:, 0:260], True, False, [zl, EMs], [O])
                for half in range(2):
                    sc = self.bank()
                    self.held.add(sc)
                    V1h = V1s[half]
                    for g2 in range(2):
                        g = half * 2 + g2
                        Kt, Vt, KTt = Kld[gi % 2], Vld[gi % 2], KTg[gi % 2]
                        gi += 1
                        fw.dma('sp', Kt.t[:, :, :], kc[b, g * 512:(g + 1) * 512, :].rearrange("(j p) c -> p j c", p=128), writes=[Kt])
                        fw.dma('sp', Vt.t[:, :, :], vc[b, g * 512:(g + 1) * 512, :].rearrange("(j p) c -> p j c", p=128), writes=[Vt])
                        for hp in range(4):
                            pb = self.bank()
                            for j in range(4):
                                self.tr(pb.t[:, j * 128:(j + 1) * 128], Kt.t[:, j, hp * 128:(hp + 1) * 128], identf.t[:, :], [Kt, identf], [pb])
                            self.cp(KTt.t[:, hp, :], pb.t[:, :], [pb], [(KTt, hp)], eng='act' if hp % 2 else 'dve')
                        if 'a_vc' not in SKIP:
                            self.cp(V1h.t[:, g2 * 4:(g2 + 1) * 4, :, 1:65], Vt.t[:, :, :].rearrange("p j (h v) -> p j h v", h=8),
                                    [Vt], [(V1h, g2)], eng=VC_ENG)
                        for j in range(4 if 'a_sc' not in SKIP else 0):
                            jj = g2 * 4 + j
                            for hp in range(4):
                                self.mm(sc.t[:, jj * 64:(jj + 1) * 64], KTt.t[:, hp, j * 128:(j + 1) * 128], Qbd.t[:, b, hp, :],
                                        hp == 0, hp == 3, [(KTt, hp), Qbd], [sc])
                    self.held.discard(sc)
                    if 'a_sc' in SKIP:
                        continue
                    self.act(Ebs.t[:, :], sc.t[:, :], AF.Exp, [sc], [Ebs])
                    Pm_ = Pms[half]
                    self.tt(Pm_.t[:, :], Ebs.t[:, :], EMs.t[:, half * 512:(half + 1) * 512], ALU.mult, [Ebs, EMs], [Pm_])
                    for jj in range(8 if 'a_pv' not in SKIP else 0):
                        for h in range(8):
                            O = O2[h // 4]
                            c0 = (jj * 8 + h) * 8
                            self.mm(O.t[0:8, (h % 4) * 65:(h % 4) * 65 + 65], Pm_.t[:, c0:c0 + 8], V1h.t[:, jj, h, :],
                                    False, False, [Pm_, V1h], [O])
                self.held.difference_update(O2)
                if 'a_pv' in SKIP:
                    continue
                self.held.update(O2)
                sc = self.bank()
                for hp in range(4):
                    self.mm(sc.t[:, 0:64], KTn.t[:, hp, :], Qbd.t[:, b, hp, :], hp == 0, hp == 3, [KTn, Qbd], [sc])
                self.act(Ebs.t[:, 0:64], sc.t[:, 0:64], AF.Exp, [sc], [Ebs])
                self.tt(Pmn.t[:, :], Ebs.t[:, 0:64], EMn.t[:, b * 64:(b + 1) * 64], ALU.mult, [Ebs, EMn], [Pmn])
                for h in range(8):
                    O = O2[h // 4]
                    self.mm(O.t[0:8, (h % 4) * 65:(h % 4) * 65 + 65], Pmn.t[:, h * 8:h * 8 + 8], Vn1.t[:, h, :], False, h % 4 == 3, [Pmn, Vn1], [O])
                self.held.difference_update(O2)
                og = ostg[b % 2]
                for oi, O in enumerate(O2):
                    ov = O.t[0:8, 0:260].rearrange("p (h c) -> p h c", h=4)
                    fw.op('dve', lambda e, ov=ov, oi=oi: e.reciprocal(orec8.t[:, oi * 4:(oi + 1) * 4].unsqueeze(2), ov[:, :, 0:1]),
                          reads=[O], writes=[(orec8, oi)])
                    self.tt(og.t[:, oi * 256:(oi + 1) * 256].rearrange("p (h v) -> p h v", h=4), ov[:, :, 1:65],
                            orec8.t[:, oi * 4:(oi + 1) * 4].unsqueeze(2).to_broadcast([8, 4, 64]), ALU.mult,
                            [O, (orec8, oi)], [(og, oi)])
                fw.dma('sp', ATtok.t[b * 8:(b + 1) * 8, :], og.t[:, :], reads=[og], writes=[(ATtok, b)])
            ss = self.ss
            self.act(self.junk.t[:, 0, :], ATtok.t[:, :], AF.Square, [ATtok], [self.junk, ss], accum=ss.t[:, :])
            self.ts(ss.t[:, :], ss.t[:, :], 1.0 / RW, RMS_EPS, ALU.mult, ALU.add, [ss], [ss])
            self.act(ss.t[:, :], ss.t[:, :], AF.Sqrt, [ss], [ss])
            fw.op('dve', lambda e: e.reciprocal(self.ss.t[:, :], self.ss.t[:, :]), reads=[ss], writes=[ss])
            self.stt(ATtok.t[:, :], ATtok.t[:, :], ss.t[:, 0:1], again.t[:, :], ALU.mult, ALU.mult, [ATtok, ss, again], [ATtok])
            pb = self.bank()
            for cb in range(4):
                self.tr(pb.t[:, cb * 128:(cb + 1) * 128], ATtok.t[:, cb * 128:(cb + 1) * 128], identf.t[:, :], [ATtok, identf], [pb])
            self.cp(atT.t[:, :, 0:128], pb.t[:, :].rearrange("p (c t) -> p c t", c=4), [pb], [atT], eng='act')
            astk.close()
            fw.barrier()
            tail(1, pps, ys)
            sstk.close()
            fw.stk = st
            fw.finish()
        return nc


def build_prompt_nc(seq, stages=99):
    b = Builder(seq, stages=stages)
    return b.build()


_NC_CACHE = {}


def kernel(**inputs):
    inputs = {k: np.asarray(v) for k, v in inputs.items()}
    B, S = inputs["x_prompt"].shape[0], inputs["x_prompt"].shape[1]
    if "nc" not in _NC_CACHE:
        _NC_CACHE["nc"] = build_prompt_nc(S)
    nc = _NC_CACHE["nc"]
    consts = _host_consts()
    f = np.ascontiguousarray
    wts = {}
    for k, shp in WEIGHT_SHAPES.items():
        v = inputs[k]
        v = v[0] if k != "norm_final" else v
        wts[k] = f(v.reshape(shp).astype(np.float32, copy=False))
    in_maps = []
    L = NSEQ
    for c in range(8):
        b = c % B
        sl = slice(c * L, (c + 1) * L)
        m = {"xp": f(inputs["x_prompt"][b]), "pp": f(inputs["p_prompt"][0, b]),
             "xs": f(inputs["x_sample"][sl].reshape(L * 8, D)),
             "pps": f(inputs["p_sample"][0, sl].reshape(L * 8, 256)),
             "swkv": f(inputs["state_wkv"][0, sl].reshape(L * 8, 4096)),
             "sshift": f(inputs["state_shift"][0, sl]),
             "kc": f(inputs["cache_k_win"][0, sl].reshape(L, MAXWIN, 512)),
             "vc": f(inputs["cache_v_win"][0, sl].reshape(L, MAXWIN, 512))}
        m.update(wts)
        m.update(consts)
        in_maps.append(m)
    res = run_bass_kernel_spmd(nc, in_maps, core_ids=list(range(8))).results
    f32 = np.float32
    keep = min(MAXWIN, S)
    y_prompt = np.stack([res[b]["yp"] for b in range(B)]).astype(f32, copy=False)
    wkv_p = np.stack([res[b]["wkvp"] for b in range(B)])[None].astype(f32, copy=False)
    shift_p = np.stack([res[b]["shp"] for b in range(B)])[None].astype(f32, copy=False)
    kwin_p = np.stack([res[b]["kwp"].reshape(keep, 8, 64) for b in range(B)])[None].astype(f32, copy=False)
    vwin_p = np.stack([res[b]["vwp"].reshape(keep, 8, 64) for b in range(B)])[None].astype(f32, copy=False)
    y_sample = np.concatenate([res[c]["ys"].reshape(L, 8, D) for c in range(8)]).astype(f32, copy=False)
    wkv_s = np.concatenate([res[c]["wkvs"].reshape(L, 8, 64, 64) for c in range(8)])[None].astype(f32, copy=False)
    shift_s = np.concatenate([res[c]["shs"] for c in range(8)])[None].astype(f32, copy=False)
    kwin_s = np.concatenate([res[c]["kws"].reshape(L, MAXWIN, 8, 64) for c in range(8)])[None].astype(f32, copy=False)
    vwin_s = np.concatenate([res[c]["vws"].reshape(L, MAXWIN, 8, 64) for c in range(8)])[None].astype(f32, copy=False)
    return (y_prompt, y_sample, wkv_p, shift_p, kwin_p, vwin_p, wkv_s, shift_s, kwin_s, vwin_s)
```

```python
import numpy as np
from contextlib import ExitStack
import concourse.bass as bass
import concourse.mybir as mybir
from concourse.bass_utils import run_bass_kernel_spmd

F32 = mybir.dt.float32
BF16 = mybir.dt.bfloat16
I32 = mybir.dt.int32
AF = mybir.ActivationFunctionType
ALU = mybir.AluOpType
AX = mybir.AxisListType


class Buf:
    def __init__(self, t, name, psum=False):
        self.t = t
        self.name = name
        self.psum = psum
        self.st = {None: [{}, {}]}


def _merge(dst, src):
    for k, v in src.items():
        if dst.get(k, 0) < v:
            dst[k] = v


class FW:
    ENGS = ('pe', 'act', 'dve', 'pool', 'sp')

    def __init__(self, nc, st, n_dma_sems=24):
        self.nc = nc
        self.stk = st
        self.ops = {e: [] for e in self.ENGS}
        self.cnt = {e: 0 for e in self.ENGS}
        self.sem = {}
        self.semobj = {}
        for e in self.ENGS:
            s = st.enter_context(nc.semaphore("c_" + e))
            self.sem[e] = id(s)
            self.semobj[id(s)] = s
        self.waited = {e: {} for e in self.ENGS}
        self.dsems = {'hw': [], 'sw': []}
        for kind in ('hw', 'sw'):
            for i in range(n_dma_sems):
                s = st.enter_context(nc.semaphore("d%s%d" % (kind, i)))
                self.semobj[id(s)] = s
                self.dsems[kind].append([s, 0])
        self.dnext = {'hw': 0, 'sw': 0}
        self.out_tokens = {}
        self.n_inst = 0
        self.extra_dsems = []

    def sb(self, name, shape, dtype):
        t = self.stk.enter_context(self.nc.sbuf_tensor("s_" + name, list(shape), dtype))
        return Buf(t, name)

    def ps(self, name, shape, dtype):
        t = self.stk.enter_context(self.nc.psum_tensor("p_" + name, list(shape), dtype))
        return Buf(t, name, psum=True)

    @staticmethod
    def _norm(x):
        if isinstance(x, tuple):
            if x[0].psum:
                return (x[0], None)
            return x
        return (x, None)

    def _deps(self, reads, writes):
        deps = {}
        for b, k in map(self._norm, reads):
            if k is None:
                for kk, (w, r) in b.st.items():
                    _merge(deps, w)
                    if b.psum:
                        _merge(deps, r)
            else:
                _merge(deps, b.st[None][0])
                if k in b.st:
                    _merge(deps, b.st[k][0])
        for b, k in map(self._norm, writes):
            if k is None:
                for kk, (w, r) in b.st.items():
                    _merge(deps, w)
                    _merge(deps, r)
            else:
                _merge(deps, b.st[None][0])
                _merge(deps, b.st[None][1])
                if k in b.st:
                    _merge(deps, b.st[k][0])
                    _merge(deps, b.st[k][1])
        return deps

    def _update(self, reads, writes, tok):
        sid, val = tok
        for b, k in map(self._norm, reads):
            s = b.st.setdefault(k, [{}, {}])
            if s[1].get(sid, 0) < val:
                s[1][sid] = val
        for b, k in map(self._norm, writes):
            if k is None:
                b.st = {None: [{sid: val}, {}]}
            else:
                b.st[k] = [{sid: val}, {}]

    def _emit_waits(self, eng, deps, skip_self):
        lst = []
        wd = self.waited[eng]
        for sid, val in deps.items():
            if skip_self and sid == self.sem[eng]:
                continue
            if wd.get(sid, 0) >= val:
                continue
            wd[sid] = val
            lst.append((self.semobj[sid], val))
        return lst

    def op(self, eng, fn, reads=(), writes=()):
        deps = self._deps(reads, writes)
        waits = self._emit_waits(eng, deps, skip_self=(eng == 'pe'))
        self.cnt[eng] += 1
        idx = self.cnt[eng]
        semo = self.semobj[self.sem[eng]]

        def run(e, fn=fn, waits=waits, semo=semo):
            for s, v in waits:
                e.wait_ge(s, v)
            fn(e).then_inc(semo, 1)
        self.ops[eng].append(run)
        self.waited[eng][self.sem[eng]] = max(self.waited[eng].get(self.sem[eng], 0), 0)
        self._update(reads, writes, (self.sem[eng], idx))
        self.n_inst += 1
        return (self.sem[eng], idx)

    def dma(self, eng, out, in_, reads=(), writes=(), out_dram=False, nc_ok=False, n=1, fn=None, ent=None):
        deps = self._deps(reads, writes)
        kind = 'sw' if eng == 'pool' else 'hw'
        if ent is None:
            ent = self.dsems[kind][self.dnext[kind]]
            self.dnext[kind] = (self.dnext[kind] + 1) % len(self.dsems[kind])
            if ent[1] > 0:
                deps[id(ent[0])] = max(deps.get(id(ent[0]), 0), ent[1])
        dsem = ent[0]
        waits = self._emit_waits(eng, deps, skip_self=False)
        ent[1] += 16 * n
        val = ent[1]

        def run(e, waits=waits, dsem=dsem):
            for s, v in waits:
                e.wait_ge(s, v)
            if fn is not None:
                for ins in fn(e):
                    ins.then_inc(dsem, 16)
            else:
                kw = {}
                if nc_ok:
                    kw['allow_slow_non_contiguous'] = True
                e.dma_start(out=out, in_=in_, **kw).then_inc(dsem, 16)
        self.ops[eng].append(run)
        tok = (id(dsem), val)
        self._update(reads, writes, tok)
        if out_dram:
            self.out_tokens[id(dsem)] = val
        self.n_inst += 1
        return tok

    def new_dsem(self, name):
        s = self.stk.enter_context(self.nc.semaphore(name))
        self.semobj[id(s)] = s
        ent = [s, 0]
        self.extra_dsems.append(ent)
        return ent

    def barrier(self):
        for eng in self.ENGS:
            waits = []
            wd = self.waited[eng]
            for e2 in self.ENGS:
                sid = self.sem[e2]
                if e2 != eng and self.cnt[e2] > wd.get(sid, 0):
                    waits.append((self.semobj[sid], self.cnt[e2]))
                    wd[sid] = self.cnt[e2]
            for kind in ('hw', 'sw'):
                for sm, v in self.dsems[kind]:
                    if v > wd.get(id(sm), 0):
                        waits.append((sm, v))
                        wd[id(sm)] = v

            def run(e, waits=waits):
                for sm, v in waits:
                    e.wait_ge(sm, v)
            self.ops[eng].append(run)

    def make_identity(self, ident, dtype_f32_tmp=None):
        def f1(e):
            return e.memset(ident.t[:, :], 1.0)
        self.op('pool', f1, writes=[ident])
        def f2(e):
            return e.affine_select(ident.t[:, :], ident.t[:, :], [[-1, 128]], ALU.is_equal, 0.0,
                                   base=0, channel_multiplier=1)
        self.op('pool', f2, reads=[ident], writes=[ident])

    def finish(self):
        finals = [(self.semobj[sid], v) for sid, v in self.out_tokens.items()]
        nc = self.nc
        with nc.Block() as block:
            @block.tensor
            def _(e):
                for f in self.ops['pe']:
                    f(e)

            @block.scalar
            def _(e):
                for f in self.ops['act']:
                    f(e)

            @block.vector
            def _(e):
                for f in self.ops['dve']:
                    f(e)

            @block.gpsimd
            def _(e):
                for f in self.ops['pool']:
                    f(e)

            @block.sync
            def _(e):
                for f in self.ops['sp']:
                    f(e)
                for s, v in finals:
                    e.wait_ge(s, v)


D = 1024
RW = 512
RW_COLS = 1664
IN_COLS = 3200
QOFF, KOFF, VOFF = 1664, 1664 + 512, 1664 + 1024
W = 512
RING = 24
NKB = 17
RMS_EPS = 1e-6
GN_EPS = 64e-5
DECAY_C = float(np.exp(-0.5))
MAXWIN = 2048
NSEQ = 16


def _host_consts():
    c = {}
    s = np.arange(64)[:, None]
    t = np.arange(64)[None, :]
    c["cI"] = (-DECAY_C * (s <= t)).astype(np.float32)
    c["cS"] = (-DECAY_C * (s < t)).astype(np.float32)
    c["cR"] = (-DECAY_C * (s > t)).astype(np.float32)
    up_s = (s < t).astype(np.float32)
    up_i = (s <= t).astype(np.float32)
    lo_s = (t < s).astype(np.float32)
    c["mk"] = np.concatenate([up_s, lo_s, up_s, up_i, up_i], axis=1)
    bo = np.zeros((128, 128), np.float32)
    bo[:64, :64] = 1
    bo[64:, 64:] = 1
    c["bones"] = bo
    slopes = 2.0 ** (-8.0 * np.arange(1, 9) / 8)
    ki = np.arange(128)[:, None]
    qi = np.arange(128)[None, :]
    em = np.zeros((8, 128, NKB, 128), np.float32)
    for j in range(NKB):
        dl = 128 * j + qi - ki
        mult = np.zeros_like(dl, dtype=np.float64)
        for wd, dil in ((128, 1), (512, 4), (2048, 16)):
            mult += ((dl >= 0) & (dl % dil == 0) & (dl <= wd))
        for h in range(8):
            em[h, :, j, :] = mult * np.exp(-slopes[h] * np.maximum(dl, 0))
    c["em"] = em
    def fac(dl):
        mult = np.zeros(dl.shape, np.float64)
        for wd, dil in ((128, 1), (512, 4), (2048, 16)):
            mult += ((dl >= 0) & (dl % dil == 0) & (dl <= wd))
        return mult[None] * np.exp(-slopes[:, None, None, None] * np.maximum(dl, 0)[None])
    p_ = np.arange(128)[:, None, None]
    blk_ = np.arange(16)[None, :, None]
    t_ = np.arange(8)[None, None, :]
    ems = fac(MAXWIN + t_ - (blk_ * 128 + p_))
    c["ems"] = np.ascontiguousarray(ems.transpose(1, 2, 0, 3)).reshape(128, 1024).astype(np.float32)
    bq_ = np.arange(NSEQ)[None, :, None]
    emn = fac(t_ - (p_ % 8) + 0 * bq_) * ((p_ // 8) == bq_)[None]
    c["emn"] = np.ascontiguousarray(emn.transpose(1, 2, 0, 3)).reshape(128, 1024).astype(np.float32)
    return c


CONST_SHAPES = {"cI": [64, 64], "cS": [64, 64], "cR": [64, 64], "mk": [64, 320],
                "bones": [128, 128], "em": [8, 128, NKB, 128], "ems": [128, 1024], "emn": [128, 1024]}

WEIGHT_SHAPES = {
    "norm_mix": [D], "w_in": [D, IN_COLS], "mu": [RW_COLS], "w0": [RW], "w2": [32, RW],
    "a0": [RW], "a2": [32, RW], "g2": [64, RW], "k_k": [RW], "k_a": [RW], "r_k": [RW],
    "ln_x_w": [RW], "ln_x_b": [RW], "attn_gain": [RW], "w_out": [D, D], "norm_ffn": [D],
    "w_group": [D, 4], "b_group": [4], "w_expert_router": [D, 32], "b_expert_router": [32],
    "w_gate": [32, D, 256], "w_up": [32, D, 256], "w_down": [32, 256, D],
    "norm_ple": [D], "w_ple": [256, D], "w_ple_gate": [D, D], "norm_final": [D],
}


import os as _os
SKIP = set(_os.environ.get('KSKIP', '').split(','))
VC_ENG = _os.environ.get('KVCENG', 'pool')


class StopBuild(Exception):
    pass


class Builder:
    def __init__(self, seq, stages=99, dbg=()):
        self.seq = seq
        self.nw = seq // W
        self.stages = stages
        self.dbgnames = set(dbg)
        self.nc = bass.Bass("TRN2", target_bir_lowering=False)
        self.dr = {}
        self.dbg_out = {}

    def din(self, name, shape):
        self.dr[name] = self.nc.dram_tensor(name, list(shape), F32, kind="ExternalInput").ap()
        return self.dr[name]

    def dout(self, name, shape):
        self.dr[name] = self.nc.dram_tensor(name, list(shape), F32, kind="ExternalOutput").ap()
        return self.dr[name]

    def ck(self, x):
        if self.stages < x:
            raise StopBuild()

    def bank(self):
        for _ in range(8):
            b = self.PS[self.pnext]
            self.pnext = (self.pnext + 1) % 8
            if b not in self.held:
                return b
        raise RuntimeError("no free bank")

    def mm(self, out, lhsT, rhs, start, stop, r, w):
        self.fw.op('pe', lambda e: e.matmul(out, lhsT, rhs, start=start, stop=stop), reads=r, writes=w)

    def tr(self, out, in_, ident, r, w):
        self.fw.op('pe', lambda e: e.transpose(out, in_, ident), reads=r, writes=w)

    def act(self, out, in_, func, r, w, bias=None, scale=None, accum=None):
        kw = {}
        if bias is not None:
            kw['bias'] = bias
        if scale is not None:
            kw['scale'] = scale
        if accum is not None:
            kw['accum_out'] = accum
        self.fw.op('act', lambda e: e.activation(out, in_, func, **kw), reads=r, writes=w)

    def tt(self, out, a, b, op, r, w, eng='dve'):
        self.fw.op(eng, lambda e: e.tensor_tensor(out, a, b, op), reads=r, writes=w)

    def ts(self, out, a, s1, s2, op0, op1, r, w, eng='dve'):
        if s2 is None:
            self.fw.op(eng, lambda e: e.tensor_scalar(out, a, s1, None, op0), reads=r, writes=w)
        else:
            self.fw.op(eng, lambda e: e.tensor_scalar(out, a, s1, s2, op0, op1), reads=r, writes=w)

    def stt(self, out, a, sc, b, op0, op1, r, w, eng='dve'):
        self.fw.op(eng, lambda e: e.scalar_tensor_tensor(out, a, sc, b, op0, op1), reads=r, writes=w)

    def cp(self, out, in_, r, w, eng='dve'):
        if eng == 'act':
            self.act(out, in_, AF.Copy, r, w)
        else:
            self.fw.op(eng, lambda e: e.tensor_copy(out, in_), reads=r, writes=w)

    def dbg(self, name, ap, shape, reads):
        if name not in self.dbgnames:
            return
        d = self.dout("dbg_" + name, shape)
        self.fw.dma('sp', d, ap, reads=reads, out_dram=True)

    def wload(self, src_ap, rows_k, cols, eng='sp', reads=()):
        t = self.WP[self.wnext]
        self.wnext = (self.wnext + 1) % len(self.WP)
        self.fw.dma(eng, t.t[:, 0:rows_k, 0:cols], src_ap, reads=list(reads), writes=[t], nc_ok=True)
        return t

    def vec_load(self, name, nk, dst=None):
        t = self.fw.sb("v_" + name, [128, nk], F32) if dst is None else dst
        self.fw.dma('sp', t.t[:, :], self.dr[name].rearrange("(k p) -> p k", p=128), writes=[t], nc_ok=True)
        return t

    def norm_T(self, Xs, gain, hT, rows=128):
        fw = self.fw
        for blk, Xb in enumerate(Xs):
            ss = self.ss
            self.act(self.junk.t[0:rows, :, :].rearrange("p a b -> p (a b)"), Xb.t[0:rows, :], AF.Square, [Xb], [self.junk, ss], accum=ss.t[0:rows, :])
            self.ts(ss.t[0:rows, :], ss.t[0:rows, :], 1.0 / D, RMS_EPS, ALU.mult, ALU.add, [ss], [ss])
            self.act(ss.t[0:rows, :], ss.t[0:rows, :], AF.Sqrt, [ss], [ss])
            fw.op('dve', lambda e, ss=ss: e.reciprocal(ss.t[0:rows, :], ss.t[0:rows, :]), reads=[ss], writes=[ss])
            xn = self.xn
            self.ts(xn.t[0:rows, :], Xb.t[0:rows, :], ss.t[0:rows, 0:1], None, ALU.mult, None, [Xb, ss], [xn])
            for half in range(2):
                pb = self.bank()
                for kk in range(4):
                    k = half * 4 + kk
                    self.tr(pb.t[:, kk * 128:kk * 128 + rows], xn.t[0:rows, k * 128:(k + 1) * 128],
                            self.identf.t[0:rows, 0:rows], [xn, self.identf], [pb])
                src = pb.t[:, :].rearrange("p (k t) -> p k t", k=4)[:, :, 0:rows]
                gb = gain.t[:, half * 4:half * 4 + 4].unsqueeze(2).to_broadcast([128, 4, rows])
                self.tt(hT.t[:, half * 4:half * 4 + 4, blk * 128:blk * 128 + rows], src, gb, ALU.mult,
                        [pb, gain], [(hT, ('b', blk, half))])

    def build(self):
        nc = self.nc
        seq, nw = self.seq, self.nw
        keep = min(MAXWIN, seq)
        xp = self.din("xp", [seq, D])
        pp = self.din("pp", [seq, 256])
        for k, shp in WEIGHT_SHAPES.items():
            self.din(k, shp)
        for k, shp in CONST_SHAPES.items():
            self.din(k, shp)
        yp = self.dout("yp", [seq, D])
        wkvp = self.dout("wkvp", [8, 64, 64])
        shp_o = self.dout("shp", [RW_COLS])
        kwp = self.dout("kwp", [keep, 512])
        vwp = self.dout("vwp", [keep, 512])
        xs = self.din("xs", [128, D])
        pps = self.din("pps", [128, 256])
        swkv = self.din("swkv", [128, 4096])
        sshift = self.din("sshift", [NSEQ, RW_COLS])
        kc = self.din("kc", [NSEQ, MAXWIN, 512])
        vc = self.din("vc", [NSEQ, MAXWIN, 512])
        ys = self.dout("ys", [128, D])
        wkvs = self.dout("wkvs", [128, 4096])
        shs = self.dout("shs", [NSEQ, RW_COLS])
        kws = self.dout("kws", [NSEQ, MAXWIN, 512])
        vws = self.dout("vws", [NSEQ, MAXWIN, 512])
        scrA = self.dout("scrA", [128, 6 * 512])
        scrB = self.dout("scrB", [128, 512])
        dr = self.dr
        with ExitStack() as st:
            fw = self.fw = FW(nc, st, n_dma_sems=16)
            sb = fw.sb
            self.PS = [fw.ps("ps%d" % i, [128, 512], F32) for i in range(8)]
            self.pnext = 0
            self.held = set()
            self.WP = [sb("wp%d" % i, [128, 8, 512], BF16) for i in range(2)]
            self.wnext = 0
            self.identf = identf = sb("identf", [128, 128], F32)
            fw.make_identity(identf)
            g_mix, g_ffn, g_ple = self.vec_load("norm_mix", 8), self.vec_load("norm_ffn", 8), self.vec_load("norm_ple", 8)
            gfin = sb("gfin", [128, D], F32)
            fw.dma('sp', gfin.t[:, :], dr["norm_final"].partition_broadcast(128), writes=[gfin])
            again = sb("again", [128, RW], F32)
            fw.dma('sp', again.t[:, :], dr["attn_gain"].partition_broadcast(128), writes=[again])
            w0rep = sb("w0rep", [128, RW], F32)
            fw.dma('sp', w0rep.t[:, :], dr["w0"].partition_broadcast(128), writes=[w0rep])
            LW = sb("LW", [128, RW], BF16)
            fw.dma('pool', LW.t[0:32, :], dr["w2"], writes=[(LW, 0)])
            fw.dma('pool', LW.t[32:64, :], dr["a2"], writes=[(LW, 1)])
            fw.dma('pool', LW.t[64:128, :], dr["g2"], writes=[(LW, 2)])
            Wrt = sb("Wrt", [128, 8, 36], BF16)
            fw.dma('pool', Wrt.t[:, :, 0:4], dr["w_group"].rearrange("(k p) c -> p k c", p=128), writes=[(Wrt, 0)])
            fw.dma('pool', Wrt.t[:, :, 4:36], dr["w_expert_router"].rearrange("(k p) c -> p k c", p=128), writes=[(Wrt, 1)])
            brep = sb("brep", [128, 36], F32)
            fw.dma('sp', brep.t[:, 0:4], dr["b_group"].partition_broadcast(128), writes=[(brep, 0)])
            fw.dma('sp', brep.t[:, 4:36], dr["b_expert_router"].partition_broadcast(128), writes=[(brep, 1)])
            X = [sb("X%d" % i, [128, D], F32) for i in range(4)]
            self.xn = sb("xn", [128, D], F32)
            self.ss = sb("ss", [128, 1], F32)
            hT = sb("hT", [128, 8, W], BF16)
            rwT = sb("rwT", [128, 4, W], BF16)
            atT = sb("atT", [128, 4, W], BF16)
            Lt = sb("Lt", [128, W], BF16)
            T = {n: sb("t_" + n, [128, W], F32) for n in ("a", "kkr", "kk", "e1", "e2", "tmp")}
            lg = sb("lg", [128, 36], F32)
            comb = sb("comb", [128, 4, 32], F32)
            rt = {n: sb("r_" + n, [128, 1], F32) for n in ("gmax", "ngmax", "sumg", "gw", "nm1", "e2", "den", "w1", "w2")}
            oh = sb("oh", [128, 4], F32)
            eg = sb("eg", [128, 4], F32)
            elm = sb("elm", [128, 32], F32)
            top8 = sb("top8", [128, 8], F32)
            c1 = sb("c1", [128, 32], F32)
            sgt = T["tmp"]
            Aff = sb("Aff", [128, 2, W], BF16)
            self.junk = Aff
            Wd = [sb("Wd%d" % i, [128, 2, D], BF16) for i in range(1)]
            pT = sb("pT", [128, 2, W], BF16)
            pin = sb("pin", [128, 256], F32)
            yst = self.xn
            P_ = dict(identf=identf, g_mix=g_mix, g_ffn=g_ffn, g_ple=g_ple, gfin=gfin, again=again, w0rep=w0rep, LW=LW,
                      X=X, hT=hT, rwT=rwT, atT=atT, Lt=Lt, T=T)
            self.P_ = P_

            roll_ent = fw.new_dsem("d_roll")
            nrows = MAXWIN - 8
            piece = nrows // 4
            for j in range(NSEQ if 'roll' not in SKIP else 0):
                for src, dst in ((dr["kc"], dr["kws"]), (dr["vc"], dr["vws"])):
                    def mkroll(e, j=j, src=src, dst=dst):
                        return [e.dma_start(out=dst[j, a:a + piece, :], in_=src[j, a + 8:a + piece + 8, :])
                                for a in range(0, nrows, piece)]
                    fw.dma('act', None, None, fn=mkroll, n=4, out_dram=True, ent=roll_ent)

            pstk = ExitStack()
            fw.stk = pstk
            cI, cS, cR = sb("cI", [64, 64], F32), sb("cS", [64, 64], F32), sb("cR", [64, 64], F32)
            mk = sb("mk", [64, 320], F32)
            bones = sb("bones", [128, 128], BF16)
            for t_, n_ in ((cI, "cI"), (cS, "cS"), (cR, "cR"), (mk, "mk")):
                fw.dma('sp', t_.t[:, :], dr[n_], writes=[t_])
            fw.dma('pool', bones.t[:, :], dr["bones"], writes=[bones])
            identb = sb("identb", [64, 64], BF16)
            self.cp(identb.t[:, :], identf.t[0:64, 0:64], [identf], [identb])
            mu = self.vec_load("mu", 13)
            w0, a0, k_k, k_a = self.vec_load("w0", 4), self.vec_load("a0", 4), self.vec_load("k_k", 4), self.vec_load("k_a", 4)
            r_k, ln_w, ln_b = self.vec_load("r_k", 4), self.vec_load("ln_x_w", 4), self.vec_load("ln_x_b", 4)
            omka = sb("omka", [128, 4], F32)
            self.ts(omka.t[:, :], k_a.t[:, :], -1.0, 1.0, ALU.mult, ALU.add, [k_a], [omka])
            carry = sb("carry", [128, 13], F32)
            fw.op('pool', lambda e: e.memset(carry.t[:, :], 0.0), writes=[carry])
            ST = sb("ST", [128, 4, 64], F32)
            STb = sb("STb", [128, 4, 128], BF16)
            fw.op('pool', lambda e: e.memset(ST.t[:, :, :], 0.0), writes=[ST])
            fw.op('pool', lambda e: e.memset(STb.t[:, :, :], 0.0), writes=[STb])
            KT = sb("KT", [128, 4, RING * 128], BF16)
            V1 = sb("V1", [128, RING, 8, 65], BF16)
            fw.op('pool', lambda e: e.memset(KT.t[:, :, :], 0.0), writes=[KT])
            fw.op('pool', lambda e: e.memset(V1.t[:, :, :, :], 0.0), writes=[V1])
            QT = sb("QT", [128, 4, W], BF16)
            EM2 = [sb("EMh%d" % i, [128, NKB, 128], BF16) for i in range(1)]
            Pk = [sb("Pk%d" % i, [128, W + 1], F32) for i in range(2)]
            dlt = sb("dlt", [128, W], F32)
            xm = [sb("xm%d" % i, [128, W], F32) for i in range(3)]
            xml = xm[0]
            T = dict(T)
            T["kf"] = T["kkr"]
            T["bb"] = T["tmp"]
            T["e3"] = dlt
            sqb = sb("sqb", [128, W], BF16)
            t3b = sb("t3b", [128, W], BF16)
            bonus = sb("bonus", [128, W], F32)
            gg = sb("gg", [128, W], F32)
            ATt, RTt, KTt, BTt = (sb(n, [128, W], BF16) for n in ("ATt", "RTt", "KTt", "BTt"))
            Pend = sb("Pend", [128, 8], F32)
            sgwT = sb("sgwT", [64, 8, 128], F32)
            eR = sb("eR", [64, 8, 128], F32)
            Vtok, Ktok, Btok = (sb(n, [64, 8, 128], BF16) for n in ("Vtok", "Ktok", "Btok"))
            Ytok = sb("Ytok", [64, 8, 128], F32)
            Ach = [[sb("Ach%d_%d" % (i, h), [64, 320], BF16) for h in range(2)] for i in range(2)]
            Xi = [[sb("Xi%d_%d" % (i, p), [64, 2, 192], BF16) for p in range(2)] for i in range(2)]
            Zb = sb("Zb", [64, 128], BF16)
            Ub = sb("Ub", [64, 128], BF16)
            gs1, gs2 = sb("gs1", [64, 16], F32), sb("gs2", [64, 16], F32)
            Otok = [sb("Otok%d" % i, [128, RW], F32) for i in range(4)]
            orec = sb("orec", [128, 1], F32)
            EbL = [sb("Eb%d" % i, [128, 512], BF16) for i in range(2)]
            PmL = [sb("Pm%d" % i, [128, 512], BF16) for i in range(2)]
            stK, stV = Otok[3], Otok[2]

            def wscr(name, shape):
                return nc.dram_tensor(name, list(shape), BF16, kind="Internal").ap(), Buf(None, name)
            kp = lambda ap: ap.rearrange("(k p) c -> p k c", p=128)
            win_bf, win_b = wscr("win_bf", [128, 8, IN_COLS])
            em_bf, em_b = wscr("em_bf", [8, 128, NKB, 128])
            wo_bf, wo_b = wscr("wo_bf", [128, 8, D])
            wgu_bf, wgu_b = wscr("wgu_bf", [32, 128, 8, 512])
            wd_bf, wd_b = wscr("wd_bf", [32, 128, 2, D])
            wpg_bf, wpg_b = wscr("wpg_bf", [128, 8, D])
            wpl_bf, wpl_b = wscr("wpl_bf", [128, 2, D])
            for c0 in range(0, IN_COLS, 640):
                fw.dma('pool', win_bf[:, :, c0:c0 + 640], kp(dr["w_in"])[:, :, c0:c0 + 640], writes=[(win_b, c0)], nc_ok=True)
            for h in range(8):
                fw.dma('pool', em_bf[h], dr["em"][h], writes=[(em_b, h)])
            for hf in range(2):
                fw.dma('pool', wo_bf[:, :, hf * 512:(hf + 1) * 512], kp(dr["w_out"])[:, :, hf * 512:(hf + 1) * 512], writes=[(wo_b, hf)], nc_ok=True)
            for ex in range(32):
                fw.dma('pool', wgu_bf[ex][:, :, 0:256], kp(dr["w_gate"][ex]), writes=[(wgu_b, (ex, 0))], nc_ok=True)
                fw.dma('pool', wgu_bf[ex][:, :, 256:512], kp(dr["w_up"][ex]), writes=[(wgu_b, (ex, 1))], nc_ok=True)
                fw.dma('pool', wd_bf[ex], kp(dr["w_down"][ex]), writes=[(wd_b, ex)], nc_ok=True)
            for hf in range(2):
                fw.dma('pool', wpg_bf[:, :, hf * 512:(hf + 1) * 512], kp(dr["w_ple_gate"])[:, :, hf * 512:(hf + 1) * 512], writes=[(wpg_b, hf)], nc_ok=True)
            fw.dma('pool', wpl_bf, kp(dr["w_ple"]), writes=[wpl_b], nc_ok=True)
            win_v = win_bf

            def tail(nblk, pp_ap, y_ap):
                nt = nblk * 128
                wo = [self.wload(wo_bf[:, :, hf * 512:(hf + 1) * 512], 8, 512, reads=[(wo_b, hf)]) for hf in range(2)]
                for blk in range(nblk):
                    bs = slice(blk * 128, (blk + 1) * 128)
                    for hf in range(2):
                        pb = self.bank()
                        for k in range(8):
                            lhs = rwT.t[:, k, bs] if k < 4 else atT.t[:, k - 4, bs]
                            self.mm(pb.t[:, :], lhs, wo[hf].t[:, k, :], k == 0, k == 7, [rwT, atT, wo[hf]], [pb])
                        self.tt(X[blk].t[:, hf * 512:(hf + 1) * 512], X[blk].t[:, hf * 512:(hf + 1) * 512], pb.t[:, :], ALU.add,
                                [pb, X[blk]], [X[blk]])

                self.norm_T(X[0:nblk], g_ffn, hT)
                for blk in range(nblk):
                    bs = slice(blk * 128, (blk + 1) * 128)
                    pb = self.bank()
                    for k in range(8):
                        self.mm(pb.t[:, 0:36], hT.t[:, k, bs], Wrt.t[:, k, :], k == 0, k == 7, [hT, Wrt], [pb])
                    self.tt(lg.t[:, :], pb.t[:, 0:36], brep.t[:, :], ALU.add, [pb, brep], [lg])
                    fw.op('dve', lambda e: e.reduce_max(rt["gmax"].t[:, :], lg.t[:, 0:4], AX.X), reads=[lg], writes=[rt["gmax"]])
                    self.ts(oh.t[:, :], lg.t[:, 0:4], rt["gmax"].t[:, 0:1], None, ALU.is_equal, None, [lg, rt["gmax"]], [oh])
                    self.ts(rt["ngmax"].t[:, :], rt["gmax"].t[:, :], -1.0, None, ALU.mult, None, [rt["gmax"]], [rt["ngmax"]])
                    self.act(eg.t[:, :], lg.t[:, 0:4], AF.Exp, [lg, rt["ngmax"]], [eg, rt["sumg"]], bias=rt["ngmax"].t[:, 0:1],
                             accum=rt["sumg"].t[:, :])
                    fw.op('dve', lambda e: e.reciprocal(rt["gw"].t[:, :], rt["sumg"].t[:, :]), reads=[rt["sumg"]], writes=[rt["gw"]])
                    self.ts(oh.t[:, :], oh.t[:, :], 1e30, -1e30, ALU.mult, ALU.add, [oh], [oh])
                    self.tt(elm.t[:, :].rearrange("p (g e) -> p g e", g=4), lg.t[:, 4:36].rearrange("p (g e) -> p g e", g=4),
                            oh.t[:, :].unsqueeze(2).to_broadcast([128, 4, 8]), ALU.add, [lg, oh], [elm])
                    fw.op('dve', lambda e: e.max(top8.t[:, :], elm.t[:, :]), reads=[elm], writes=[top8])
                    self.ts(rt["nm1"].t[:, :], top8.t[:, 0:1], -1.0, None, ALU.mult, None, [top8], [rt["nm1"]])
                    self.act(rt["e2"].t[:, :], top8.t[:, 1:2], AF.Exp, [top8, rt["nm1"]], [rt["e2"]], bias=rt["nm1"].t[:, 0:1])
                    self.ts(rt["den"].t[:, :], rt["e2"].t[:, :], 1.0, None, ALU.add, None, [rt["e2"]], [rt["den"]])
                    fw.op('dve', lambda e: e.reciprocal(rt["den"].t[:, :], rt["den"].t[:, :]), reads=[rt["den"]], writes=[rt["den"]])
                    self.tt(rt["w1"].t[:, :], rt["den"].t[:, :], rt["gw"].t[:, :], ALU.mult, [rt["den"], rt["gw"]], [rt["w1"]])
                    self.tt(rt["w2"].t[:, :], rt["w1"].t[:, :], rt["e2"].t[:, :], ALU.mult, [rt["w1"], rt["e2"]], [rt["w2"]])
                    self.ts(c1.t[:, :], elm.t[:, :], top8.t[:, 0:1], rt["w1"].t[:, 0:1], ALU.is_equal, ALU.mult, [elm, top8, rt["w1"]], [c1])
                    self.ts(comb.t[:, blk, :], elm.t[:, :], top8.t[:, 1:2], rt["w2"].t[:, 0:1], ALU.is_equal, ALU.mult,
                            [elm, top8, rt["w2"]], [(comb, blk)])
                    self.tt(comb.t[:, blk, :], comb.t[:, blk, :], c1.t[:, :], ALU.add, [(comb, blk), c1], [(comb, blk)])
                for ex in range(32):
                    wgu = self.WP[self.wnext]
                    self.wnext = (self.wnext + 1) % len(self.WP)
                    fw.dma('sp', wgu.t[:, :, :], wgu_bf[ex], reads=[(wgu_b, (ex, 0)), (wgu_b, (ex, 1))], writes=[wgu])
                    wd = Wd[ex % len(Wd)]
                    fw.dma('sp', wd.t[:, :, :], wd_bf[ex], reads=[(wd_b, ex)], writes=[wd])
                    for fb in range(2):
                        gbk, ubk = self.bank(), self.bank()
                        for k in range(8):
                            self.mm(gbk.t[:, 0:nt], wgu.t[:, k, fb * 128:(fb + 1) * 128], hT.t[:, k, 0:nt], k == 0, k == 7, [wgu, hT], [gbk])
                        for k in range(8):
                            self.mm(ubk.t[:, 0:nt], wgu.t[:, k, 256 + fb * 128:256 + (fb + 1) * 128], hT.t[:, k, 0:nt], k == 0, k == 7, [wgu, hT], [ubk])
                        self.act(sgt.t[:, 0:nt], gbk.t[:, 0:nt], AF.Silu, [gbk], [sgt])
                        self.tt(Aff.t[:, fb, 0:nt], sgt.t[:, 0:nt], ubk.t[:, 0:nt], ALU.mult, [sgt, ubk], [(Aff, fb)])
                    for blk in range(nblk):
                        bs = slice(blk * 128, (blk + 1) * 128)
                        for hf in range(2):
                            pb = self.bank()
                            for fb in range(2):
                                self.mm(pb.t[:, :], Aff.t[:, fb, bs], wd.t[:, fb, hf * 512:(hf + 1) * 512], fb == 0, fb == 1, [Aff, wd], [pb])
                            xs_ = X[blk].t[:, hf * 512:(hf + 1) * 512]
                            self.stt(xs_, pb.t[:, :], comb.t[:, blk, ex:ex + 1], xs_, ALU.mult, ALU.add, [pb, (comb, blk), X[blk]], [X[blk]])

                self.norm_T(X[0:nblk], g_ple, hT)
                for blk in range(nblk):
                    fw.dma('sp', pin.t[:, :], pp_ap[blk * 128:(blk + 1) * 128, :], writes=[pin])
                    pb = self.bank()
                    for k2 in range(2):
                        self.tr(pb.t[:, k2 * 128:(k2 + 1) * 128], pin.t[:, k2 * 128:(k2 + 1) * 128], identf.t[:, :], [pin, identf], [pb])
                    self.cp(pT.t[:, :, blk * 128:(blk + 1) * 128], pb.t[:, 0:256].rearrange("p (k t) -> p k t", k=2), [pb], [(pT, blk)])
                for hf in range(2):
                    wpg = self.wload(wpg_bf[:, :, hf * 512:(hf + 1) * 512], 8, 512, reads=[(wpg_b, hf)])
                    wpl = self.wload(wpl_bf[:, :, hf * 512:(hf + 1) * 512], 2, 512, reads=[wpl_b])
                    for blk in range(nblk):
                        bs = slice(blk * 128, (blk + 1) * 128)
                        gbk, pbk = self.bank(), self.bank()
                        for k in range(8):
                            self.mm(gbk.t[:, :], hT.t[:, k, bs], wpg.t[:, k, :], k == 0, k == 7, [hT, wpg], [gbk])
                        for k2 in range(2):
                            self.mm(pbk.t[:, :], pT.t[:, k2, bs], wpl.t[:, k2, :], k2 == 0, k2 == 1, [pT, wpl], [pbk])
                        self.act(sgt.t[:, :], gbk.t[:, :], AF.Sigmoid, [gbk], [sgt])
                        self.tt(sgt.t[:, :], sgt.t[:, :], pbk.t[:, :], ALU.mult, [sgt, pbk], [sgt])
                        xs_ = X[blk].t[:, hf * 512:(hf + 1) * 512]
                        self.tt(xs_, xs_, sgt.t[:, :], ALU.add, [X[blk], sgt], [X[blk]])
                for blk in range(nblk):
                    ss = self.ss
                    Xb = X[blk]
                    self.act(self.junk.t[:, :, :].rearrange("p a b -> p (a b)"), Xb.t[:, :], AF.Square, [Xb], [self.junk, ss], accum=ss.t[:, :])
                    self.ts(ss.t[:, :], ss.t[:, :], 1.0 / D, RMS_EPS, ALU.mult, ALU.add, [ss], [ss])
                    self.act(ss.t[:, :], ss.t[:, :], AF.Sqrt, [ss], [ss])
                    fw.op('dve', lambda e: e.reciprocal(self.ss.t[:, :], self.ss.t[:, :]), reads=[ss], writes=[ss])
                    self.stt(yst.t[:, :], Xb.t[:, :], ss.t[:, 0:1], gfin.t[:, :], ALU.mult, ALU.mult, [Xb, ss, gfin], [yst])
                    fw.dma('sp', y_ap[blk * 128:(blk + 1) * 128, :], yst.t[:, :], reads=[yst], out_dram=True)

            for w in range(nw if self.stages >= 0.05 else 0):
              try:
                  t0 = w * W
                  for blk in range(4):
                      fw.dma('sp', X[blk].t[:, :], xp[t0 + blk * 128:t0 + (blk + 1) * 128, :], writes=[X[blk]])
                  self.ck(0.1)
                  self.norm_T(X, g_mix, hT)
                  self.ck(0.2)
                  self.dbg("hT", hT.t[:, :, :], None, [hT]) if False else None

                  def rw_block(ci, col0, dst, wt, wc0):
                      pb = self.bank()
                      for k in range(8):
                          self.mm(pb.t[:, :], wt.t[:, k, wc0:wc0 + 128], hT.t[:, k, :], k == 0, k == 7, [wt, hT], [pb])
                      P = Pk[ci % 2]
                      self.cp(P.t[:, 1:W + 1], pb.t[:, :], [pb], [(P, 1)], eng='act')
                      self.cp(P.t[:, 0:1], carry.t[:, ci:ci + 1], [(carry, ci)], [(P, 0)])
                      self.cp(carry.t[:, ci:ci + 1], P.t[:, W:W + 1], [(P, 1)], [(carry, ci)])
                      self.tt(dlt.t[:, :], P.t[:, 0:W], P.t[:, 1:W + 1], ALU.subtract, [P], [dlt])
                      self.stt(dst.t[:, :], dlt.t[:, :], mu.t[:, ci:ci + 1], P.t[:, 1:W + 1], ALU.mult, ALU.add,
                               [dlt, mu, P], [dst])

                  wl = self.wload(win_v[:, :, 1536:1664], 8, 128, reads=[win_b])
                  rw_block(12, 1536, xml, wl, 0)
                  self.act(Lt.t[0:32, :], xml.t[0:32, :], AF.Tanh, [xml], [(Lt, 0)])
                  self.cp(Lt.t[32:64, :], xml.t[32:64, :], [xml], [(Lt, 1)])
                  self.act(Lt.t[64:128, :], xml.t[64:128, :], AF.Sigmoid, [xml], [(Lt, 2)])

                  self.ck(0.3)
                  for hp in range(4):
                      wt3 = self.WP[self.wnext]
                      self.wnext = (self.wnext + 1) % len(self.WP)
                      for i3 in range(3):
                          c0 = i3 * 512 + hp * 128
                          fw.dma('sp', wt3.t[:, :, i3 * 128:(i3 + 1) * 128], win_v[:, :, c0:c0 + 128], reads=[win_b], writes=[(wt3, i3)], nc_ok=True)
                      rw_block(hp, hp * 128, xm[0], wt3, 0)
                      rw_block(4 + hp, 512 + hp * 128, xm[1], wt3, 128)
                      rw_block(8 + hp, 1024 + hp * 128, xm[2], wt3, 256)
                      self.ck(0.4)
                      xr, xk, xv = xm
                      hc = slice(hp * 128, (hp + 1) * 128)
                      pb = self.bank()
                      self.mm(pb.t[:, :], LW.t[32:64, hc], Lt.t[32:64, :], True, True, [LW, Lt], [pb])
                      self.act(T["a"].t[:, :], pb.t[:, :], AF.Sigmoid, [pb, a0], [T["a"]], bias=a0.t[:, hp:hp + 1])
                      pb = self.bank()
                      self.mm(pb.t[:, :], LW.t[64:128, hc], Lt.t[64:128, :], True, True, [LW, Lt], [pb])
                      self.cp(gg.t[:, :], pb.t[:, :], [pb], [gg], eng='act')
                      for half in range(2):
                          pb = self.bank()
                          for cc in range(4):
                              c = half * 4 + cc
                              self.mm(pb.t[0:64, cc * 128:(cc + 1) * 128], Lt.t[0:32, c * 64:(c + 1) * 64], LW.t[0:32, hc],
                                      True, True, [LW, Lt], [pb])
                          self.tt(sgwT.t[:, half * 4:half * 4 + 4, :], pb.t[0:64, :].rearrange("p (c f) -> p c f", c=4),
                                  w0rep.t[0:64, hc].unsqueeze(1).to_broadcast([64, 4, 128]), ALU.add,
                                  [pb, w0rep], [(sgwT, half)])
                          self.act(sgwT.t[:, half * 4:half * 4 + 4, :], sgwT.t[:, half * 4:half * 4 + 4, :], AF.Sigmoid,
                                   [(sgwT, half)], [(sgwT, half)])
                      self.ck(0.5)
                      pinc, pexc = self.bank(), self.bank()
                      for c in range(8):
                          self.mm(pinc.t[:, c * 64:(c + 1) * 64], sgwT.t[:, c, :], cI.t[:, :], True, True, [sgwT, cI], [pinc])
                      for c in range(8):
                          self.mm(pexc.t[:, c * 64:(c + 1) * 64], sgwT.t[:, c, :], cS.t[:, :], True, True, [sgwT, cS], [pexc])
                      self.act(T["e1"].t[:, :], pinc.t[:, :], AF.Exp, [pinc], [T["e1"]])
                      self.act(T["e2"].t[:, :], pinc.t[:, :], AF.Exp, [pinc], [T["e2"]], scale=-1.0)
                      self.act(T["e3"].t[:, :], pexc.t[:, :], AF.Exp, [pexc], [T["e3"]])
                      for half in range(2):
                          pb = self.bank()
                          for cc in range(4):
                              c = half * 4 + cc
                              self.mm(pb.t[0:64, cc * 128:(cc + 1) * 128], cR.t[:, :], sgwT.t[:, c, :], True, True, [sgwT, cR], [pb])
                          self.act(eR.t[:, half * 4:half * 4 + 4, :], pb.t[0:64, :].rearrange("p (c f) -> p c f", c=4), AF.Exp,
                                   [pb], [(eR, half)])
                      self.cp(Pend.t[:, :], T["e1"].t[:, 63::64], [T["e1"]], [Pend])
                      self.ck(0.6)
                      self.ts(T["kkr"].t[:, :], xk.t[:, :], k_k.t[:, hp:hp + 1], None, ALU.mult, None, [xk, k_k], [T["kkr"]])
                      self.act(sqb.t[:, :], T["kkr"].t[:, :], AF.Square, [T["kkr"]], [sqb])
                      pb = self.bank()
                      self.mm(pb.t[:, :], bones.t[:, :], sqb.t[:, :], True, True, [bones, sqb], [pb])
                      self.act(T["tmp"].t[:, :], pb.t[:, :], AF.Sqrt, [pb], [T["tmp"]])
                      self.ts(T["tmp"].t[:, :], T["tmp"].t[:, :], 1e-12, None, ALU.max, None, [T["tmp"]], [T["tmp"]])
                      fw.op('dve', lambda e: e.reciprocal(T["tmp"].t[:, :], T["tmp"].t[:, :]), reads=[T["tmp"]], writes=[T["tmp"]])
                      self.tt(T["kk"].t[:, :], T["kkr"].t[:, :], T["tmp"].t[:, :], ALU.mult, [T["kkr"], T["tmp"]], [T["kk"]])
                      self.ts(T["tmp"].t[:, :], T["a"].t[:, :], k_a.t[:, hp:hp + 1], omka.t[:, hp:hp + 1], ALU.mult, ALU.add,
                              [T["a"], k_a, omka], [T["tmp"]])
                      self.tt(T["kf"].t[:, :], xk.t[:, :], T["tmp"].t[:, :], ALU.mult, [xk, T["tmp"]], [T["kf"]])
                      self.tt(T["bb"].t[:, :], T["kk"].t[:, :], T["a"].t[:, :], ALU.mult, [T["kk"], T["a"]], [T["bb"]])
                      self.stt(ATt.t[:, :], T["kk"].t[:, :], -1.0, T["e3"].t[:, :], ALU.mult, ALU.mult, [T["kk"], T["e3"]], [ATt])
                      self.tt(RTt.t[:, :], xr.t[:, :], T["e1"].t[:, :], ALU.mult, [xr, T["e1"]], [RTt])
                      self.tt(KTt.t[:, :], T["kf"].t[:, :], T["e2"].t[:, :], ALU.mult, [T["kf"], T["e2"]], [KTt])
                      self.tt(BTt.t[:, :], T["bb"].t[:, :], T["e2"].t[:, :], ALU.mult, [T["bb"], T["e2"]], [BTt])
                      self.stt(t3b.t[:, :], xr.t[:, :], r_k.t[:, hp:hp + 1], T["kf"].t[:, :], ALU.mult, ALU.mult,
                               [xr, r_k, T["kf"]], [t3b])
                      pb = self.bank()
                      self.mm(pb.t[:, :], bones.t[:, :], t3b.t[:, :], True, True, [bones, t3b], [pb])
                      self.tt(bonus.t[:, :], pb.t[:, :], xv.t[:, :], ALU.mult, [pb, xv], [bonus])
                      self.ck(0.7)
                      for half in range(2):
                          for (srcT, dstT, mode) in ((T["kf"], Ktok, 1), (T["bb"], Btok, 1), (xv, Vtok, 0)):
                              pb = self.bank()
                              for cc in range(4):
                                  c = half * 4 + cc
                                  self.tr(pb.t[0:64, cc * 128:(cc + 1) * 128], srcT.t[:, c * 64:(c + 1) * 64], identf.t[:, :],
                                          [srcT, identf], [pb])
                              pv = pb.t[0:64, :].rearrange("p (c f) -> p c f", c=4)
                              if mode:
                                  self.tt(dstT.t[:, half * 4:half * 4 + 4, :], pv, eR.t[:, half * 4:half * 4 + 4, :], ALU.mult,
                                          [pb, (eR, half)], [(dstT, half)])
                              else:
                                  self.cp(dstT.t[:, half * 4:half * 4 + 4, :], pv, [pb], [(dstT, half)], eng='act')

                      self.ck(0.8)
                      def chunk_prep(c, res):
                          cc = slice(c * 64, (c + 1) * 64)
                          A = Ach[c % 2]
                          Xc = Xi[c % 2]
                          for h in range(2):
                              hr = slice(h * 64, (h + 1) * 64)
                              pb = self.bank()
                              ops = ((BTt, ATt), (ATt, BTt), (KTt, ATt), (BTt, RTt), (KTt, RTt))
                              for i, (l_, r_) in enumerate(ops):
                                  self.mm(pb.t[0:64, i * 64:(i + 1) * 64], l_.t[hr, cc], r_.t[hr, cc], True, True, [l_, r_], [pb])
                              self.tt(A[h].t[:, :], pb.t[0:64, 0:320], mk.t[:, :], ALU.mult, [pb, mk], [A[h]])
                              self.cp(Xc[0].t[:, h, 0:128], A[h].t[:, 0:128], [A[h]], [(Xc[0], (h, 'n'))], eng='act')
                              self.tt(Xc[0].t[:, h, 128:192], A[h].t[:, 0:64], identb.t[:, :], ALU.add, [A[h], identb], [(Xc[0], (h, 'm'))])
                          yield
                          cur = 0
                          for ph in range(6):
                              pb = self.bank()
                              src, dstx = Xc[cur], Xc[1 - cur]
                              for h in range(2):
                                  N_, NT_, M_ = src.t[:, h, 0:64], src.t[:, h, 64:128], src.t[:, h, 128:192]
                                  o = h * 192
                                  if ph < 5:
                                      self.mm(pb.t[0:64, o:o + 64], NT_, N_, True, True, [src], [pb])
                                      self.mm(pb.t[0:64, o + 64:o + 128], N_, NT_, True, True, [src], [pb])
                                  if ph >= 1:
                                      self.mm(pb.t[0:64, o + 128:o + 192], NT_, M_, True, True, [src], [pb])
                              pv = pb.t[0:64, 0:384].rearrange("p (h x) -> p h x", h=2)
                              if ph < 5:
                                  self.cp(dstx.t[:, :, 0:128], pv[:, :, 0:128], [pb], [(dstx, (0, 'n')), (dstx, (1, 'n'))], eng='act')
                              if ph >= 1:
                                  self.tt(dstx.t[:, :, 128:192], pv[:, :, 128:192], src.t[:, :, 128:192], ALU.add, [pb, src], [(dstx, (0, 'm')), (dstx, (1, 'm'))])
                              else:
                                  self.cp(dstx.t[:, :, 128:192], src.t[:, :, 128:192], [src], [(dstx, (0, 'm')), (dstx, (1, 'm'))])
                              cur = 1 - cur
                              yield
                          res[c] = (A, Xc[cur])

                      def chunk_chain(c, A, Xf):
                          cc = slice(c * 64, (c + 1) * 64)
                          zb, ub, yb, sbk = self.bank(), self.bank(), self.bank(), self.bank()
                          hold = [zb, ub, yb, sbk]
                          self.held.update(hold)
                          self.mm(zb.t[0:64, 0:128], ATt.t[:, cc], STb.t[:, hp, :], True, False, [ATt, (STb, hp)], [zb])
                          for h in range(2):
                              hr = slice(h * 64, (h + 1) * 64)
                              self.mm(zb.t[0:64, hr], A[h].t[:, 128:192], Vtok.t[:, c, hr], False, h == 1, [A[h], Vtok], [zb])
                          self.cp(Zb.t[:, :], zb.t[0:64, 0:128], [zb], [Zb], eng='act')
                          yield
                          for h in range(2):
                              hr = slice(h * 64, (h + 1) * 64)
                              self.mm(ub.t[0:64, hr], Xf.t[:, h, 128:192], Zb.t[:, hr], True, True, [Xf, Zb], [ub])
                          self.cp(Ub.t[:, :], ub.t[0:64, 0:128], [ub], [Ub])
                          yield
                          self.mm(sbk.t[:, 0:128], Btok.t[:, c, :], Ub.t[:, :], True, False, [Btok, Ub], [sbk])
                          self.mm(sbk.t[:, 0:128], Ktok.t[:, c, :], Vtok.t[:, c, :], False, True, [Ktok, Vtok], [sbk])
                          for h in range(2):
                              hr = slice(h * 64, (h + 1) * 64)
                              self.stt(ST.t[hr, hp, :], ST.t[hr, hp, :], Pend.t[hr, c:c + 1], sbk.t[hr, hr], ALU.mult, ALU.add,
                                       [(ST, hp), Pend, sbk], [(ST, hp)])
                          yield
                          self.mm(yb.t[0:64, 0:128], RTt.t[:, cc], STb.t[:, hp, :], True, False, [RTt, (STb, hp)], [yb])
                          for h in range(2):
                              hr = slice(h * 64, (h + 1) * 64)
                              self.mm(yb.t[0:64, hr], A[h].t[:, 256:320], Vtok.t[:, c, hr], False, False, [A[h], Vtok], [yb])
                              self.mm(yb.t[0:64, hr], A[h].t[:, 192:256], Ub.t[:, hr], False, h == 1, [A[h], Ub], [yb])
                          self.cp(Ytok.t[:, c, :], yb.t[0:64, 0:128], [yb], [(Ytok, c)], eng='act')
                          for h in range(2):
                              hr = slice(h * 64, (h + 1) * 64)
                              self.cp(STb.t[hr, hp, hr], ST.t[hr, hp, :], [(ST, hp)], [(STb, hp)], eng='act')
                          self.held.difference_update(hold)
                          yield

                      preps = {}
                      for _ in chunk_prep(0, preps):
                          pass
                      for c in range(8):
                          A, Xf = preps[c]
                          g1 = chunk_prep(c + 1, preps) if c + 1 < 8 else iter(())
                          g2 = chunk_chain(c, A, Xf)
                          while True:
                              d2 = next(g2, 0) == 0
                              d1 = next(g1, 0) == 0
                              if d1 and d2:
                                  break

                      self.ck(0.9)
                      yv = Ytok.t[:, :, :].rearrange("p c (h v) -> p (c h) v", h=2)
                      fw.op('dve', lambda e: e.reduce_sum(gs1.t[:, :], yv, AX.X), reads=[Ytok], writes=[gs1])
                      ysq = self.xn
                      ysq3 = ysq.t[0:64, :].rearrange("p (c f) -> p c f", c=8)
                      self.act(ysq3, Ytok.t[:, :, :], AF.Square, [Ytok], [ysq])
                      ysv = ysq.t[0:64, :].rearrange("p (c h v) -> p (c h) v", c=8, h=2)
                      fw.op('dve', lambda e: e.reduce_sum(gs2.t[:, :], ysv, AX.X), reads=[ysq], writes=[gs2])
                      self.ts(gs1.t[:, :], gs1.t[:, :], 1.0 / 64, None, ALU.mult, None, [gs1], [gs1])
                      self.tt(ysq.t[0:64, 0:16], gs1.t[:, :], gs1.t[:, :], ALU.mult, [gs1], [ysq])
                      self.stt(gs2.t[:, :], gs2.t[:, :], 1.0 / 64, ysq.t[0:64, 0:16], ALU.mult, ALU.subtract, [gs2, ysq], [gs2])
                      self.ts(gs2.t[:, :], gs2.t[:, :], GN_EPS, None, ALU.add, None, [gs2], [gs2])
                      self.act(gs2.t[:, :], gs2.t[:, :], AF.Sqrt, [gs2], [gs2])
                      fw.op('dve', lambda e: e.reciprocal(gs2.t[:, :], gs2.t[:, :]), reads=[gs2], writes=[gs2])
                      mb = gs1.t[:, :].unsqueeze(2).to_broadcast([64, 16, 64])
                      rb = gs2.t[:, :].unsqueeze(2).to_broadcast([64, 16, 64])
                      self.tt(yv, yv, mb, ALU.subtract, [Ytok, gs1], [Ytok])
                      self.tt(yv, yv, rb, ALU.mult, [Ytok, gs2], [Ytok])
                      pb = self.bank()
                      for c in range(8):
                          self.tr(pb.t[:, c * 64:(c + 1) * 64], Ytok.t[:, c, :], identf.t[0:64, 0:64], [Ytok, identf], [pb])
                      self.act(T["tmp"].t[:, :], pb.t[:, :], AF.Identity, [pb, ln_w, ln_b], [T["tmp"]],
                               bias=ln_b.t[:, hp:hp + 1], scale=ln_w.t[:, hp:hp + 1])
                      self.tt(T["tmp"].t[:, :], T["tmp"].t[:, :], bonus.t[:, :], ALU.add, [T["tmp"], bonus], [T["tmp"]])
                      self.tt(rwT.t[:, hp, :], T["tmp"].t[:, :], gg.t[:, :], ALU.mult, [T["tmp"], gg], [(rwT, hp)])

                  if self.stages < 2:
                      continue
                  wq_ = self.wload(win_v[:, :, QOFF:QOFF + 512], 8, 512, reads=[win_b])
                  for cb in range(4):
                      pb = self.bank()
                      for k in range(8):
                          self.mm(pb.t[:, :], wq_.t[:, k, cb * 128:(cb + 1) * 128], hT.t[:, k, :], k == 0, k == 7, [wq_, hT], [pb])
                      self.act(QT.t[:, cb, :], pb.t[:, :], AF.Copy, [pb], [(QT, cb)], scale=0.125)
                  wk2 = self.wload(win_v[:, :, KOFF:KOFF + 512], 8, 512, reads=[win_b])
                  slot0 = (w * 4) % RING
                  for cb in range(4):
                      pb = self.bank()
                      for k in range(8):
                          self.mm(pb.t[:, :], wk2.t[:, k, cb * 128:(cb + 1) * 128], hT.t[:, k, :], k == 0, k == 7, [wk2, hT], [pb])
                      self.cp(KT.t[:, cb, slot0 * 128:slot0 * 128 + W], pb.t[:, :], [pb], [KT], eng='act' if cb % 2 else 'dve')
                  wv2 = self.wload(win_v[:, :, VOFF:VOFF + 512], 8, 512, reads=[win_b])
                  for (wt, isv) in ((wk2, 0), (wv2, 1)):
                      for blk in range(4):
                          tok0 = t0 + blk * 128
                          need_out = tok0 >= seq - keep
                          if not isv and not need_out:
                              continue
                          pb = self.bank()
                          for k in range(8):
                              self.mm(pb.t[:, :], hT.t[:, k, blk * 128:(blk + 1) * 128], wt.t[:, k, :], k == 0, k == 7, [wt, hT], [pb])
                          if isv:
                              slot = (w * 4 + blk) % RING
                              self.cp(V1.t[:, slot, :, 1:65], pb.t[:, :].rearrange("p (h v) -> p h v", h=8), [pb], [V1], eng='act')
                              fw.op('pool', lambda e, slot=slot: e.memset(V1.t[:, slot, :, 0:1], 1.0), writes=[V1])
                          if need_out:
                              stg = stV if isv else stK
                              self.cp(stg.t[:, :], pb.t[:, :], [pb], [stg])
                              dst = (vwp if isv else kwp)[tok0 - (seq - keep):tok0 - (seq - keep) + 128, :]
                              fw.dma('sp', dst, stg.t[:, :], reads=[stg], out_dram=True)

                  pend = []

                  def flush():
                      while pend:
                          pend.pop(0)()
                  gcount = 0
                  for h in range(8):
                      hp, hr = h // 2, slice((h % 2) * 64, (h % 2) * 64 + 64)
                      EMh = EM2[0]
                      flush()
                      fw.dma('sp', EMh.t[:, :, :], em_bf[h], reads=[(em_b, h)], writes=[EMh])
                      for bq in range(4):
                          gbq = w * 4 + bq
                          ob = self.bank()
                          self.held.add(ob)
                          for g0 in range(0, NKB, 4):
                              n = min(4, NKB - g0)
                              sb_ = self.bank()
                              self.held.add(sb_)
                              for jj in range(n):
                                  slot = (gbq - (g0 + jj)) % RING
                                  self.mm(sb_.t[:, jj * 128:(jj + 1) * 128], KT.t[hr, hp, slot * 128:(slot + 1) * 128],
                                          QT.t[hr, hp, bq * 128:(bq + 1) * 128], True, True, [KT, (QT, hp)], [sb_])
                              Eb_, Pm_a = EbL[gcount % 2], PmL[gcount % 2]
                              gcount += 1

                              def rest(sb_=sb_, n=n, g0=g0, ob=ob, bq=bq, h=h, gbq=gbq, Eb_=Eb_, Pm_a=Pm_a, EMh=EMh):
                                  self.act(Eb_.t[:, 0:n * 128], sb_.t[:, 0:n * 128], AF.Exp, [sb_], [Eb_])
                                  self.held.discard(sb_)
                                  self.tt(Pm_a.t[:, 0:n * 128], Eb_.t[:, 0:n * 128],
                                          EMh.t[:, g0:g0 + n, :].rearrange("p j q -> p (j q)"), ALU.mult, [Eb_, EMh], [Pm_a])
                                  for jj in range(n):
                                      j = g0 + jj
                                      slot = (gbq - j) % RING
                                      self.mm(ob.t[:, 0:65], Pm_a.t[:, jj * 128:(jj + 1) * 128], V1.t[:, slot, h, :],
                                              j == 0, j == NKB - 1, [Pm_a, V1], [ob])
                                  if g0 + n >= NKB:
                                      self.held.discard(ob)
                                      fw.op('dve', lambda e, ob=ob: e.reciprocal(orec.t[:, :], ob.t[:, 0:1]), reads=[ob], writes=[orec])
                                      self.ts(Otok[bq].t[:, h * 64:(h + 1) * 64], ob.t[:, 1:65], orec.t[:, 0:1], None, ALU.mult, None,
                                              [ob, orec], [(Otok[bq], h)])
                              flush()
                              pend.append(rest)
                  flush()
                  for bq in range(4):
                      Ot = Otok[bq]
                      ss = self.ss
                      self.act(self.junk.t[:, 0, :], Ot.t[:, :], AF.Square, [Ot], [self.junk, ss], accum=ss.t[:, :])
                      self.ts(ss.t[:, :], ss.t[:, :], 1.0 / RW, RMS_EPS, ALU.mult, ALU.add, [ss], [ss])
                      self.act(ss.t[:, :], ss.t[:, :], AF.Sqrt, [ss], [ss])
                      fw.op('dve', lambda e: e.reciprocal(self.ss.t[:, :], self.ss.t[:, :]), reads=[ss], writes=[ss])
                      self.stt(Ot.t[:, :], Ot.t[:, :], ss.t[:, 0:1], again.t[:, :], ALU.mult, ALU.mult, [Ot, ss, again], [Ot])
                      pb = self.bank()
                      for cb in range(4):
                          self.tr(pb.t[:, cb * 128:(cb + 1) * 128], Ot.t[:, cb * 128:(cb + 1) * 128], identf.t[:, :], [Ot, identf], [pb])
                      self.cp(atT.t[:, :, bq * 128:(bq + 1) * 128], pb.t[:, :].rearrange("p (c t) -> p c t", c=4), [pb], [(atT, bq)], eng='act')

                  if self.stages < 4:
                      continue
                  tail(4, pp[t0:t0 + W, :], yp[t0:t0 + W, :])
              except StopBuild:
                break

            fw.dma('sp', shp_o.rearrange("(k p) -> p k", p=128), carry.t[:, :], reads=[carry], out_dram=True, nc_ok=True)
            for hp in range(4):
                pb = self.bank()
                self.tr(pb.t[0:64, 0:128], ST.t[:, hp, :], identf.t[:, :], [ST, identf], [pb])
                self.cp(stK.t[0:64, 0:128], pb.t[0:64, 0:128], [pb], [stK])
                for h2 in range(2):
                    fw.dma('sp', wkvp[hp * 2 + h2], stK.t[0:64, h2 * 64:(h2 + 1) * 64], reads=[stK], out_dram=True)

            pstk.close()
            fw.barrier()
            if 'sample' in SKIP:
                fw.stk = st
                fw.finish()
                return nc
            sstk = ExitStack()
            fw.stk = sstk
            T = P_["T"]
            Xs = X[0]
            Ptok = sb("Ptok", [128, IN_COLS], F32)
            XM = sb("XM", [128, RW_COLS], F32)
            ATtok = sb("ATtok", [128, RW], F32)
            Gtok = sb("Gtok", [128, RW], F32)
            Btk = sb("Btk", [128, RW], F32)
            n8 = sb("n8", [128, 8], F32)
            m8 = sb("m8", [128, 8], F32)
            v8 = sb("v8", [128, 8], F32)
            QTs = sb("QTs", [128, 4, 128], BF16)
            KTn = sb("KTn", [128, 4, 128], BF16)
            Vn1 = sb("Vn1", [128, 8, 65], BF16)
            scrA_b = Buf(None, "scrA")
            scrB_b = Buf(None, "scrB")

            fw.dma('sp', Xs.t[:, :], xs, writes=[Xs])
            self.norm_T([Xs], g_mix, hT)
            for ci, c0 in enumerate(range(0, IN_COLS, 512)):
                cw = min(512, IN_COLS - c0)
                wl = self.wload(win_v[:, :, c0:c0 + cw], 8, cw, reads=[win_b])
                pb = self.bank()
                for k in range(8):
                    self.mm(pb.t[:, 0:cw], hT.t[:, k, 0:128], wl.t[:, k, 0:cw], k == 0, k == 7, [wl, hT], [pb])
                self.cp(Ptok.t[:, c0:c0 + cw], pb.t[:, 0:cw], [pb], [(Ptok, ci)], eng='act' if ci % 2 else 'dve')
            fw.dma('sp', shs, Ptok.t[7:128:8, 0:RW_COLS], reads=[Ptok], out_dram=True)
            for (off_, dst_) in ((KOFF, kws), (VOFF, vws)):
                def mknew(e, off_=off_, dst_=dst_):
                    return [e.dma_start(out=dst_[j, MAXWIN - 8:MAXWIN, :], in_=Ptok.t[8 * j:8 * j + 8, off_:off_ + 512])
                            for j in range(NSEQ)]
                fw.dma('sp', None, None, fn=mknew, n=NSEQ, reads=[Ptok], out_dram=True)
            for cb in range(4):
                pb = self.bank()
                self.tr(pb.t[:, 0:128], Ptok.t[:, QOFF + cb * 128:QOFF + (cb + 1) * 128], identf.t[:, :], [Ptok, identf], [pb])
                self.act(QTs.t[:, cb, :], pb.t[:, 0:128], AF.Copy, [pb], [(QTs, cb)], scale=0.125)
                pb = self.bank()
                self.tr(pb.t[:, 0:128], Ptok.t[:, KOFF + cb * 128:KOFF + (cb + 1) * 128], identf.t[:, :], [Ptok, identf], [pb])
                self.cp(KTn.t[:, cb, :], pb.t[:, 0:128], [pb], [(KTn, cb)])
            fw.op('pool', lambda e: e.memset(Vn1.t[:, :, 0:1], 1.0), writes=[Vn1])
            self.cp(Vn1.t[:, :, 1:65], Ptok.t[:, VOFF:VOFF + 512].rearrange("p (h v) -> p h v", h=8), [Ptok, Vn1], [Vn1])

            rstk = ExitStack()
            fw.stk = rstk
            S = sb("S", [128, 4096], F32)
            TM = [sb("TM%d" % i, [128, 4096], F32) for i in range(2)]
            RLh = sb("RLh", [128, 6, 8, 64], F32)
            Yrl = sb("Yrl", [128, 8, 64], F32)
            skk = sb("skk", [128, 64], F32)
            reps = {}
            for n_ in ("a0", "k_k", "k_a", "r_k", "ln_x_w", "ln_x_b"):
                reps[n_] = sb("rep_" + n_, [128, RW], F32)
                fw.dma('sp', reps[n_].t[:, :], dr[n_].partition_broadcast(128), writes=[reps[n_]])
            fw.dma('sp', S.t[:, :], swkv, writes=[S])
            mu_rep = TM[0]
            fw.dma('sp', mu_rep.t[:, 0:RW_COLS], dr["mu"].partition_broadcast(128), writes=[mu_rep])
            fw.dma('sp', XM.t[1:128, :], Ptok.t[0:127, 0:RW_COLS], reads=[Ptok], writes=[XM])
            fw.dma('sp', XM.t[0:128:8, :], sshift, writes=[XM])
            Pr = Ptok.t[:, 0:RW_COLS]
            self.tt(XM.t[:, :], XM.t[:, :], Pr, ALU.subtract, [XM, Ptok], [XM])
            self.tt(XM.t[:, :], XM.t[:, :], mu_rep.t[:, 0:RW_COLS], ALU.mult, [XM, mu_rep], [XM])
            self.tt(XM.t[:, :], XM.t[:, :], Pr, ALU.add, [XM, Ptok], [XM])
            xr, xk, xv = XM.t[:, 0:512], XM.t[:, 512:1024], XM.t[:, 1024:1536]
            RLb = [X[1], X[1], X[2], X[2], X[3], X[3]]
            RL = [RLb[j].t[:, (j % 2) * 512:(j % 2) * 512 + 512] for j in range(6)]
            h3 = lambda ap: ap.rearrange("p (h n) -> p h n", h=8)
            bc3 = lambda b8: b8.t[:, :].unsqueeze(2).to_broadcast([128, 8, 64])
            pb = self.bank()
            self.tr(pb.t[:, 0:128], XM.t[:, 1536:1664], identf.t[:, :], [XM, identf], [pb])
            self.act(Lt.t[0:32, 0:128], pb.t[0:32, 0:128], AF.Tanh, [pb], [(Lt, 0)])
            self.cp(Lt.t[32:64, 0:128], pb.t[32:64, 0:128], [pb], [(Lt, 1)])
            self.act(Lt.t[64:128, 0:128], pb.t[64:128, 0:128], AF.Sigmoid, [pb], [(Lt, 2)])
            tA, tB, Atok, kkr = T["e1"], T["e2"], T["a"], T["kkr"]
            pb = self.bank()
            self.mm(pb.t[:, :], Lt.t[0:32, 0:128], LW.t[0:32, :], True, True, [Lt, LW], [pb])
            self.tt(tA.t[:, :], pb.t[:, :], w0rep.t[:, :], ALU.add, [pb, w0rep], [tA])
            self.act(tA.t[:, :], tA.t[:, :], AF.Sigmoid, [tA], [tA])
            self.act(RL[1], tA.t[:, :], AF.Exp, [tA], [X[1]], scale=-DECAY_C)
            pb = self.bank()
            self.mm(pb.t[:, :], Lt.t[32:64, 0:128], LW.t[32:64, :], True, True, [Lt, LW], [pb])
            self.tt(Atok.t[:, :], pb.t[:, :], reps["a0"].t[:, :], ALU.add, [pb, reps["a0"]], [Atok])
            self.act(Atok.t[:, :], Atok.t[:, :], AF.Sigmoid, [Atok], [Atok])
            pb = self.bank()
            self.mm(pb.t[:, :], Lt.t[64:128, 0:128], LW.t[64:128, :], True, True, [Lt, LW], [pb])
            self.cp(Gtok.t[:, :], pb.t[:, :], [pb], [Gtok], eng='act')
            self.tt(kkr.t[:, :], xk, reps["k_k"].t[:, :], ALU.mult, [XM, reps["k_k"]], [kkr])
            self.act(tB.t[:, :], kkr.t[:, :], AF.Square, [kkr], [tB])
            fw.op('dve', lambda e: e.reduce_sum(n8.t[:, :], h3(tB.t[:, :]), AX.X), reads=[tB], writes=[n8])
            self.act(n8.t[:, :], n8.t[:, :], AF.Sqrt, [n8], [n8])
            self.ts(n8.t[:, :], n8.t[:, :], 1e-12, None, ALU.max, None, [n8], [n8])
            fw.op('dve', lambda e: e.reciprocal(n8.t[:, :], n8.t[:, :]), reads=[n8], writes=[n8])
            self.tt(h3(RL[4]), h3(kkr.t[:, :]), bc3(n8), ALU.mult, [kkr, n8], [X[3]])
            self.ts(tA.t[:, :], Atok.t[:, :], -1.0, None, ALU.add, None, [Atok], [tA])
            self.tt(tA.t[:, :], tA.t[:, :], reps["k_a"].t[:, :], ALU.mult, [tA, reps["k_a"]], [tA])
            self.ts(tA.t[:, :], tA.t[:, :], 1.0, None, ALU.add, None, [tA], [tA])
            self.tt(RL[2], xk, tA.t[:, :], ALU.mult, [XM, tA], [X[2]])
            self.tt(RL[5], RL[4], Atok.t[:, :], ALU.mult, [X[3], Atok], [X[3]])
            self.cp(RL[0], xr, [XM], [X[1]])
            self.cp(RL[3], xv, [XM], [X[2]], eng='act')
            self.tt(tA.t[:, :], xr, RL[2], ALU.mult, [XM, X[2]], [tA])
            self.tt(tA.t[:, :], tA.t[:, :], reps["r_k"].t[:, :], ALU.mult, [tA, reps["r_k"]], [tA])
            fw.op('dve', lambda e: e.reduce_sum(m8.t[:, :], h3(tA.t[:, :]), AX.X), reads=[tA], writes=[m8])
            self.tt(h3(Btk.t[:, :]), h3(xv), bc3(m8), ALU.mult, [XM, m8], [Btk])
            for i3 in range(3):
                fw.dma('sp', scrA[:, i3 * 1024:(i3 + 1) * 1024], X[1 + i3].t[:, :], reads=[X[1 + i3]], writes=[(scrA_b, i3)])
            scrA_v = scrA.rearrange("p (j h n) -> p j h n", j=6, h=8)
            for b in range(NSEQ):
                def mkrl(e, b=b):
                    return [e.dma_start(out=RLh.t[b * 8:(b + 1) * 8, j, :, :],
                                        in_=scrA_v[b * 8:(b + 1) * 8, j, :, :].rearrange("t h n -> h t n"),
                                        allow_slow_non_contiguous=True) for j in range(6)]
                fw.dma('sp' if b % 2 else 'act', None, None, fn=mkrl, n=6, reads=[scrA_b], writes=[(RLh, b)])
            S3 = S.t[:, :].rearrange("p (v k) -> p v k", v=64)
            TM3 = [t_.t[:, :].rearrange("p (v k) -> p v k", v=64) for t_ in TM]
            kbc = lambda j, t: RLh.t[:, j, t, :].unsqueeze(1).to_broadcast([128, 64, 64])
            vbc = lambda ap: ap.unsqueeze(2).to_broadcast([128, 64, 64])
            for t in range(8 if 'rec' not in SKIP else 0):
                self.tt(TM3[0], S3, kbc(4, t), ALU.mult, [S, RLh], [TM[0]])
                fw.op('dve', lambda e: e.reduce_sum(skk.t[:, :], TM3[0], AX.X), reads=[TM[0]], writes=[skk])
                self.tt(S3, S3, kbc(1, t), ALU.mult, [S, RLh], [S])
                self.tt(TM3[1], vbc(skk.t[:, :]), kbc(5, t), ALU.mult, [skk, RLh], [TM[1]], eng='pool')
                self.tt(S3, S3, TM3[1], ALU.subtract, [S, TM[1]], [S])
                self.tt(TM3[0], vbc(RLh.t[:, 3, t, :]), kbc(2, t), ALU.mult, [RLh], [TM[0]], eng='pool')
                self.tt(S3, S3, TM3[0], ALU.add, [S, TM[0]], [S])
                self.tt(TM3[1], S3, kbc(0, t), ALU.mult, [S, RLh], [TM[1]])
                fw.op('dve', lambda e, t=t: e.reduce_sum(Yrl.t[:, t, :], TM3[1], AX.X), reads=[TM[1]], writes=[(Yrl, t)])
            fw.dma('sp', wkvs, S.t[:, :], reads=[S], out_dram=True)
            fw.dma('sp', scrB, Yrl.t[:, :, :].rearrange("p t v -> p (t v)"), reads=[Yrl], writes=[scrB_b])
            Ytk = T["kk"]
            scrB_v = scrB.rearrange("p (t v) -> p t v", t=8)
            for b in range(NSEQ):
                fw.dma('sp' if b % 2 else 'act', Ytk.t[b * 8:(b + 1) * 8, :].rearrange("t (h v) -> t h v", h=8),
                       scrB_v[b * 8:(b + 1) * 8, :, :].rearrange("h t v -> t h v"),
                       reads=[scrB_b], writes=[(Ytk, b)], nc_ok=True)
            yvs = h3(Ytk.t[:, :])
            fw.op('dve', lambda e: e.reduce_sum(m8.t[:, :], yvs, AX.X), reads=[Ytk], writes=[m8])
            self.act(tB.t[:, :], Ytk.t[:, :], AF.Square, [Ytk], [tB])
            fw.op('dve', lambda e: e.reduce_sum(v8.t[:, :], h3(tB.t[:, :]), AX.X), reads=[tB], writes=[v8])
            self.ts(m8.t[:, :], m8.t[:, :], 1.0 / 64, None, ALU.mult, None, [m8], [m8])
            self.tt(n8.t[:, :], m8.t[:, :], m8.t[:, :], ALU.mult, [m8], [n8])
            self.stt(v8.t[:, :], v8.t[:, :], 1.0 / 64, n8.t[:, :], ALU.mult, ALU.subtract, [v8, n8], [v8])
            self.ts(v8.t[:, :], v8.t[:, :], GN_EPS, None, ALU.add, None, [v8], [v8])
            self.act(v8.t[:, :], v8.t[:, :], AF.Sqrt, [v8], [v8])
            fw.op('dve', lambda e: e.reciprocal(v8.t[:, :], v8.t[:, :]), reads=[v8], writes=[v8])
            self.tt(yvs, yvs, bc3(m8), ALU.subtract, [Ytk, m8], [Ytk])
            self.tt(yvs, yvs, bc3(v8), ALU.mult, [Ytk, v8], [Ytk])
            self.tt(Ytk.t[:, :], Ytk.t[:, :], reps["ln_x_w"].t[:, :], ALU.mult, [Ytk, reps["ln_x_w"]], [Ytk])
            self.tt(Ytk.t[:, :], Ytk.t[:, :], reps["ln_x_b"].t[:, :], ALU.add, [Ytk, reps["ln_x_b"]], [Ytk])
            self.tt(Ytk.t[:, :], Ytk.t[:, :], Btk.t[:, :], ALU.add, [Ytk, Btk], [Ytk])
            self.tt(Ytk.t[:, :], Ytk.t[:, :], Gtok.t[:, :], ALU.mult, [Ytk, Gtok], [Ytk])
            pb = self.bank()
            for cb in range(4):
                self.tr(pb.t[:, cb * 128:(cb + 1) * 128], Ytk.t[:, cb * 128:(cb + 1) * 128], identf.t[:, :], [Ytk, identf], [pb])
            self.cp(rwT.t[:, :, 0:128], pb.t[:, :].rearrange("p (c t) -> p c t", c=4), [pb], [rwT], eng='act')
            rstk.close()
            fw.barrier()

            astk = ExitStack()
            fw.stk = astk
            EMs = sb("EMs", [128, 1024], BF16)
            EMn = sb("EMn", [128, 1024], BF16)
            fw.dma('pool', EMs.t[:, :], dr["ems"], writes=[EMs])
            fw.dma('pool', EMn.t[:, :], dr["emn"], writes=[EMn])
            Kld = [sb("Kld%d" % i, [128, 4, 512], F32) for i in range(2)]
            Vld = [sb("Vld%d" % i, [128, 4, 512], F32) for i in range(2)]
            KTg = [sb("KTg%d" % i, [128, 4, 512], BF16) for i in range(2)]
            V1s = [sb("V1s%d" % i, [128, 8, 8, 65], BF16) for i in range(2)]
            for v1 in V1s:
                fw.op('pool', lambda e, v1=v1: e.memset(v1.t[:, :, :, 0:1], 1.0), writes=[v1])
            Ebs = sb("Ebs", [128, 512], BF16)
            Pms = [sb("Pms%d" % i, [128, 512], BF16) for i in range(2)]
            Pmn = sb("Pmn", [128, 64], BF16)
            ostg = [sb("ostg%d" % i, [8, 512], F32) for i in range(2)]
            orec8 = sb("orec8", [8, 8], F32)
            Qbd = sb("Qbd", [128, NSEQ, 4, 64], BF16)
            fw.op('pool', lambda e: e.memset(Qbd.t[:, :, :, :], 0.0), writes=[Qbd])
            for hp in range(4):
                for h2 in range(2):
                    hr = slice(h2 * 64, (h2 + 1) * 64)
                    c0 = (2 * hp + h2) * 8
                    self.cp(Qbd.t[hr, :, hp, c0:c0 + 8], QTs.t[hr, hp, :].rearrange("p (b t) -> p b t", b=NSEQ), [QTs, Qbd], [Qbd],
                            eng='act' if h2 else 'dve')
            zl = sb("zl", [128, 8], BF16)
            fw.op('pool', lambda e: e.memset(zl.t[:, :], 0.0), writes=[zl])
            gi = 0
            for b in range(NSEQ if 'attn' not in SKIP else 0):
                O2 = [self.bank(), self.bank()]
                self.held.update(O2)
                qs = slice(b * 8, (b + 1) * 8)
                for O in (O2 if 'a_pv' not in SKIP else []):
                    self.mm(O.t[0:8, 0:260], zl.t[:, :], EMs.t[:, 0:260], True, False, [zl, EMs], [O])
                for half in range(2):
                    sc = self.bank()
                    self.held.add(sc)
                    V1h = V1s[half]
                    for g2 in range(2):
                        g = half * 2 + g2
                        Kt, Vt, KTt = Kld[gi % 2], Vld[gi % 2], KTg[gi % 2]
                        gi += 1
                        fw.dma('sp', Kt.t[:, :, :], kc[b, g * 512:(g + 1) * 512, :].rearrange("(j p) c -> p j c", p=128), writes=[Kt])
                        fw.dma('sp', Vt.t[:, :, :], vc[b, g * 512:(g + 1) * 512, :].rearrange("(j p) c -> p j c", p=128), writes=[Vt])
                        for hp in range(4):
                            pb = self.bank()
                            for j in range(4):
                                self.tr(pb.t[:, j * 128:(j + 1) * 128], Kt.t[:, j, hp * 128:(hp + 1) * 128], identf.t[:, :], [Kt, identf], [pb])
                            self.cp(KTt.t[:, hp, :], pb.t[:, :], [pb], [(KTt, hp)], eng='act' if hp % 2 else 'dve')
                        if 'a_vc' not in SKIP:
                            self.cp(V1h.t[:, g2 * 4:(g2 + 1) * 4, :, 1:65], Vt.t[:, :, :].rearrange("p j (h v) -> p j h v", h=8),
                                    [Vt], [(V1h, g2)], eng=VC_ENG)
                        for j in range(4 if 'a_sc' not in SKIP else 0):
                            jj = g2 * 4 + j
                            for hp in range(4):
                                self.mm(sc.t[:, jj * 64:(jj + 1) * 64], KTt.t[:, hp, j * 128:(j + 1) * 128], Qbd.t[:, b, hp, :],
                                        hp == 0, hp == 3, [(KTt, hp), Qbd], [sc])
                    self.held.discard(sc)
                    if 'a_sc' in SKIP:
                        continue
                    self.act(Ebs.t[:, :], sc.t[:, :], AF.Exp, [sc], [Ebs])
                    Pm_ = Pms[half]
                    self.tt(Pm_.t[:, :], Ebs.t[:, :], EMs.t[:, half * 512:(half + 1) * 512], ALU.mult, [Ebs, EMs], [Pm_])
                    for jj in range(8 if 'a_pv' not in SKIP else 0):
                        for h in range(8):
                            O = O2[h // 4]
                            c0 = (jj * 8 + h) * 8
                            self.mm(O.t[0:8, (h % 4) * 65:(h % 4) * 65 + 65], Pm_.t[:, c0:c0 + 8], V1h.t[:, jj, h, :],
                                    False, False, [Pm_, V1h], [O])
                self.held.difference_update(O2)
                if 'a_pv' in SKIP:
                    continue
                self.held.update(O2)
                sc = self.bank()
                for hp in range(4):
                    self.mm(sc.t[:, 0:64], KTn.t[:, hp, :], Qbd.t[:, b, hp, :], hp == 0, hp == 3, [KTn, Qbd], [sc])
                self.act(Ebs.t[:, 0:64], sc.t[:, 0:64], AF.Exp, [sc], [Ebs])
                self.tt(Pmn.t[:, :], Ebs.t[:, 0:64], EMn.t[:, b * 64:(b + 1) * 64], ALU.mult, [Ebs, EMn], [Pmn])
                for h in range(8):
                    O = O2[h // 4]
                    self.mm(O.t[0:8, (h % 4) * 65:(h % 4) * 65 + 65], Pmn.t[:, h * 8:h * 8 + 8], Vn1.t[:, h, :], False, h % 4 == 3, [Pmn, Vn1], [O])
                self.held.difference_update(O2)
                og = ostg[b % 2]
                for oi, O in enumerate(O2):
                    ov = O.t[0:8, 0:260].rearrange("p (h c) -> p h c", h=4)
                    fw.op('dve', lambda e, ov=ov, oi=oi: e.reciprocal(orec8.t[:, oi * 4:(oi + 1) * 4].unsqueeze(2), ov[:, :, 0:1]),
                          reads=[O], writes=[(orec8, oi)])
                    self.tt(og.t[:, oi * 256:(oi + 1) * 256].rearrange("p (h v) -> p h v", h=4), ov[:, :, 1:65],
                            orec8.t[:, oi * 4:(oi + 1) * 4].unsqueeze(2).to_broadcast([8, 4, 64]), ALU.mult,
                            [O, (orec8, oi)], [(og, oi)])
                fw.dma('sp', ATtok.t[b * 8:(b + 1) * 8, :], og.t[:, :], reads=[og], writes=[(ATtok, b)])
            ss = self.ss
            self.act(self.junk.t[:, 0, :], ATtok.t[:, :], AF.Square, [ATtok], [self.junk, ss], accum=ss.t[:, :])
            self.ts(ss.t[:, :], ss.t[:, :], 1.0 / RW, RMS_EPS, ALU.mult, ALU.add, [ss], [ss])
            self.act(ss.t[:, :], ss.t[:, :], AF.Sqrt, [ss], [ss])
            fw.op('dve', lambda e: e.reciprocal(self.ss.t[:, :], self.ss.t[:, :]), reads=[ss], writes=[ss])
            self.stt(ATtok.t[:, :], ATtok.t[:, :], ss.t[:, 0:1], again.t[:, :], ALU.mult, ALU.mult, [ATtok, ss, again], [ATtok])
            pb = self.bank()
            for cb in range(4):
                self.tr(pb.t[:, cb * 128:(cb + 1) * 128], ATtok.t[:, cb * 128:(cb + 1) * 128], identf.t[:, :], [ATtok, identf], [pb])
            self.cp(atT.t[:, :, 0:128], pb.t[:, :].rearrange("p (c t) -> p c t", c=4), [pb], [atT], eng='act')
            astk.close()
            fw.barrier()
            tail(1, pps, ys)
            sstk.close()
            fw.stk = st
            fw.finish()
        return nc


def build_prompt_nc(seq, stages=99):
    b = Builder(seq, stages=stages)
    return b.build()


_NC_CACHE = {}


def kernel(**inputs):
    inputs = {k: np.asarray(v) for k, v in inputs.items()}
    B, S = inputs["x_prompt"].shape[0], inputs["x_prompt"].shape[1]
    if "nc" not in _NC_CACHE:
        _NC_CACHE["nc"] = build_prompt_nc(S)
    nc = _NC_CACHE["nc"]
    consts = _host_consts()
    f = np.ascontiguousarray
    wts = {}
    for k, shp in WEIGHT_SHAPES.items():
        v = inputs[k]
        v = v[0] if k != "norm_final" else v
        wts[k] = f(v.reshape(shp).astype(np.float32, copy=False))
    in_maps = []
    L = NSEQ
    for c in range(8):
        b = c % B
        sl = slice(c * L, (c + 1) * L)
        m = {"xp": f(inputs["x_prompt"][b]), "pp": f(inputs["p_prompt"][0, b]),
             "xs": f(inputs["x_sample"][sl].reshape(L * 8, D)),
             "pps": f(inputs["p_sample"][0, sl].reshape(L * 8, 256)),
             "swkv": f(inputs["state_wkv"][0, sl].reshape(L * 8, 4096)),
             "sshift": f(inputs["state_shift"][0, sl]),
             "kc": f(inputs["cache_k_win"][0, sl].reshape(L, MAXWIN, 512)),
             "vc": f(inputs["cache_v_win"][0, sl].reshape(L, MAXWIN, 512))}
        m.update(wts)
        m.update(consts)
        in_maps.append(m)
    res = run_bass_kernel_spmd(nc, in_maps, core_ids=list(range(8))).results
    f32 = np.float32
    keep = min(MAXWIN, S)
    y_prompt = np.stack([res[b]["yp"] for b in range(B)]).astype(f32, copy=False)
    wkv_p = np.stack([res[b]["wkvp"] for b in range(B)])[None].astype(f32, copy=False)
    shift_p = np.stack([res[b]["shp"] for b in range(B)])[None].astype(f32, copy=False)
    kwin_p = np.stack([res[b]["kwp"].reshape(keep, 8, 64) for b in range(B)])[None].astype(f32, copy=False)
    vwin_p = np.stack([res[b]["vwp"].reshape(keep, 8, 64) for b in range(B)])[None].astype(f32, copy=False)
    y_sample = np.concatenate([res[c]["ys"].reshape(L, 8, D) for c in range(8)]).astype(f32, copy=False)
    wkv_s = np.concatenate([res[c]["wkvs"].reshape(L, 8, 64, 64) for c in range(8)])[None].astype(f32, copy=False)
    shift_s = np.concatenate([res[c]["shs"] for c in range(8)])[None].astype(f32, copy=False)
    kwin_s = np.concatenate([res[c]["kws"].reshape(L, MAXWIN, 8, 64) for c in range(8)])[None].astype(f32, copy=False)
    vwin_s = np.concatenate([res[c]["vws"].reshape(L, MAXWIN, 8, 64) for c in range(8)])[None].astype(f32, copy=False)
    return (y_prompt, y_sample, wkv_p, shift_p, kwin_p, vwin_p, wkv_s, shift_s, kwin_s, vwin_s)
```

```python
import numpy as np
from contextlib import ExitStack
import concourse.bass as bass
import concourse.mybir as mybir
from concourse.bass_utils import run_bass_kernel_spmd

F32 = mybir.dt.float32
BF16 = mybir.dt.bfloat16
I32 = mybir.dt.int32
AF = mybir.ActivationFunctionType
ALU = mybir.AluOpType
AX = mybir.AxisListType


class Buf:
    def __init__(self, t, name, psum=False):
        self.t = t
        self.name = name
        self.psum = psum
        self.st = {None: [{}, {}]}


def _merge(dst, src):
    for k, v in src.items():
        if dst.get(k, 0) < v:
            dst[k] = v


class FW:
    ENGS = ('pe', 'act', 'dve', 'pool', 'sp')

    def __init__(self, nc, st, n_dma_sems=24):
        self.nc = nc
        self.stk = st
        self.ops = {e: [] for e in self.ENGS}
        self.cnt = {e: 0 for e in self.ENGS}
        self.sem = {}
        self.semobj = {}
        for e in self.ENGS:
            s = st.enter_context(nc.semaphore("c_" + e))
            self.sem[e] = id(s)
            self.semobj[id(s)] = s
        self.waited = {e: {} for e in self.ENGS}
        self.dsems = {'hw': [], 'sw': []}
        for kind in ('hw', 'sw'):
            for i in range(n_dma_sems):
                s = st.enter_context(nc.semaphore("d%s%d" % (kind, i)))
                self.semobj[id(s)] = s
                self.dsems[kind].append([s, 0])
        self.dnext = {'hw': 0, 'sw': 0}
        self.out_tokens = {}
        self.n_inst = 0
        self.extra_dsems = []

    def sb(self, name, shape, dtype):
        t = self.stk.enter_context(self.nc.sbuf_tensor("s_" + name, list(shape), dtype))
        return Buf(t, name)

    def ps(self, name, shape, dtype):
        t = self.stk.enter_context(self.nc.psum_tensor("p_" + name, list(shape), dtype))
        return Buf(t, name, psum=True)

    @staticmethod
    def _norm(x):
        if isinstance(x, tuple):
            if x[0].psum:
                return (x[0], None)
            return x
        return (x, None)

    def _deps(self, reads, writes):
        deps = {}
        for b, k in map(self._norm, reads):
            if k is None:
                for kk, (w, r) in b.st.items():
                    _merge(deps, w)
                    if b.psum:
                        _merge(deps, r)
            else:
                _merge(deps, b.st[None][0])
                if k in b.st:
                    _merge(deps, b.st[k][0])
        for b, k in map(self._norm, writes):
            if k is None:
                for kk, (w, r) in b.st.items():
                    _merge(deps, w)
                    _merge(deps, r)
            else:
                _merge(deps, b.st[None][0])
                _merge(deps, b.st[None][1])
                if k in b.st:
                    _merge(deps, b.st[k][0])
                    _merge(deps, b.st[k][1])
        return deps

    def _update(self, reads, writes, tok):
        sid, val = tok
        for b, k in map(self._norm, reads):
            s = b.st.setdefault(k, [{}, {}])
            if s[1].get(sid, 0) < val:
                s[1][sid] = val
        for b, k in map(self._norm, writes):
            if k is None:
                b.st = {None: [{sid: val}, {}]}
            else:
                b.st[k] = [{sid: val}, {}]

    def _emit_waits(self, eng, deps, skip_self):
        lst = []
        wd = self.waited[eng]
        for sid, val in deps.items():
            if skip_self and sid == self.sem[eng]:
                continue
            if wd.get(sid, 0) >= val:
                continue
            wd[sid] = val
            lst.append((self.semobj[sid], val))
        return lst

    def op(self, eng, fn, reads=(), writes=()):
        deps = self._deps(reads, writes)
        waits = self._emit_waits(eng, deps, skip_self=(eng == 'pe'))
        self.cnt[eng] += 1
        idx = self.cnt[eng]
        semo = self.semobj[self.sem[eng]]

        def run(e, fn=fn, waits=waits, semo=semo):
            for s, v in waits:
                e.wait_ge(s, v)
            fn(e).then_inc(semo, 1)
        self.ops[eng].append(run)
        self.waited[eng][self.sem[eng]] = max(self.waited[eng].get(self.sem[eng], 0), 0)
        self._update(reads, writes, (self.sem[eng], idx))
        self.n_inst += 1
        return (self.sem[eng], idx)

    def dma(self, eng, out, in_, reads=(), writes=(), out_dram=False, nc_ok=False, n=1, fn=None, ent=None):
        deps = self._deps(reads, writes)
        kind = 'sw' if eng == 'pool' else 'hw'
        if ent is None:
            ent = self.dsems[kind][self.dnext[kind]]
            self.dnext[kind] = (self.dnext[kind] + 1) % len(self.dsems[kind])
            if ent[1] > 0:
                deps[id(ent[0])] = max(deps.get(id(ent[0]), 0), ent[1])
        dsem = ent[0]
        waits = self._emit_waits(eng, deps, skip_self=False)
        ent[1] += 16 * n
        val = ent[1]

        def run(e, waits=waits, dsem=dsem):
            for s, v in waits:
                e.wait_ge(s, v)
            if fn is not None:
                for ins in fn(e):
                    ins.then_inc(dsem, 16)
            else:
                kw = {}
                if nc_ok:
                    kw['allow_slow_non_contiguous'] = True
                e.dma_start(out=out, in_=in_, **kw).then_inc(dsem, 16)
        self.ops[eng].append(run)
        tok = (id(dsem), val)
        self._update(reads, writes, tok)
        if out_dram:
            self.out_tokens[id(dsem)] = val
        self.n_inst += 1
        return tok

    def new_dsem(self, name):
        s = self.stk.enter_context(self.nc.semaphore(name))
        self.semobj[id(s)] = s
        ent = [s, 0]
        self.extra_dsems.append(ent)
        return ent

    def barrier(self):
        for eng in self.ENGS:
            waits = []
            wd = self.waited[eng]
            for e2 in self.ENGS:
                sid = self.sem[e2]
                if e2 != eng and self.cnt[e2] > wd.get(sid, 0):
                    waits.append((self.semobj[sid], self.cnt[e2]))
                    wd[sid] = self.cnt[e2]
            for kind in ('hw', 'sw'):
                for sm, v in self.dsems[kind]:
                    if v > wd.get(id(sm), 0):
                        waits.append((sm, v))
                        wd[id(sm)] = v

            def run(e, waits=waits):
                for sm, v in waits:
                    e.wait_ge(sm, v)
            self.ops[eng].append(run)

    def make_identity(self, ident, dtype_f32_tmp=None):
        def f1(e):
            return e.memset(ident.t[:, :], 1.0)
        self.op('pool', f1, writes=[ident])
        def f2(e):
            return e.affine_select(ident.t[:, :], ident.t[:, :], [[-1, 128]], ALU.is_equal, 0.0,
                                   base=0, channel_multiplier=1)
        self.op('pool', f2, reads=[ident], writes=[ident])

    def finish(self):
        finals = [(self.semobj[sid], v) for sid, v in self.out_tokens.items()]
        nc = self.nc
        with nc.Block() as block:
            @block.tensor
            def _(e):
                for f in self.ops['pe']:
                    f(e)

            @block.scalar
            def _(e):
                for f in self.ops['act']:
                    f(e)

            @block.vector
            def _(e):
                for f in self.ops['dve']:
                    f(e)

            @block.gpsimd
            def _(e):
                for f in self.ops['pool']:
                    f(e)

            @block.sync
            def _(e):
                for f in self.ops['sp']:
                    f(e)
                for s, v in finals:
                    e.wait_ge(s, v)


D = 1024
RW = 512
RW_COLS = 1664
IN_COLS = 3200
QOFF, KOFF, VOFF = 1664, 1664 + 512, 1664 + 1024
W = 512
RING = 24
NKB = 17
RMS_EPS = 1e-6
GN_EPS = 64e-5
DECAY_C = float(np.exp(-0.5))
MAXWIN = 2048
NSEQ = 16


def _host_consts():
    c = {}
    s = np.arange(64)[:, None]
    t = np.arange(64)[None, :]
    c["cI"] = (-DECAY_C * (s <= t)).astype(np.float32)
    c["cS"] = (-DECAY_C * (s < t)).astype(np.float32)
    c["cR"] = (-DECAY_C * (s > t)).astype(np.float32)
    up_s = (s < t).astype(np.float32)
    up_i = (s <= t).astype(np.float32)
    lo_s = (t < s).astype(np.float32)
    c["mk"] = np.concatenate([up_s, lo_s, up_s, up_i, up_i], axis=1)
    bo = np.zeros((128, 128), np.float32)
    bo[:64, :64] = 1
    bo[64:, 64:] = 1
    c["bones"] = bo
    slopes = 2.0 ** (-8.0 * np.arange(1, 9) / 8)
    ki = np.arange(128)[:, None]
    qi = np.arange(128)[None, :]
    em = np.zeros((8, 128, NKB, 128), np.float32)
    for j in range(NKB):
        dl = 128 * j + qi - ki
        mult = np.zeros_like(dl, dtype=np.float64)
        for wd, dil in ((128, 1), (512, 4), (2048, 16)):
            mult += ((dl >= 0) & (dl % dil == 0) & (dl <= wd))
        for h in range(8):
            em[h, :, j, :] = mult * np.exp(-slopes[h] * np.maximum(dl, 0))
    c["em"] = em
    def fac(dl):
        mult = np.zeros(dl.shape, np.float64)
        for wd, dil in ((128, 1), (512, 4), (2048, 16)):
            mult += ((dl >= 0) & (dl % dil == 0) & (dl <= wd))
        return mult[None] * np.exp(-slopes[:, None, None, None] * np.maximum(dl, 0)[None])
    p_ = np.arange(128)[:, None, None]
    blk_ = np.arange(16)[None, :, None]
    t_ = np.arange(8)[None, None, :]
    ems = fac(MAXWIN + t_ - (blk_ * 128 + p_))
    c["ems"] = np.ascontiguousarray(ems.transpose(1, 2, 0, 3)).reshape(128, 1024).astype(np.float32)
    bq_ = np.arange(NSEQ)[None, :, None]
    emn = fac(t_ - (p_ % 8) + 0 * bq_) * ((p_ // 8) == bq_)[None]
    c["emn"] = np.ascontiguousarray(emn.transpose(1, 2, 0, 3)).reshape(128, 1024).astype(np.float32)
    return c


CONST_SHAPES = {"cI": [64, 64], "cS": [64, 64], "cR": [64, 64], "mk": [64, 320],
                "bones": [128, 128], "em": [8, 128, NKB, 128], "ems": [128, 1024], "emn": [128, 1024]}

WEIGHT_SHAPES = {
    "norm_mix": [D], "w_in": [D, IN_COLS], "mu": [RW_COLS], "w0": [RW], "w2": [32, RW],
    "a0": [RW], "a2": [32, RW], "g2": [64, RW], "k_k": [RW], "k_a": [RW], "r_k": [RW],
    "ln_x_w": [RW], "ln_x_b": [RW], "attn_gain": [RW], "w_out": [D, D], "norm_ffn": [D],
    "w_group": [D, 4], "b_group": [4], "w_expert_router": [D, 32], "b_expert_router": [32],
    "w_gate": [32, D, 256], "w_up": [32, D, 256], "w_down": [32, 256, D],
    "norm_ple": [D], "w_ple": [256, D], "w_ple_gate": [D, D], "norm_final": [D],
}


import os as _os
SKIP = set(_os.environ.get('KSKIP', '').split(','))
VC_ENG = _os.environ.get('KVCENG', 'pool')


class StopBuild(Exception):
    pass


class Builder:
    def __init__(self, seq, stages=99, dbg=()):
        self.seq = seq
        self.nw = seq // W
        self.stages = stages
        self.dbgnames = set(dbg)
        self.nc = bass.Bass("TRN2", target_bir_lowering=False)
        self.dr = {}
        self.dbg_out = {}

    def din(self, name, shape):
        self.dr[name] = self.nc.dram_tensor(name, list(shape), F32, kind="ExternalInput").ap()
        return self.dr[name]

    def dout(self, name, shape):
        self.dr[name] = self.nc.dram_tensor(name, list(shape), F32, kind="ExternalOutput").ap()
        return self.dr[name]

    def ck(self, x):
        if self.stages < x:
            raise StopBuild()

    def bank(self):
        for _ in range(8):
            b = self.PS[self.pnext]
            self.pnext = (self.pnext + 1) % 8
            if b not in self.held:
                return b
        raise RuntimeError("no free bank")

    def mm(self, out, lhsT, rhs, start, stop, r, w):
        self.fw.op('pe', lambda e: e.matmul(out, lhsT, rhs, start=start, stop=stop), reads=r, writes=w)

    def tr(self, out, in_, ident, r, w):
        self.fw.op('pe', lambda e: e.transpose(out, in_, ident), reads=r, writes=w)

    def act(self, out, in_, func, r, w, bias=None, scale=None, accum=None):
        kw = {}
        if bias is not None:
            kw['bias'] = bias
        if scale is not None:
            kw['scale'] = scale
        if accum is not None:
            kw['accum_out'] = accum
        self.fw.op('act', lambda e: e.activation(out, in_, func, **kw), reads=r, writes=w)

    def tt(self, out, a, b, op, r, w, eng='dve'):
        self.fw.op(eng, lambda e: e.tensor_tensor(out, a, b, op), reads=r, writes=w)

    def ts(self, out, a, s1, s2, op0, op1, r, w, eng='dve'):
        if s2 is None:
            self.fw.op(eng, lambda e: e.tensor_scalar(out, a, s1, None, op0), reads=r, writes=w)
        else:
            self.fw.op(eng, lambda e: e.tensor_scalar(out, a, s1, s2, op0, op1), reads=r, writes=w)

    def stt(self, out, a, sc, b, op0, op1, r, w, eng='dve'):
        self.fw.op(eng, lambda e: e.scalar_tensor_tensor(out, a, sc, b, op0, op1), reads=r, writes=w)

    def cp(self, out, in_, r, w, eng='dve'):
        if eng == 'act':
            self.act(out, in_, AF.Copy, r, w)
        else:
            self.fw.op(eng, lambda e: e.tensor_copy(out, in_), reads=r, writes=w)

    def dbg(self, name, ap, shape, reads):
        if name not in self.dbgnames:
            return
        d = self.dout("dbg_" + name, shape)
        self.fw.dma('sp', d, ap, reads=reads, out_dram=True)

    def wload(self, src_ap, rows_k, cols, eng='sp', reads=()):
        t = self.WP[self.wnext]
        self.wnext = (self.wnext + 1) % len(self.WP)
        self.fw.dma(eng, t.t[:, 0:rows_k, 0:cols], src_ap, reads=list(reads), writes=[t], nc_ok=True)
        return t

    def vec_load(self, name, nk, dst=None):
        t = self.fw.sb("v_" + name, [128, nk], F32) if dst is None else dst
        self.fw.dma('sp', t.t[:, :], self.dr[name].rearrange("(k p) -> p k", p=128), writes=[t], nc_ok=True)
        return t

    def norm_T(self, Xs, gain, hT, rows=128):
        fw = self.fw
        for blk, Xb in enumerate(Xs):
            ss = self.ss
            self.act(self.junk.t[0:rows, :, :].rearrange("p a b -> p (a b)"), Xb.t[0:rows, :], AF.Square, [Xb], [self.junk, ss], accum=ss.t[0:rows, :])
            self.ts(ss.t[0:rows, :], ss.t[0:rows, :], 1.0 / D, RMS_EPS, ALU.mult, ALU.add, [ss], [ss])
            self.act(ss.t[0:rows, :], ss.t[0:rows, :], AF.Sqrt, [ss], [ss])
            fw.op('dve', lambda e, ss=ss: e.reciprocal(ss.t[0:rows, :], ss.t[0:rows, :]), reads=[ss], writes=[ss])
            xn = self.xn
            self.ts(xn.t[0:rows, :], Xb.t[0:rows, :], ss.t[0:rows, 0:1], None, ALU.mult, None, [Xb, ss], [xn])
            for half in range(2):
                pb = self.bank()
                for kk in range(4):
                    k = half * 4 + kk
                    self.tr(pb.t[:, kk * 128:kk * 128 + rows], xn.t[0:rows, k * 128:(k + 1) * 128],
                            self.identf.t[0:rows, 0:rows], [xn, self.identf], [pb])
                src = pb.t[:, :].rearrange("p (k t) -> p k t", k=4)[:, :, 0:rows]
                gb = gain.t[:, half * 4:half * 4 + 4].unsqueeze(2).to_broadcast([128, 4, rows])
                self.tt(hT.t[:, half * 4:half * 4 + 4, blk * 128:blk * 128 + rows], src, gb, ALU.mult,
                        [pb, gain], [(hT, ('b', blk, half))])

    def build(self):
        nc = self.nc
        seq, nw = self.seq, self.nw
        keep = min(MAXWIN, seq)
        xp = self.din("xp", [seq, D])
        pp = self.din("pp", [seq, 256])
        for k, shp in WEIGHT_SHAPES.items():
            self.din(k, shp)
        for k, shp in CONST_SHAPES.items():
            self.din(k, shp)
        yp = self.dout("yp", [seq, D])
        wkvp = self.dout("wkvp", [8, 64, 64])
        shp_o = self.dout("shp", [RW_COLS])
        kwp = self.dout("kwp", [keep, 512])
        vwp = self.dout("vwp", [keep, 512])
        xs = self.din("xs", [128, D])
        pps = self.din("pps", [128, 256])
        swkv = self.din("swkv", [128, 4096])
        sshift = self.din("sshift", [NSEQ, RW_COLS])
        kc = self.din("kc", [NSEQ, MAXWIN, 512])
        vc = self.din("vc", [NSEQ, MAXWIN, 512])
        ys = self.dout("ys", [128, D])
        wkvs = self.dout("wkvs", [128, 4096])
        shs = self.dout("shs", [NSEQ, RW_COLS])
        kws = self.dout("kws", [NSEQ, MAXWIN, 512])
        vws = self.dout("vws", [NSEQ, MAXWIN, 512])
        scrA = self.dout("scrA", [128, 6 * 512])
        scrB = self.dout("scrB", [128, 512])
        dr = self.dr
        with ExitStack() as st:
            fw = self.fw = FW(nc, st, n_dma_sems=16)
            sb = fw.sb
            self.PS = [fw.ps("ps%d" % i, [128, 512], F32) for i in range(8)]
            self.pnext = 0
            self.held = set()
            self.WP = [sb("wp%d" % i, [128, 8, 512], BF16) for i in range(2)]
            self.wnext = 0
            self.identf = identf = sb("identf", [128, 128], F32)
            fw.make_identity(identf)
            g_mix, g_ffn, g_ple = self.vec_load("norm_mix", 8), self.vec_load("norm_ffn", 8), self.vec_load("norm_ple", 8)
            gfin = sb("gfin", [128, D], F32)
            fw.dma('sp', gfin.t[:, :], dr["norm_final"].partition_broadcast(128), writes=[gfin])
            again = sb("again", [128, RW], F32)
            fw.dma('sp', again.t[:, :], dr["attn_gain"].partition_broadcast(128), writes=[again])
            w0rep = sb("w0rep", [128, RW], F32)
            fw.dma('sp', w0rep.t[:, :], dr["w0"].partition_broadcast(128), writes=[w0rep])
            LW = sb("LW", [128, RW], BF16)
            fw.dma('pool', LW.t[0:32, :], dr["w2"], writes=[(LW, 0)])
            fw.dma('pool', LW.t[32:64, :], dr["a2"], writes=[(LW, 1)])
            fw.dma('pool', LW.t[64:128, :], dr["g2"], writes=[(LW, 2)])
            Wrt = sb("Wrt", [128, 8, 36], BF16)
            fw.dma('pool', Wrt.t[:, :, 0:4], dr["w_group"].rearrange("(k p) c -> p k c", p=128), writes=[(Wrt, 0)])
            fw.dma('pool', Wrt.t[:, :, 4:36], dr["w_expert_router"].rearrange("(k p) c -> p k c", p=128), writes=[(Wrt, 1)])
            brep = sb("brep", [128, 36], F32)
            fw.dma('sp', brep.t[:, 0:4], dr["b_group"].partition_broadcast(128), writes=[(brep, 0)])
            fw.dma('sp', brep.t[:, 4:36], dr["b_expert_router"].partition_broadcast(128), writes=[(brep, 1)])
            X = [sb("X%d" % i, [128, D], F32) for i in range(4)]
            self.xn = sb("xn", [128, D], F32)
            self.ss = sb("ss", [128, 1], F32)
            hT = sb("hT", [128, 8, W], BF16)
            rwT = sb("rwT", [128, 4, W], BF16)
            atT = sb("atT", [128, 4, W], BF16)
            Lt = sb("Lt", [128, W], BF16)
            T = {n: sb("t_" + n, [128, W], F32) for n in ("a", "kkr", "kk", "e1", "e2", "tmp")}
            lg = sb("lg", [128, 36], F32)
            comb = sb("comb", [128, 4, 32], F32)
            rt = {n: sb("r_" + n, [128, 1], F32) for n in ("gmax", "ngmax", "sumg", "gw", "nm1", "e2", "den", "w1", "w2")}
            oh = sb("oh", [128, 4], F32)
            eg = sb("eg", [128, 4], F32)
            elm = sb("elm", [128, 32], F32)
            top8 = sb("top8", [128, 8], F32)
            c1 = sb("c1", [128, 32], F32)
            sgt = T["tmp"]
            Aff = sb("Aff", [128, 2, W], BF16)
            self.junk = Aff
            Wd = [sb("Wd%d" % i, [128, 2, D], BF16) for i in range(1)]
            pT = sb("pT", [128, 2, W], BF16)
            pin = sb("pin", [128, 256], F32)
            yst = self.xn
            P_ = dict(identf=identf, g_mix=g_mix, g_ffn=g_ffn, g_ple=g_ple, gfin=gfin, again=again, w0rep=w0rep, LW=LW,
                      X=X, hT=hT, rwT=rwT, atT=atT, Lt=Lt, T=T)
            self.P_ = P_

            roll_ent = fw.new_dsem("d_roll")
            nrows = MAXWIN - 8
            piece = nrows // 4
            for j in range(NSEQ if 'roll' not in SKIP else 0):
                for src, dst in ((dr["kc"], dr["kws"]), (dr["vc"], dr["vws"])):
                    def mkroll(e, j=j, src=src, dst=dst):
                        return [e.dma_start(out=dst[j, a:a + piece, :], in_=src[j, a + 8:a + piece + 8, :])
                                for a in range(0, nrows, piece)]
                    fw.dma('act', None, None, fn=mkroll, n=4, out_dram=True, ent=roll_ent)

            pstk = ExitStack()
            fw.stk = pstk
            cI, cS, cR = sb("cI", [64, 64], F32), sb("cS", [64, 64], F32), sb("cR", [64, 64], F32)
            mk = sb("mk", [64, 320], F32)
            bones = sb("bones", [128, 128], BF16)
            for t_, n_ in ((cI, "cI"), (cS, "cS"), (cR, "cR"), (mk, "mk")):
                fw.dma('sp', t_.t[:, :], dr[n_], writes=[t_])
            fw.dma('pool', bones.t[:, :], dr["bones"], writes=[bones])
            identb = sb("identb", [64, 64], BF16)
            self.cp(identb.t[:, :], identf.t[0:64, 0:64], [identf], [identb])
            mu = self.vec_load("mu", 13)
            w0, a0, k_k, k_a = self.vec_load("w0", 4), self.vec_load("a0", 4), self.vec_load("k_k", 4), self.vec_load("k_a", 4)
            r_k, ln_w, ln_b = self.vec_load("r_k", 4), self.vec_load("ln_x_w", 4), self.vec_load("ln_x_b", 4)
            omka = sb("omka", [128, 4], F32)
            self.ts(omka.t[:, :], k_a.t[:, :], -1.0, 1.0, ALU.mult, ALU.add, [k_a], [omka])
            carry = sb("carry", [128, 13], F32)
            fw.op('pool', lambda e: e.memset(carry.t[:, :], 0.0), writes=[carry])
            ST = sb("ST", [128, 4, 64], F32)
            STb = sb("STb", [128, 4, 128], BF16)
            fw.op('pool', lambda e: e.memset(ST.t[:, :, :], 0.0), writes=[ST])
            fw.op('pool', lambda e: e.memset(STb.t[:, :, :], 0.0), writes=[STb])
            KT = sb("KT", [128, 4, RING * 128], BF16)
            V1 = sb("V1", [128, RING, 8, 65], BF16)
            fw.op('pool', lambda e: e.memset(KT.t[:, :, :], 0.0), writes=[KT])
            fw.op('pool', lambda e: e.memset(V1.t[:, :, :, :], 0.0), writes=[V1])
            QT = sb("QT", [128, 4, W], BF16)
            EMa = sb("EMa", [128, 8, 128], BF16)
            EMb = sb("EMb", [128, NKB - 8, 128], BF16)
            Pk = [sb("Pk%d" % i, [128, W + 1], F32) for i in range(2)]
            dlt = sb("dlt", [128, W], F32)
            xm = [sb("xm%d" % i, [128, W], F32) for i in range(3)]
            xml = xm[0]
            T = dict(T)
            T["kf"] = T["kkr"]
            T["bb"] = T["tmp"]
            T["e3"] = dlt
            sqb = sb("sqb", [128, W], BF16)
            t3b = sb("t3b", [128, W], BF16)
            bonus = sb("bonus", [128, W], F32)
            gg = sb("gg", [128, W], F32)
            ATt, RTt, KTt, BTt = (sb(n, [128, W], BF16) for n in ("ATt", "RTt", "KTt", "BTt"))
            Pend = sb("Pend", [128, 8], F32)
            sgwT = sb("sgwT", [64, 8, 128], F32)
            eR = sb("eR", [64, 8, 128], F32)
            Vtok, Ktok, Btok = (sb(n, [64, 8, 128], BF16) for n in ("Vtok", "Ktok", "Btok"))
            Ytok = sb("Ytok", [64, 8, 128], F32)
            Ach = [[sb("Ach%d_%d" % (i, h), [64, 320], BF16) for h in range(2)] for i in range(2)]
            Xi = [[sb("Xi%d_%d" % (i, p), [64, 2, 192], BF16) for p in range(2)] for i in range(2)]
            Zb = sb("Zb", [64, 128], BF16)
            Ub = sb("Ub", [64, 128], BF16)
            gs1, gs2 = sb("gs1", [64, 16], F32), sb("gs2", [64, 16], F32)
            Otok = [sb("Otok%d" % i, [128, RW], F32) for i in range(4)]
            orec = sb("orec", [128, 1], F32)
            EbL = [sb("Eb%d" % i, [128, 512], BF16) for i in range(2)]
            PmL = [sb("Pm%d" % i, [128, 512], BF16) for i in range(2)]
            stK, stV = Otok[3], Otok[2]

            def wscr(name, shape):
                return nc.dram_tensor(name, list(shape), BF16, kind="Internal").ap(), Buf(None, name)
            kp = lambda ap: ap.rearrange("(k p) c -> p k c", p=128)
            win_bf, win_b = wscr("win_bf", [128, 8, IN_COLS])
            em_bf, em_b = wscr("em_bf", [8, 128, NKB, 128])
            wo_bf, wo_b = wscr("wo_bf", [128, 8, D])
            wgu_bf, wgu_b = wscr("wgu_bf", [32, 128, 8, 512])
            wd_bf, wd_b = wscr("wd_bf", [32, 128, 2, D])
            wpg_bf, wpg_b = wscr("wpg_bf", [128, 8, D])
            wpl_bf, wpl_b = wscr("wpl_bf", [128, 2, D])
            for c0 in range(0, IN_COLS, 640):
                fw.dma('pool', win_bf[:, :, c0:c0 + 640], kp(dr["w_in"])[:, :, c0:c0 + 640], writes=[(win_b, c0)], nc_ok=True)
            for h in range(8):
                fw.dma('pool', em_bf[h], dr["em"][h], writes=[(em_b, h)])
            for hf in range(2):
                fw.dma('pool', wo_bf[:, :, hf * 512:(hf + 1) * 512], kp(dr["w_out"])[:, :, hf * 512:(hf + 1) * 512], writes=[(wo_b, hf)], nc_ok=True)
            for ex in range(32):
                fw.dma('pool', wgu_bf[ex][:, :, 0:256], kp(dr["w_gate"][ex]), writes=[(wgu_b, (ex, 0))], nc_ok=True)
                fw.dma('pool', wgu_bf[ex][:, :, 256:512], kp(dr["w_up"][ex]), writes=[(wgu_b, (ex, 1))], nc_ok=True)
                fw.dma('pool', wd_bf[ex], kp(dr["w_down"][ex]), writes=[(wd_b, ex)], nc_ok=True)
            for hf in range(2):
                fw.dma('pool', wpg_bf[:, :, hf * 512:(hf + 1) * 512], kp(dr["w_ple_gate"])[:, :, hf * 512:(hf + 1) * 512], writes=[(wpg_b, hf)], nc_ok=True)
            fw.dma('pool', wpl_bf, kp(dr["w_ple"]), writes=[wpl_b], nc_ok=True)
            win_v = win_bf

            def tail(nblk, pp_ap, y_ap):
                nt = nblk * 128
                wo = [self.wload(wo_bf[:, :, hf * 512:(hf + 1) * 512], 8, 512, reads=[(wo_b, hf)]) for hf in range(2)]
                for blk in range(nblk):
                    bs = slice(blk * 128, (blk + 1) * 128)
                    for hf in range(2):
                        pb = self.bank()
                        for k in range(8):
                            lhs = rwT.t[:, k, bs] if k < 4 else atT.t[:, k - 4, bs]
                            self.mm(pb.t[:, :], lhs, wo[hf].t[:, k, :], k == 0, k == 7, [rwT, atT, wo[hf]], [pb])
                        self.tt(X[blk].t[:, hf * 512:(hf + 1) * 512], X[blk].t[:, hf * 512:(hf + 1) * 512], pb.t[:, :], ALU.add,
                                [pb, X[blk]], [X[blk]])

                self.norm_T(X[0:nblk], g_ffn, hT)
                for blk in range(nblk):
                    bs = slice(blk * 128, (blk + 1) * 128)
                    pb = self.bank()
                    for k in range(8):
                        self.mm(pb.t[:, 0:36], hT.t[:, k, bs], Wrt.t[:, k, :], k == 0, k == 7, [hT, Wrt], [pb])
                    self.tt(lg.t[:, :], pb.t[:, 0:36], brep.t[:, :], ALU.add, [pb, brep], [lg])
                    fw.op('dve', lambda e: e.reduce_max(rt["gmax"].t[:, :], lg.t[:, 0:4], AX.X), reads=[lg], writes=[rt["gmax"]])
                    self.ts(oh.t[:, :], lg.t[:, 0:4], rt["gmax"].t[:, 0:1], None, ALU.is_equal, None, [lg, rt["gmax"]], [oh])
                    self.ts(rt["ngmax"].t[:, :], rt["gmax"].t[:, :], -1.0, None, ALU.mult, None, [rt["gmax"]], [rt["ngmax"]])
                    self.act(eg.t[:, :], lg.t[:, 0:4], AF.Exp, [lg, rt["ngmax"]], [eg, rt["sumg"]], bias=rt["ngmax"].t[:, 0:1],
                             accum=rt["sumg"].t[:, :])
                    fw.op('dve', lambda e: e.reciprocal(rt["gw"].t[:, :], rt["sumg"].t[:, :]), reads=[rt["sumg"]], writes=[rt["gw"]])
                    self.ts(oh.t[:, :], oh.t[:, :], 1e30, -1e30, ALU.mult, ALU.add, [oh], [oh])
                    self.tt(elm.t[:, :].rearrange("p (g e) -> p g e", g=4), lg.t[:, 4:36].rearrange("p (g e) -> p g e", g=4),
                            oh.t[:, :].unsqueeze(2).to_broadcast([128, 4, 8]), ALU.add, [lg, oh], [elm])
                    fw.op('dve', lambda e: e.max(top8.t[:, :], elm.t[:, :]), reads=[elm], writes=[top8])
                    self.ts(rt["nm1"].t[:, :], top8.t[:, 0:1], -1.0, None, ALU.mult, None, [top8], [rt["nm1"]])
                    self.act(rt["e2"].t[:, :], top8.t[:, 1:2], AF.Exp, [top8, rt["nm1"]], [rt["e2"]], bias=rt["nm1"].t[:, 0:1])
                    self.ts(rt["den"].t[:, :], rt["e2"].t[:, :], 1.0, None, ALU.add, None, [rt["e2"]], [rt["den"]])
                    fw.op('dve', lambda e: e.reciprocal(rt["den"].t[:, :], rt["den"].t[:, :]), reads=[rt["den"]], writes=[rt["den"]])
                    self.tt(rt["w1"].t[:, :], rt["den"].t[:, :], rt["gw"].t[:, :], ALU.mult, [rt["den"], rt["gw"]], [rt["w1"]])
                    self.tt(rt["w2"].t[:, :], rt["w1"].t[:, :], rt["e2"].t[:, :], ALU.mult, [rt["w1"], rt["e2"]], [rt["w2"]])
                    self.ts(c1.t[:, :], elm.t[:, :], top8.t[:, 0:1], rt["w1"].t[:, 0:1], ALU.is_equal, ALU.mult, [elm, top8, rt["w1"]], [c1])
                    self.ts(comb.t[:, blk, :], elm.t[:, :], top8.t[:, 1:2], rt["w2"].t[:, 0:1], ALU.is_equal, ALU.mult,
                            [elm, top8, rt["w2"]], [(comb, blk)])
                    self.tt(comb.t[:, blk, :], comb.t[:, blk, :], c1.t[:, :], ALU.add, [(comb, blk), c1], [(comb, blk)])
                for ex in range(32):
                    wgu = self.WP[self.wnext]
                    self.wnext = (self.wnext + 1) % len(self.WP)
                    fw.dma('sp', wgu.t[:, :, :], wgu_bf[ex], reads=[(wgu_b, (ex, 0)), (wgu_b, (ex, 1))], writes=[wgu])
                    wd = Wd[ex % len(Wd)]
                    fw.dma('sp', wd.t[:, :, :], wd_bf[ex], reads=[(wd_b, ex)], writes=[wd])
                    for fb in range(2):
                        gbk, ubk = self.bank(), self.bank()
                        for k in range(8):
                            self.mm(gbk.t[:, 0:nt], wgu.t[:, k, fb * 128:(fb + 1) * 128], hT.t[:, k, 0:nt], k == 0, k == 7, [wgu, hT], [gbk])
                        for k in range(8):
                            self.mm(ubk.t[:, 0:nt], wgu.t[:, k, 256 + fb * 128:256 + (fb + 1) * 128], hT.t[:, k, 0:nt], k == 0, k == 7, [wgu, hT], [ubk])
                        self.act(sgt.t[:, 0:nt], gbk.t[:, 0:nt], AF.Silu, [gbk], [sgt])
                        self.tt(Aff.t[:, fb, 0:nt], sgt.t[:, 0:nt], ubk.t[:, 0:nt], ALU.mult, [sgt, ubk], [(Aff, fb)])
                    for blk in range(nblk):
                        bs = slice(blk * 128, (blk + 1) * 128)
                        for hf in range(2):
                            pb = self.bank()
                            for fb in range(2):
                                self.mm(pb.t[:, :], Aff.t[:, fb, bs], wd.t[:, fb, hf * 512:(hf + 1) * 512], fb == 0, fb == 1, [Aff, wd], [pb])
                            xs_ = X[blk].t[:, hf * 512:(hf + 1) * 512]
                            self.stt(xs_, pb.t[:, :], comb.t[:, blk, ex:ex + 1], xs_, ALU.mult, ALU.add, [pb, (comb, blk), X[blk]], [X[blk]])

                self.norm_T(X[0:nblk], g_ple, hT)
                for blk in range(nblk):
                    fw.dma('sp', pin.t[:, :], pp_ap[blk * 128:(blk + 1) * 128, :], writes=[pin])
                    pb = self.bank()
                    for k2 in range(2):
                        self.tr(pb.t[:, k2 * 128:(k2 + 1) * 128], pin.t[:, k2 * 128:(k2 + 1) * 128], identf.t[:, :], [pin, identf], [pb])
                    self.cp(pT.t[:, :, blk * 128:(blk + 1) * 128], pb.t[:, 0:256].rearrange("p (k t) -> p k t", k=2), [pb], [(pT, blk)])
                for hf in range(2):
                    wpg = self.wload(wpg_bf[:, :, hf * 512:(hf + 1) * 512], 8, 512, reads=[(wpg_b, hf)])
                    wpl = self.wload(wpl_bf[:, :, hf * 512:(hf + 1) * 512], 2, 512, reads=[wpl_b])
                    for blk in range(nblk):
                        bs = slice(blk * 128, (blk + 1) * 128)
                        gbk, pbk = self.bank(), self.bank()
                        for k in range(8):
                            self.mm(gbk.t[:, :], hT.t[:, k, bs], wpg.t[:, k, :], k == 0, k == 7, [hT, wpg], [gbk])
                        for k2 in range(2):
                            self.mm(pbk.t[:, :], pT.t[:, k2, bs], wpl.t[:, k2, :], k2 == 0, k2 == 1, [pT, wpl], [pbk])
                        self.act(sgt.t[:, :], gbk.t[:, :], AF.Sigmoid, [gbk], [sgt])
                        self.tt(sgt.t[:, :], sgt.t[:, :], pbk.t[:, :], ALU.mult, [sgt, pbk], [sgt])
                        xs_ = X[blk].t[:, hf * 512:(hf + 1) * 512]
                        self.tt(xs_, xs_, sgt.t[:, :], ALU.add, [X[blk], sgt], [X[blk]])
                for blk in range(nblk):
                    ss = self.ss
                    Xb = X[blk]
                    self.act(self.junk.t[:, :, :].rearrange("p a b -> p (a b)"), Xb.t[:, :], AF.Square, [Xb], [self.junk, ss], accum=ss.t[:, :])
                    self.ts(ss.t[:, :], ss.t[:, :], 1.0 / D, RMS_EPS, ALU.mult, ALU.add, [ss], [ss])
                    self.act(ss.t[:, :], ss.t[:, :], AF.Sqrt, [ss], [ss])
                    fw.op('dve', lambda e: e.reciprocal(self.ss.t[:, :], self.ss.t[:, :]), reads=[ss], writes=[ss])
                    self.stt(yst.t[:, :], Xb.t[:, :], ss.t[:, 0:1], gfin.t[:, :], ALU.mult, ALU.mult, [Xb, ss, gfin], [yst])
                    fw.dma('sp', y_ap[blk * 128:(blk + 1) * 128, :], yst.t[:, :], reads=[yst], out_dram=True)

            for w in range(nw if self.stages >= 0.05 else 0):
              try:
                  t0 = w * W
                  for blk in range(4):
                      fw.dma('sp', X[blk].t[:, :], xp[t0 + blk * 128:t0 + (blk + 1) * 128, :], writes=[X[blk]])
                  self.ck(0.1)
                  self.norm_T(X, g_mix, hT)
                  self.ck(0.2)
                  self.dbg("hT", hT.t[:, :, :], None, [hT]) if False else None

                  def rw_block(ci, col0, dst, wt, wc0):
                      pb = self.bank()
                      for k in range(8):
                          self.mm(pb.t[:, :], wt.t[:, k, wc0:wc0 + 128], hT.t[:, k, :], k == 0, k == 7, [wt, hT], [pb])
                      P = Pk[ci % 2]
                      self.cp(P.t[:, 1:W + 1], pb.t[:, :], [pb], [(P, 1)], eng='act')
                      self.cp(P.t[:, 0:1], carry.t[:, ci:ci + 1], [(carry, ci)], [(P, 0)])
                      self.cp(carry.t[:, ci:ci + 1], P.t[:, W:W + 1], [(P, 1)], [(carry, ci)])
                      self.tt(dlt.t[:, :], P.t[:, 0:W], P.t[:, 1:W + 1], ALU.subtract, [P], [dlt])
                      self.stt(dst.t[:, :], dlt.t[:, :], mu.t[:, ci:ci + 1], P.t[:, 1:W + 1], ALU.mult, ALU.add,
                               [dlt, mu, P], [dst])

                  wl = self.wload(win_v[:, :, 1536:1664], 8, 128, reads=[win_b])
                  rw_block(12, 1536, xml, wl, 0)
                  self.act(Lt.t[0:32, :], xml.t[0:32, :], AF.Tanh, [xml], [(Lt, 0)])
                  self.cp(Lt.t[32:64, :], xml.t[32:64, :], [xml], [(Lt, 1)])
                  self.act(Lt.t[64:128, :], xml.t[64:128, :], AF.Sigmoid, [xml], [(Lt, 2)])

                  self.ck(0.3)
                  for hp in range(4):
                      wt3 = self.WP[self.wnext]
                      self.wnext = (self.wnext + 1) % len(self.WP)
                      for i3 in range(3):
                          c0 = i3 * 512 + hp * 128
                          fw.dma('sp', wt3.t[:, :, i3 * 128:(i3 + 1) * 128], win_v[:, :, c0:c0 + 128], reads=[win_b], writes=[(wt3, i3)], nc_ok=True)
                      rw_block(hp, hp * 128, xm[0], wt3, 0)
                      rw_block(4 + hp, 512 + hp * 128, xm[1], wt3, 128)
                      rw_block(8 + hp, 1024 + hp * 128, xm[2], wt3, 256)
                      self.ck(0.4)
                      xr, xk, xv = xm
                      hc = slice(hp * 128, (hp + 1) * 128)
                      pb = self.bank()
                      self.mm(pb.t[:, :], LW.t[32:64, hc], Lt.t[32:64, :], True, True, [LW, Lt], [pb])
                      self.act(T["a"].t[:, :], pb.t[:, :], AF.Sigmoid, [pb, a0], [T["a"]], bias=a0.t[:, hp:hp + 1])
                      pb = self.bank()
                      self.mm(pb.t[:, :], LW.t[64:128, hc], Lt.t[64:128, :], True, True, [LW, Lt], [pb])
                      self.cp(gg.t[:, :], pb.t[:, :], [pb], [gg], eng='act')
                      for half in range(2):
                          pb = self.bank()
                          for cc in range(4):
                              c = half * 4 + cc
                              self.mm(pb.t[0:64, cc * 128:(cc + 1) * 128], Lt.t[0:32, c * 64:(c + 1) * 64], LW.t[0:32, hc],
                                      True, True, [LW, Lt], [pb])
                          self.tt(sgwT.t[:, half * 4:half * 4 + 4, :], pb.t[0:64, :].rearrange("p (c f) -> p c f", c=4),
                                  w0rep.t[0:64, hc].unsqueeze(1).to_broadcast([64, 4, 128]), ALU.add,
                                  [pb, w0rep], [(sgwT, half)])
                          self.act(sgwT.t[:, half * 4:half * 4 + 4, :], sgwT.t[:, half * 4:half * 4 + 4, :], AF.Sigmoid,
                                   [(sgwT, half)], [(sgwT, half)])
                      self.ck(0.5)
                      pinc, pexc = self.bank(), self.bank()
                      for c in range(8):
                          self.mm(pinc.t[:, c * 64:(c + 1) * 64], sgwT.t[:, c, :], cI.t[:, :], True, True, [sgwT, cI], [pinc])
                      for c in range(8):
                          self.mm(pexc.t[:, c * 64:(c + 1) * 64], sgwT.t[:, c, :], cS.t[:, :], True, True, [sgwT, cS], [pexc])
                      self.act(T["e1"].t[:, :], pinc.t[:, :], AF.Exp, [pinc], [T["e1"]])
                      self.act(T["e2"].t[:, :], pinc.t[:, :], AF.Exp, [pinc], [T["e2"]], scale=-1.0)
                      self.act(T["e3"].t[:, :], pexc.t[:, :], AF.Exp, [pexc], [T["e3"]])
                      for half in range(2):
                          pb = self.bank()
                          for cc in range(4):
                              c = half * 4 + cc
                              self.mm(pb.t[0:64, cc * 128:(cc + 1) * 128], cR.t[:, :], sgwT.t[:, c, :], True, True, [sgwT, cR], [pb])
                          self.act(eR.t[:, half * 4:half * 4 + 4, :], pb.t[0:64, :].rearrange("p (c f) -> p c f", c=4), AF.Exp,
                                   [pb], [(eR, half)])
                      self.cp(Pend.t[:, :], T["e1"].t[:, 63::64], [T["e1"]], [Pend])
                      self.ck(0.6)
                      self.ts(T["kkr"].t[:, :], xk.t[:, :], k_k.t[:, hp:hp + 1], None, ALU.mult, None, [xk, k_k], [T["kkr"]])
                      self.act(sqb.t[:, :], T["kkr"].t[:, :], AF.Square, [T["kkr"]], [sqb])
                      pb = self.bank()
                      self.mm(pb.t[:, :], bones.t[:, :], sqb.t[:, :], True, True, [bones, sqb], [pb])
                      self.act(T["tmp"].t[:, :], pb.t[:, :], AF.Sqrt, [pb], [T["tmp"]])
                      self.ts(T["tmp"].t[:, :], T["tmp"].t[:, :], 1e-12, None, ALU.max, None, [T["tmp"]], [T["tmp"]])
                      fw.op('dve', lambda e: e.reciprocal(T["tmp"].t[:, :], T["tmp"].t[:, :]), reads=[T["tmp"]], writes=[T["tmp"]])
                      self.tt(T["kk"].t[:, :], T["kkr"].t[:, :], T["tmp"].t[:, :], ALU.mult, [T["kkr"], T["tmp"]], [T["kk"]])
                      self.ts(T["tmp"].t[:, :], T["a"].t[:, :], k_a.t[:, hp:hp + 1], omka.t[:, hp:hp + 1], ALU.mult, ALU.add,
                              [T["a"], k_a, omka], [T["tmp"]])
                      self.tt(T["kf"].t[:, :], xk.t[:, :], T["tmp"].t[:, :], ALU.mult, [xk, T["tmp"]], [T["kf"]])
                      self.tt(T["bb"].t[:, :], T["kk"].t[:, :], T["a"].t[:, :], ALU.mult, [T["kk"], T["a"]], [T["bb"]])
                      self.stt(ATt.t[:, :], T["kk"].t[:, :], -1.0, T["e3"].t[:, :], ALU.mult, ALU.mult, [T["kk"], T["e3"]], [ATt])
                      self.tt(RTt.t[:, :], xr.t[:, :], T["e1"].t[:, :], ALU.mult, [xr, T["e1"]], [RTt])
                      self.tt(KTt.t[:, :], T["kf"].t[:, :], T["e2"].t[:, :], ALU.mult, [T["kf"], T["e2"]], [KTt])
                      self.tt(BTt.t[:, :], T["bb"].t[:, :], T["e2"].t[:, :], ALU.mult, [T["bb"], T["e2"]], [BTt])
                      self.stt(t3b.t[:, :], xr.t[:, :], r_k.t[:, hp:hp + 1], T["kf"].t[:, :], ALU.mult, ALU.mult,
                               [xr, r_k, T["kf"]], [t3b])
                      pb = self.bank()
                      self.mm(pb.t[:, :], bones.t[:, :], t3b.t[:, :], True, True, [bones, t3b], [pb])
                      self.tt(bonus.t[:, :], pb.t[:, :], xv.t[:, :], ALU.mult, [pb, xv], [bonus])
                      self.ck(0.7)
                      for half in range(2):
                          for (srcT, dstT, mode) in ((T["kf"], Ktok, 1), (T["bb"], Btok, 1), (xv, Vtok, 0)):
                              pb = self.bank()
                              for cc in range(4):
                                  c = half * 4 + cc
                                  self.tr(pb.t[0:64, cc * 128:(cc + 1) * 128], srcT.t[:, c * 64:(c + 1) * 64], identf.t[:, :],
                                          [srcT, identf], [pb])
                              pv = pb.t[0:64, :].rearrange("p (c f) -> p c f", c=4)
                              if mode:
                                  self.tt(dstT.t[:, half * 4:half * 4 + 4, :], pv, eR.t[:, half * 4:half * 4 + 4, :], ALU.mult,
                                          [pb, (eR, half)], [(dstT, half)])
                              else:
                                  self.cp(dstT.t[:, half * 4:half * 4 + 4, :], pv, [pb], [(dstT, half)], eng='act')

                      self.ck(0.8)
                      def chunk_prep(c, res):
                          cc = slice(c * 64, (c + 1) * 64)
                          A = Ach[c % 2]
                          Xc = Xi[c % 2]
                          for h in range(2):
                              hr = slice(h * 64, (h + 1) * 64)
                              pb = self.bank()
                              ops = ((BTt, ATt), (ATt, BTt), (KTt, ATt), (BTt, RTt), (KTt, RTt))
                              for i, (l_, r_) in enumerate(ops):
                                  self.mm(pb.t[0:64, i * 64:(i + 1) * 64], l_.t[hr, cc], r_.t[hr, cc], True, True, [l_, r_], [pb])
                              self.tt(A[h].t[:, :], pb.t[0:64, 0:320], mk.t[:, :], ALU.mult, [pb, mk], [A[h]])
                              self.cp(Xc[0].t[:, h, 0:128], A[h].t[:, 0:128], [A[h]], [(Xc[0], (h, 'n'))], eng='act')
                              self.tt(Xc[0].t[:, h, 128:192], A[h].t[:, 0:64], identb.t[:, :], ALU.add, [A[h], identb], [(Xc[0], (h, 'm'))])
                          yield
                          cur = 0
                          for ph in range(6):
                              pb = self.bank()
                              src, dstx = Xc[cur], Xc[1 - cur]
                              for h in range(2):
                                  N_, NT_, M_ = src.t[:, h, 0:64], src.t[:, h, 64:128], src.t[:, h, 128:192]
                                  o = h * 192
                                  if ph < 5:
                                      self.mm(pb.t[0:64, o:o + 64], NT_, N_, True, True, [src], [pb])
                                      self.mm(pb.t[0:64, o + 64:o + 128], N_, NT_, True, True, [src], [pb])
                                  if ph >= 1:
                                      self.mm(pb.t[0:64, o + 128:o + 192], NT_, M_, True, True, [src], [pb])
                              pv = pb.t[0:64, 0:384].rearrange("p (h x) -> p h x", h=2)
                              if ph < 5:
                                  self.cp(dstx.t[:, :, 0:128], pv[:, :, 0:128], [pb], [(dstx, (0, 'n')), (dstx, (1, 'n'))], eng='act')
                              if ph >= 1:
                                  self.tt(dstx.t[:, :, 128:192], pv[:, :, 128:192], src.t[:, :, 128:192], ALU.add, [pb, src], [(dstx, (0, 'm')), (dstx, (1, 'm'))])
                              else:
                                  self.cp(dstx.t[:, :, 128:192], src.t[:, :, 128:192], [src], [(dstx, (0, 'm')), (dstx, (1, 'm'))])
                              cur = 1 - cur
                              yield
                          res[c] = (A, Xc[cur])

                      def chunk_chain(c, A, Xf):
                          cc = slice(c * 64, (c + 1) * 64)
                          zb, ub, yb, sbk = self.bank(), self.bank(), self.bank(), self.bank()
                          hold = [zb, ub, yb, sbk]
                          self.held.update(hold)
                          self.mm(zb.t[0:64, 0:128], ATt.t[:, cc], STb.t[:, hp, :], True, False, [ATt, (STb, hp)], [zb])
                          for h in range(2):
                              hr = slice(h * 64, (h + 1) * 64)
                              self.mm(zb.t[0:64, hr], A[h].t[:, 128:192], Vtok.t[:, c, hr], False, h == 1, [A[h], Vtok], [zb])
                          self.cp(Zb.t[:, :], zb.t[0:64, 0:128], [zb], [Zb], eng='act')
                          yield
                          for h in range(2):
                              hr = slice(h * 64, (h + 1) * 64)
                              self.mm(ub.t[0:64, hr], Xf.t[:, h, 128:192], Zb.t[:, hr], True, True, [Xf, Zb], [ub])
                          self.cp(Ub.t[:, :], ub.t[0:64, 0:128], [ub], [Ub])
                          yield
                          self.mm(sbk.t[:, 0:128], Btok.t[:, c, :], Ub.t[:, :], True, False, [Btok, Ub], [sbk])
                          self.mm(sbk.t[:, 0:128], Ktok.t[:, c, :], Vtok.t[:, c, :], False, True, [Ktok, Vtok], [sbk])
                          for h in range(2):
                              hr = slice(h * 64, (h + 1) * 64)
                              self.stt(ST.t[hr, hp, :], ST.t[hr, hp, :], Pend.t[hr, c:c + 1], sbk.t[hr, hr], ALU.mult, ALU.add,
                                       [(ST, hp), Pend, sbk], [(ST, hp)])
                          yield
                          self.mm(yb.t[0:64, 0:128], RTt.t[:, cc], STb.t[:, hp, :], True, False, [RTt, (STb, hp)], [yb])
                          for h in range(2):
                              hr = slice(h * 64, (h + 1) * 64)
                              self.mm(yb.t[0:64, hr], A[h].t[:, 256:320], Vtok.t[:, c, hr], False, False, [A[h], Vtok], [yb])
                              self.mm(yb.t[0:64, hr], A[h].t[:, 192:256], Ub.t[:, hr], False, h == 1, [A[h], Ub], [yb])
                          self.cp(Ytok.t[:, c, :], yb.t[0:64, 0:128], [yb], [(Ytok, c)], eng='act')
                          for h in range(2):
                              hr = slice(h * 64, (h + 1) * 64)
                              self.cp(STb.t[hr, hp, hr], ST.t[hr, hp, :], [(ST, hp)], [(STb, hp)], eng='act')
                          self.held.difference_update(hold)
                          yield

                      preps = {}
                      for _ in chunk_prep(0, preps):
                          pass
                      for c in range(8):
                          A, Xf = preps[c]
                          g1 = chunk_prep(c + 1, preps) if c + 1 < 8 else iter(())
                          g2 = chunk_chain(c, A, Xf)
                          while True:
                              d2 = next(g2, 0) == 0
                              d1 = next(g1, 0) == 0
                              if d1 and d2:
                                  break

                      self.ck(0.9)
                      yv = Ytok.t[:, :, :].rearrange("p c (h v) -> p (c h) v", h=2)
                      fw.op('dve', lambda e: e.reduce_sum(gs1.t[:, :], yv, AX.X), reads=[Ytok], writes=[gs1])
                      ysq = self.xn
                      ysq3 = ysq.t[0:64, :].rearrange("p (c f) -> p c f", c=8)
                      self.act(ysq3, Ytok.t[:, :, :], AF.Square, [Ytok], [ysq])
                      ysv = ysq.t[0:64, :].rearrange("p (c h v) -> p (c h) v", c=8, h=2)
                      fw.op('dve', lambda e: e.reduce_sum(gs2.t[:, :], ysv, AX.X), reads=[ysq], writes=[gs2])
                      self.ts(gs1.t[:, :], gs1.t[:, :], 1.0 / 64, None, ALU.mult, None, [gs1], [gs1])
                      self.tt(ysq.t[0:64, 0:16], gs1.t[:, :], gs1.t[:, :], ALU.mult, [gs1], [ysq])
                      self.stt(gs2.t[:, :], gs2.t[:, :], 1.0 / 64, ysq.t[0:64, 0:16], ALU.mult, ALU.subtract, [gs2, ysq], [gs2])
                      self.ts(gs2.t[:, :], gs2.t[:, :], GN_EPS, None, ALU.add, None, [gs2], [gs2])
                      self.act(gs2.t[:, :], gs2.t[:, :], AF.Sqrt, [gs2], [gs2])
                      fw.op('dve', lambda e: e.reciprocal(gs2.t[:, :], gs2.t[:, :]), reads=[gs2], writes=[gs2])
                      mb = gs1.t[:, :].unsqueeze(2).to_broadcast([64, 16, 64])
                      rb = gs2.t[:, :].unsqueeze(2).to_broadcast([64, 16, 64])
                      self.tt(yv, yv, mb, ALU.subtract, [Ytok, gs1], [Ytok])
                      self.tt(yv, yv, rb, ALU.mult, [Ytok, gs2], [Ytok])
                      pb = self.bank()
                      for c in range(8):
                          self.tr(pb.t[:, c * 64:(c + 1) * 64], Ytok.t[:, c, :], identf.t[0:64, 0:64], [Ytok, identf], [pb])
                      self.act(T["tmp"].t[:, :], pb.t[:, :], AF.Identity, [pb, ln_w, ln_b], [T["tmp"]],
                               bias=ln_b.t[:, hp:hp + 1], scale=ln_w.t[:, hp:hp + 1])
                      self.tt(T["tmp"].t[:, :], T["tmp"].t[:, :], bonus.t[:, :], ALU.add, [T["tmp"], bonus], [T["tmp"]])
                      self.tt(rwT.t[:, hp, :], T["tmp"].t[:, :], gg.t[:, :], ALU.mult, [T["tmp"], gg], [(rwT, hp)])

                  if self.stages < 2:
                      continue
                  wq_ = self.wload(win_v[:, :, QOFF:QOFF + 512], 8, 512, reads=[win_b])
                  for cb in range(4):
                      pb = self.bank()
                      for k in range(8):
                          self.mm(pb.t[:, :], wq_.t[:, k, cb * 128:(cb + 1) * 128], hT.t[:, k, :], k == 0, k == 7, [wq_, hT], [pb])
                      self.act(QT.t[:, cb, :], pb.t[:, :], AF.Copy, [pb], [(QT, cb)], scale=0.125)
                  wk2 = self.wload(win_v[:, :, KOFF:KOFF + 512], 8, 512, reads=[win_b])
                  slot0 = (w * 4) % RING
                  for cb in range(4):
                      pb = self.bank()
                      for k in range(8):
                          self.mm(pb.t[:, :], wk2.t[:, k, cb * 128:(cb + 1) * 128], hT.t[:, k, :], k == 0, k == 7, [wk2, hT], [pb])
                      self.cp(KT.t[:, cb, slot0 * 128:slot0 * 128 + W], pb.t[:, :], [pb], [KT], eng='act' if cb % 2 else 'dve')
                  wv2 = self.wload(win_v[:, :, VOFF:VOFF + 512], 8, 512, reads=[win_b])
                  for (wt, isv) in ((wk2, 0), (wv2, 1)):
                      for blk in range(4):
                          tok0 = t0 + blk * 128
                          need_out = tok0 >= seq - keep
                          if not isv and not need_out:
                              continue
                          pb = self.bank()
                          for k in range(8):
                              self.mm(pb.t[:, :], hT.t[:, k, blk * 128:(blk + 1) * 128], wt.t[:, k, :], k == 0, k == 7, [wt, hT], [pb])
                          if isv:
                              slot = (w * 4 + blk) % RING
                              self.cp(V1.t[:, slot, :, 1:65], pb.t[:, :].rearrange("p (h v) -> p h v", h=8), [pb], [V1], eng='act')
                              fw.op('pool', lambda e, slot=slot: e.memset(V1.t[:, slot, :, 0:1], 1.0), writes=[V1])
                          if need_out:
                              stg = stV if isv else stK
                              self.cp(stg.t[:, :], pb.t[:, :], [pb], [stg])
                              dst = (vwp if isv else kwp)[tok0 - (seq - keep):tok0 - (seq - keep) + 128, :]
                              fw.dma('sp', dst, stg.t[:, :], reads=[stg], out_dram=True)

                  pend = []

                  def flush():
                      while pend:
                          pend.pop(0)()
                  gcount = 0
                  for h in range(8):
                      hp, hr = h // 2, slice((h % 2) * 64, (h % 2) * 64 + 64)
                      flush()
                      if h == 0:
                          fw.dma('sp', EMa.t[:, :, :], em_bf[0][:, 0:8, :], reads=[(em_b, 0)], writes=[EMa])
                      fw.dma('sp', EMb.t[:, :, :], em_bf[h][:, 8:NKB, :], reads=[(em_b, h)], writes=[EMb])
                      for bq in range(4):
                          gbq = w * 4 + bq
                          ob = self.bank()
                          self.held.add(ob)
                          for g0 in range(0, NKB, 4):
                              n = min(4, NKB - g0)
                              sb_ = self.bank()
                              self.held.add(sb_)
                              for jj in range(n):
                                  slot = (gbq - (g0 + jj)) % RING
                                  self.mm(sb_.t[:, jj * 128:(jj + 1) * 128], KT.t[hr, hp, slot * 128:(slot + 1) * 128],
                                          QT.t[hr, hp, bq * 128:(bq + 1) * 128], True, True, [KT, (QT, hp)], [sb_])
                              Eb_, Pm_a = EbL[gcount % 2], PmL[gcount % 2]
                              gcount += 1

                              EMh, e0 = (EMa, g0) if g0 < 8 else (EMb, g0 - 8)

                              def rest(sb_=sb_, n=n, g0=g0, ob=ob, bq=bq, h=h, gbq=gbq, Eb_=Eb_, Pm_a=Pm_a, EMh=EMh, e0=e0):
                                  self.act(Eb_.t[:, 0:n * 128], sb_.t[:, 0:n * 128], AF.Exp, [sb_], [Eb_])
                                  self.held.discard(sb_)
                                  self.tt(Pm_a.t[:, 0:n * 128], Eb_.t[:, 0:n * 128],
                                          EMh.t[:, e0:e0 + n, :].rearrange("p j q -> p (j q)"), ALU.mult, [Eb_, EMh], [Pm_a])
                                  for jj in range(n):
                                      j = g0 + jj
                                      slot = (gbq - j) % RING
                                      self.mm(ob.t[:, 0:65], Pm_a.t[:, jj * 128:(jj + 1) * 128], V1.t[:, slot, h, :],
                                              j == 0, j == NKB - 1, [Pm_a, V1], [ob])
                                  if g0 + n >= NKB:
                                      self.held.discard(ob)
                                      fw.op('dve', lambda e, ob=ob: e.reciprocal(orec.t[:, :], ob.t[:, 0:1]), reads=[ob], writes=[orec])
                                      self.ts(Otok[bq].t[:, h * 64:(h + 1) * 64], ob.t[:, 1:65], orec.t[:, 0:1], None, ALU.mult, None,
                                              [ob, orec], [(Otok[bq], h)])
                              flush()
                              pend.append(rest)
                              if bq == 3 and g0 == 8 and h + 1 < 8:
                                  fw.dma('sp', EMa.t[:, :, :], em_bf[h + 1][:, 0:8, :], reads=[(em_b, h + 1)], writes=[EMa])
                  flush()
                  for bq in range(4):
                      Ot = Otok[bq]
                      ss = self.ss
                      self.act(self.junk.t[:, 0, :], Ot.t[:, :], AF.Square, [Ot], [self.junk, ss], accum=ss.t[:, :])
                      self.ts(ss.t[:, :], ss.t[:, :], 1.0 / RW, RMS_EPS, ALU.mult, ALU.add, [ss], [ss])
                      self.act(ss.t[:, :], ss.t[:, :], AF.Sqrt, [ss], [ss])
                      fw.op('dve', lambda e: e.reciprocal(self.ss.t[:, :], self.ss.t[:, :]), reads=[ss], writes=[ss])
                      self.stt(Ot.t[:, :], Ot.t[:, :], ss.t[:, 0:1], again.t[:, :], ALU.mult, ALU.mult, [Ot, ss, again], [Ot])
                      pb = self.bank()
                      for cb in range(4):
                          self.tr(pb.t[:, cb * 128:(cb + 1) * 128], Ot.t[:, cb * 128:(cb + 1) * 128], identf.t[:, :], [Ot, identf], [pb])
                      self.cp(atT.t[:, :, bq * 128:(bq + 1) * 128], pb.t[:, :].rearrange("p (c t) -> p c t", c=4), [pb], [(atT, bq)], eng='act')

                  if self.stages < 4:
                      continue
                  tail(4, pp[t0:t0 + W, :], yp[t0:t0 + W, :])
              except StopBuild:
                break

            fw.dma('sp', shp_o.rearrange("(k p) -> p k", p=128), carry.t[:, :], reads=[carry], out_dram=True, nc_ok=True)
            for hp in range(4):
                pb = self.bank()
                self.tr(pb.t[0:64, 0:128], ST.t[:, hp, :], identf.t[:, :], [ST, identf], [pb])
                self.cp(stK.t[0:64, 0:128], pb.t[0:64, 0:128], [pb], [stK])
                for h2 in range(2):
                    fw.dma('sp', wkvp[hp * 2 + h2], stK.t[0:64, h2 * 64:(h2 + 1) * 64], reads=[stK], out_dram=True)

            pstk.close()
            fw.barrier()
            if 'sample' in SKIP:
                fw.stk = st
                fw.finish()
                return nc
            sstk = ExitStack()
            fw.stk = sstk
            T = P_["T"]
            Xs = X[0]
            Ptok = sb("Ptok", [128, IN_COLS], F32)
            XM = sb("XM", [128, RW_COLS], F32)
            ATtok = sb("ATtok", [128, RW], F32)
            Gtok = sb("Gtok", [128, RW], F32)
            Btk = sb("Btk", [128, RW], F32)
            n8 = sb("n8", [128, 8], F32)
            m8 = sb("m8", [128, 8], F32)
            v8 = sb("v8", [128, 8], F32)
            QTs = sb("QTs", [128, 4, 128], BF16)
            KTn = sb("KTn", [128, 4, 128], BF16)
            Vn1 = sb("Vn1", [128, 8, 65], BF16)
            scrA_b = Buf(None, "scrA")
            scrB_b = Buf(None, "scrB")

            fw.dma('sp', Xs.t[:, :], xs, writes=[Xs])
            self.norm_T([Xs], g_mix, hT)
            for ci, c0 in enumerate(range(0, IN_COLS, 512)):
                cw = min(512, IN_COLS - c0)
                wl = self.wload(win_v[:, :, c0:c0 + cw], 8, cw, reads=[win_b])
                pb = self.bank()
                for k in range(8):
                    self.mm(pb.t[:, 0:cw], hT.t[:, k, 0:128], wl.t[:, k, 0:cw], k == 0, k == 7, [wl, hT], [pb])
                self.cp(Ptok.t[:, c0:c0 + cw], pb.t[:, 0:cw], [pb], [(Ptok, ci)], eng='act' if ci % 2 else 'dve')
            fw.dma('sp', shs, Ptok.t[7:128:8, 0:RW_COLS], reads=[Ptok], out_dram=True)
            for (off_, dst_) in ((KOFF, kws), (VOFF, vws)):
                def mknew(e, off_=off_, dst_=dst_):
                    return [e.dma_start(out=dst_[j, MAXWIN - 8:MAXWIN, :], in_=Ptok.t[8 * j:8 * j + 8, off_:off_ + 512])
                            for j in range(NSEQ)]
                fw.dma('sp', None, None, fn=mknew, n=NSEQ, reads=[Ptok], out_dram=True)
            for cb in range(4):
                pb = self.bank()
                self.tr(pb.t[:, 0:128], Ptok.t[:, QOFF + cb * 128:QOFF + (cb + 1) * 128], identf.t[:, :], [Ptok, identf], [pb])
                self.act(QTs.t[:, cb, :], pb.t[:, 0:128], AF.Copy, [pb], [(QTs, cb)], scale=0.125)
                pb = self.bank()
                self.tr(pb.t[:, 0:128], Ptok.t[:, KOFF + cb * 128:KOFF + (cb + 1) * 128], identf.t[:, :], [Ptok, identf], [pb])
                self.cp(KTn.t[:, cb, :], pb.t[:, 0:128], [pb], [(KTn, cb)])
            fw.op('pool', lambda e: e.memset(Vn1.t[:, :, 0:1], 1.0), writes=[Vn1])
            self.cp(Vn1.t[:, :, 1:65], Ptok.t[:, VOFF:VOFF + 512].rearrange("p (h v) -> p h v", h=8), [Ptok, Vn1], [Vn1])

            rstk = ExitStack()
            fw.stk = rstk
            S = sb("S", [128, 4096], F32)
            TM = [sb("TM%d" % i, [128, 4096], F32) for i in range(2)]
            RLh = sb("RLh", [128, 6, 8, 64], F32)
            Yrl = sb("Yrl", [128, 8, 64], F32)
            skk = sb("skk", [128, 64], F32)
            reps = {}
            for n_ in ("a0", "k_k", "k_a", "r_k", "ln_x_w", "ln_x_b"):
                reps[n_] = sb("rep_" + n_, [128, RW], F32)
                fw.dma('sp', reps[n_].t[:, :], dr[n_].partition_broadcast(128), writes=[reps[n_]])
            fw.dma('sp', S.t[:, :], swkv, writes=[S])
            mu_rep = TM[0]
            fw.dma('sp', mu_rep.t[:, 0:RW_COLS], dr["mu"].partition_broadcast(128), writes=[mu_rep])
            fw.dma('sp', XM.t[1:128, :], Ptok.t[0:127, 0:RW_COLS], reads=[Ptok], writes=[XM])
            fw.dma('sp', XM.t[0:128:8, :], sshift, writes=[XM])
            Pr = Ptok.t[:, 0:RW_COLS]
            self.tt(XM.t[:, :], XM.t[:, :], Pr, ALU.subtract, [XM, Ptok], [XM])
            self.tt(XM.t[:, :], XM.t[:, :], mu_rep.t[:, 0:RW_COLS], ALU.mult, [XM, mu_rep], [XM])
            self.tt(XM.t[:, :], XM.t[:, :], Pr, ALU.add, [XM, Ptok], [XM])
            xr, xk, xv = XM.t[:, 0:512], XM.t[:, 512:1024], XM.t[:, 1024:1536]
            RLb = [X[1], X[1], X[2], X[2], X[3], X[3]]
            RL = [RLb[j].t[:, (j % 2) * 512:(j % 2) * 512 + 512] for j in range(6)]
            h3 = lambda ap: ap.rearrange("p (h n) -> p h n", h=8)
            bc3 = lambda b8: b8.t[:, :].unsqueeze(2).to_broadcast([128, 8, 64])
            pb = self.bank()
            self.tr(pb.t[:, 0:128], XM.t[:, 1536:1664], identf.t[:, :], [XM, identf], [pb])
            self.act(Lt.t[0:32, 0:128], pb.t[0:32, 0:128], AF.Tanh, [pb], [(Lt, 0)])
            self.cp(Lt.t[32:64, 0:128], pb.t[32:64, 0:128], [pb], [(Lt, 1)])
            self.act(Lt.t[64:128, 0:128], pb.t[64:128, 0:128], AF.Sigmoid, [pb], [(Lt, 2)])
            tA, tB, Atok, kkr = T["e1"], T["e2"], T["a"], T["kkr"]
            pb = self.bank()
            self.mm(pb.t[:, :], Lt.t[0:32, 0:128], LW.t[0:32, :], True, True, [Lt, LW], [pb])
            self.tt(tA.t[:, :], pb.t[:, :], w0rep.t[:, :], ALU.add, [pb, w0rep], [tA])
            self.act(tA.t[:, :], tA.t[:, :], AF.Sigmoid, [tA], [tA])
            self.act(RL[1], tA.t[:, :], AF.Exp, [tA], [X[1]], scale=-DECAY_C)
            pb = self.bank()
            self.mm(pb.t[:, :], Lt.t[32:64, 0:128], LW.t[32:64, :], True, True, [Lt, LW], [pb])
            self.tt(Atok.t[:, :], pb.t[:, :], reps["a0"].t[:, :], ALU.add, [pb, reps["a0"]], [Atok])
            self.act(Atok.t[:, :], Atok.t[:, :], AF.Sigmoid, [Atok], [Atok])
            pb = self.bank()
            self.mm(pb.t[:, :], Lt.t[64:128, 0:128], LW.t[64:128, :], True, True, [Lt, LW], [pb])
            self.cp(Gtok.t[:, :], pb.t[:, :], [pb], [Gtok], eng='act')
            self.tt(kkr.t[:, :], xk, reps["k_k"].t[:, :], ALU.mult, [XM, reps["k_k"]], [kkr])
            self.act(tB.t[:, :], kkr.t[:, :], AF.Square, [kkr], [tB])
            fw.op('dve', lambda e: e.reduce_sum(n8.t[:, :], h3(tB.t[:, :]), AX.X), reads=[tB], writes=[n8])
            self.act(n8.t[:, :], n8.t[:, :], AF.Sqrt, [n8], [n8])
            self.ts(n8.t[:, :], n8.t[:, :], 1e-12, None, ALU.max, None, [n8], [n8])
            fw.op('dve', lambda e: e.reciprocal(n8.t[:, :], n8.t[:, :]), reads=[n8], writes=[n8])
            self.tt(h3(RL[4]), h3(kkr.t[:, :]), bc3(n8), ALU.mult, [kkr, n8], [X[3]])
            self.ts(tA.t[:, :], Atok.t[:, :], -1.0, None, ALU.add, None, [Atok], [tA])
            self.tt(tA.t[:, :], tA.t[:, :], reps["k_a"].t[:, :], ALU.mult, [tA, reps["k_a"]], [tA])
            self.ts(tA.t[:, :], tA.t[:, :], 1.0, None, ALU.add, None, [tA], [tA])
            self.tt(RL[2], xk, tA.t[:, :], ALU.mult, [XM, tA], [X[2]])
            self.tt(RL[5], RL[4], Atok.t[:, :], ALU.mult, [X[3], Atok], [X[3]])
            self.cp(RL[0], xr, [XM], [X[1]])
            self.cp(RL[3], xv, [XM], [X[2]], eng='act')
            self.tt(tA.t[:, :], xr, RL[2], ALU.mult, [XM, X[2]], [tA])
            self.tt(tA.t[:, :], tA.t[:, :], reps["r_k"].t[:, :], ALU.mult, [tA, reps["r_k"]], [tA])
            fw.op('dve', lambda e: e.reduce_sum(m8.t[:, :], h3(tA.t[:, :]), AX.X), reads=[tA], writes=[m8])
            self.tt(h3(Btk.t[:, :]), h3(xv), bc3(m8), ALU.mult, [XM, m8], [Btk])
            for i3 in range(3):
                fw.dma('sp', scrA[:, i3 * 1024:(i3 + 1) * 1024], X[1 + i3].t[:, :], reads=[X[1 + i3]], writes=[(scrA_b, i3)])
            scrA_v = scrA.rearrange("p (j h n) -> p j h n", j=6, h=8)
            for b in range(NSEQ):
                def mkrl(e, b=b):
                    return [e.dma_start(out=RLh.t[b * 8:(b + 1) * 8, j, :, :],
                                        in_=scrA_v[b * 8:(b + 1) * 8, j, :, :].rearrange("t h n -> h t n"),
                                        allow_slow_non_contiguous=True) for j in range(6)]
                fw.dma('sp' if b % 2 else 'act', None, None, fn=mkrl, n=6, reads=[scrA_b], writes=[(RLh, b)])
            S3 = S.t[:, :].rearrange("p (v k) -> p v k", v=64)
            TM3 = [t_.t[:, :].rearrange("p (v k) -> p v k", v=64) for t_ in TM]
            kbc = lambda j, t: RLh.t[:, j, t, :].unsqueeze(1).to_broadcast([128, 64, 64])
            vbc = lambda ap: ap.unsqueeze(2).to_broadcast([128, 64, 64])
            for t in range(8 if 'rec' not in SKIP else 0):
                self.tt(TM3[0], S3, kbc(4, t), ALU.mult, [S, RLh], [TM[0]])
                fw.op('dve', lambda e: e.reduce_sum(skk.t[:, :], TM3[0], AX.X), reads=[TM[0]], writes=[skk])
                self.tt(S3, S3, kbc(1, t), ALU.mult, [S, RLh], [S])
                self.tt(TM3[1], vbc(skk.t[:, :]), kbc(5, t), ALU.mult, [skk, RLh], [TM[1]], eng='pool')
                self.tt(S3, S3, TM3[1], ALU.subtract, [S, TM[1]], [S])
                self.tt(TM3[0], vbc(RLh.t[:, 3, t, :]), kbc(2, t), ALU.mult, [RLh], [TM[0]], eng='pool')
                self.tt(S3, S3, TM3[0], ALU.add, [S, TM[0]], [S])
                self.tt(TM3[1], S3, kbc(0, t), ALU.mult, [S, RLh], [TM[1]])
                fw.op('dve', lambda e, t=t: e.reduce_sum(Yrl.t[:, t, :], TM3[1], AX.X), reads=[TM[1]], writes=[(Yrl, t)])
            fw.dma('sp', wkvs, S.t[:, :], reads=[S], out_dram=True)
            fw.dma('sp', scrB, Yrl.t[:, :, :].rearrange("p t v -> p (t v)"), reads=[Yrl], writes=[scrB_b])
            Ytk = T["kk"]
            scrB_v = scrB.rearrange("p (t v) -> p t v", t=8)
            for b in range(NSEQ):
                fw.dma('sp' if b % 2 else 'act', Ytk.t[b * 8:(b + 1) * 8, :].rearrange("t (h v) -> t h v", h=8),
                       scrB_v[b * 8:(b + 1) * 8, :, :].rearrange("h t v -> t h v"),
                       reads=[scrB_b], writes=[(Ytk, b)], nc_ok=True)
            yvs = h3(Ytk.t[:, :])
            fw.op('dve', lambda e: e.reduce_sum(m8.t[:, :], yvs, AX.X), reads=[Ytk], writes=[m8])
            self.act(tB.t[:, :], Ytk.t[:, :], AF.Square, [Ytk], [tB])
            fw.op('dve', lambda e: e.reduce_sum(v8.t[:, :], h3(tB.t[:, :]), AX.X), reads=[tB], writes=[v8])
            self.ts(m8.t[:, :], m8.t[:, :], 1.0 / 64, None, ALU.mult, None, [m8], [m8])
            self.tt(n8.t[:, :], m8.t[:, :], m8.t[:, :], ALU.mult, [m8], [n8])
            self.stt(v8.t[:, :], v8.t[:, :], 1.0 / 64, n8.t[:, :], ALU.mult, ALU.subtract, [v8, n8], [v8])
            self.ts(v8.t[:, :], v8.t[:, :], GN_EPS, None, ALU.add, None, [v8], [v8])
            self.act(v8.t[:, :], v8.t[:, :], AF.Sqrt, [v8], [v8])
            fw.op('dve', lambda e: e.reciprocal(v8.t[:, :], v8.t[:, :]), reads=[v8], writes=[v8])
            self.tt(yvs, yvs, bc3(m8), ALU.subtract, [Ytk, m8], [Ytk])
            self.tt(yvs, yvs, bc3(v8), ALU.mult, [Ytk, v8], [Ytk])
            self.tt(Ytk.t[:, :], Ytk.t[:, :], reps["ln_x_w"].t[:, :], ALU.mult, [Ytk, reps["ln_x_w"]], [Ytk])
            self.tt(Ytk.t[:, :], Ytk.t[:, :], reps["ln_x_b"].t[:, :], ALU.add, [Ytk, reps["ln_x_b"]], [Ytk])
            self.tt(Ytk.t[:, :], Ytk.t[:, :], Btk.t[:, :], ALU.add, [Ytk, Btk], [Ytk])
            self.tt(Ytk.t[:, :], Ytk.t[:, :], Gtok.t[:, :], ALU.mult, [Ytk, Gtok], [Ytk])
            pb = self.bank()
            for cb in range(4):
                self.tr(pb.t[:, cb * 128:(cb + 1) * 128], Ytk.t[:, cb * 128:(cb + 1) * 128], identf.t[:, :], [Ytk, identf], [pb])
            self.cp(rwT.t[:, :, 0:128], pb.t[:, :].rearrange("p (c t) -> p c t", c=4), [pb], [rwT], eng='act')
            rstk.close()
            fw.barrier()

            astk = ExitStack()
            fw.stk = astk
            EMs = sb("EMs", [128, 1024], BF16)
            EMn = sb("EMn", [128, 1024], BF16)
            fw.dma('pool', EMs.t[:, :], dr["ems"], writes=[EMs])
            fw.dma('pool', EMn.t[:, :], dr["emn"], writes=[EMn])
            Kld = [sb("Kld%d" % i, [128, 4, 512], F32) for i in range(2)]
            Vld = [sb("Vld%d" % i, [128, 4, 512], F32) for i in range(2)]
            KTg = [sb("KTg%d" % i, [128, 4, 512], BF16) for i in range(2)]
            V1s = [sb("V1s%d" % i, [128, 8, 8, 65], BF16) for i in range(2)]
            for v1 in V1s:
                fw.op('pool', lambda e, v1=v1: e.memset(v1.t[:, :, :, 0:1], 1.0), writes=[v1])
            Ebs = sb("Ebs", [128, 512], BF16)
            Pms = [sb("Pms%d" % i, [128, 512], BF16) for i in range(2)]
            Pmn = sb("Pmn", [128, 64], BF16)
            ostg = [sb("ostg%d" % i, [8, 512], F32) for i in range(2)]
            orec8 = sb("orec8", [8, 8], F32)
            Qbd = sb("Qbd", [128, NSEQ, 4, 64], BF16)
            fw.op('pool', lambda e: e.memset(Qbd.t[:, :, :, :], 0.0), writes=[Qbd])
            for hp in range(4):
                for h2 in range(2):
                    hr = slice(h2 * 64, (h2 + 1) * 64)
                    c0 = (2 * hp + h2) * 8
                    self.cp(Qbd.t[hr, :, hp, c0:c0 + 8], QTs.t[hr, hp, :].rearrange("p (b t) -> p b t", b=NSEQ), [QTs, Qbd], [Qbd],
                            eng='act' if h2 else 'dve')
            zl = sb("zl", [128, 8], BF16)
            fw.op('pool', lambda e: e.memset(zl.t[:, :], 0.0), writes=[zl])
            gi = 0
            for b in range(NSEQ if 'attn' not in SKIP else 0):
                O2 = [self.bank(), self.bank()]
                self.held.update(O2)
                qs = slice(b * 8, (b + 1) * 8)
                for O in (O2 if 'a_pv' not in SKIP else []):
                    self.mm(O.t[0:8, 0:260], zl.t[:, :], EMs.t[:, 0:260], True, False, [zl, EMs], [O])
                for half in range(2):
                    sc = self.bank()
                    self.held.add(sc)
                    V1h = V1s[half]
                    for g2 in range(2):
                        g = half * 2 + g2
                        Kt, Vt, KTt = Kld[gi % 2], Vld[gi % 2], KTg[gi % 2]
                        gi += 1
                        fw.dma('sp', Kt.t[:, :, :], kc[b, g * 512:(g + 1) * 512, :].rearrange("(j p) c -> p j c", p=128), writes=[Kt])
                        fw.dma('sp', Vt.t[:, :, :], vc[b, g * 512:(g + 1) * 512, :].rearrange("(j p) c -> p j c", p=128), writes=[Vt])
                        for hp in range(4):
                            pb = self.bank()
                            for j in range(4):
                                self.tr(pb.t[:, j * 128:(j + 1) * 128], Kt.t[:, j, hp * 128:(hp + 1) * 128], identf.t[:, :], [Kt, identf], [pb])
                            self.cp(KTt.t[:, hp, :], pb.t[:, :], [pb], [(KTt, hp)], eng='act' if hp % 2 else 'dve')
                        if 'a_vc' not in SKIP:
                            self.cp(V1h.t[:, g2 * 4:(g2 + 1) * 4, :, 1:65], Vt.t[:, :, :].rearrange("p j (h v) -> p j h v", h=8),
                                    [Vt], [(V1h, g2)], eng=VC_ENG)
                        for j in range(4 if 'a_sc' not in SKIP else 0):
                            jj = g2 * 4 + j
                            for hp in range(4):
                                self.mm(sc.t[:, jj * 64:(jj + 1) * 64], KTt.t[:, hp, j * 128:(j + 1) * 128], Qbd.t[:, b, hp, :],
                                        hp == 0, hp == 3, [(KTt, hp), Qbd], [sc])
                    self.held.discard(sc)
                    if 'a_sc' in SKIP:
                        continue
                    self.act(Ebs.t[:, :], sc.t[:, :], AF.Exp, [sc], [Ebs])
                    Pm_ = Pms[half]
                    self.tt(Pm_.t[:, :], Ebs.t[:, :], EMs.t[:, half * 512:(half + 1) * 512], ALU.mult, [Ebs, EMs], [Pm_])
                    for jj in range(8 if 'a_pv' not in SKIP else 0):
                        for h in range(8):
                            O = O2[h // 4]
                            c0 = (jj * 8 + h) * 8
                            self.mm(O.t[0:8, (h % 4) * 65:(h % 4) * 65 + 65], Pm_.t[:, c0:c0 + 8], V1h.t[:, jj, h, :],
                                    False, False, [Pm_, V1h], [O])
                self.held.difference_update(O2)
                if 'a_pv' in SKIP:
                    continue
                self.held.update(O2)
                sc = self.bank()
                for hp in range(4):
                    self.mm(sc.t[:, 0:64], KTn.t[:, hp, :], Qbd.t[:, b, hp, :], hp == 0, hp == 3, [KTn, Qbd], [sc])
                self.act(Ebs.t[:, 0:64], sc.t[:, 0:64], AF.Exp, [sc], [Ebs])
                self.tt(Pmn.t[:, :], Ebs.t[:, 0:64], EMn.t[:, b * 64:(b + 1) * 64], ALU.mult, [Ebs, EMn], [Pmn])
                for h in range(8):
                    O = O2[h // 4]
                    self.mm(O.t[0:8, (h % 4) * 65:(h % 4) * 65 + 65], Pmn.t[:, h * 8:h * 8 + 8], Vn1.t[:, h, :], False, h % 4 == 3, [Pmn, Vn1], [O])
                self.held.difference_update(O2)
                og = ostg[b % 2]
                for oi, O in enumerate(O2):
                    ov = O.t[0:8, 0:260].rearrange("p (h c) -> p h c", h=4)
                    fw.op('dve', lambda e, ov=ov, oi=oi: e.reciprocal(orec8.t[:, oi * 4:(oi + 1) * 4].unsqueeze(2), ov[:, :, 0:1]),
                          reads=[O], writes=[(orec8, oi)])
                    self.tt(og.t[:, oi * 256:(oi + 1) * 256].rearrange("p (h v) -> p h v", h=4), ov[:, :, 1:65],
                            orec8.t[:, oi * 4:(oi + 1) * 4].unsqueeze(2).to_broadcast([8, 4, 64]), ALU.mult,
                            [O, (orec8, oi)], [(og, oi)])
                fw.dma('sp', ATtok.t[b * 8:(b + 1) * 8, :], og.t[:, :], reads=[og], writes=[(ATtok, b)])
            ss = self.ss
            self.act(self.junk.t[:, 0, :], ATtok.t[:, :], AF.Square, [ATtok], [self.junk, ss], accum=ss.t[:, :])
            self.ts(ss.t[:, :], ss.t[:, :], 1.0 / RW, RMS_EPS, ALU.mult, ALU.add, [ss], [ss])
            self.act(ss.t[:, :], ss.t[:, :], AF.Sqrt, [ss], [ss])
            fw.op('dve', lambda e: e.reciprocal(self.ss.t[:, :], self.ss.t[:, :]), reads=[ss], writes=[ss])
            self.stt(ATtok.t[:, :], ATtok.t[:, :], ss.t[:, 0:1], again.t[:, :], ALU.mult, ALU.mult, [ATtok, ss, again], [ATtok])
            pb = self.bank()
            for cb in range(4):
                self.tr(pb.t[:, cb * 128:(cb + 1) * 128], ATtok.t[:, cb * 128:(cb + 1) * 128], identf.t[:, :], [ATtok, identf], [pb])
            self.cp(atT.t[:, :, 0:128], pb.t[:, :].rearrange("p (c t) -> p c t", c=4), [pb], [atT], eng='act')
            astk.close()
            fw.barrier()
            tail(1, pps, ys)
            sstk.close()
            fw.stk = st
            fw.finish()
        return nc


def build_prompt_nc(seq, stages=99):
    b = Builder(seq, stages=stages)
    return b.build()


_NC_CACHE = {}


def kernel(**inputs):
    inputs = {k: np.asarray(v) for k, v in inputs.items()}
    B, S = inputs["x_prompt"].shape[0], inputs["x_prompt"].shape[1]
    if "nc" not in _NC_CACHE:
        _NC_CACHE["nc"] = build_prompt_nc(S)
    nc = _NC_CACHE["nc"]
    consts = _host_consts()
    f = np.ascontiguousarray
    wts = {}
    for k, shp in WEIGHT_SHAPES.items():
        v = inputs[k]
        v = v[0] if k != "norm_final" else v
        wts[k] = f(v.reshape(shp).astype(np.float32, copy=False))
    in_maps = []
    L = NSEQ
    for c in range(8):
        b = c % B
        sl = slice(c * L, (c + 1) * L)
        m = {"xp": f(inputs["x_prompt"][b]), "pp": f(inputs["p_prompt"][0, b]),
             "xs": f(inputs["x_sample"][sl].reshape(L * 8, D)),
             "pps": f(inputs["p_sample"][0, sl].reshape(L * 8, 256)),
             "swkv": f(inputs["state_wkv"][0, sl].reshape(L * 8, 4096)),
             "sshift": f(inputs["state_shift"][0, sl]),
             "kc": f(inputs["cache_k_win"][0, sl].reshape(L, MAXWIN, 512)),
             "vc": f(inputs["cache_v_win"][0, sl].reshape(L, MAXWIN, 512))}
        m.update(wts)
        m.update(consts)
        in_maps.append(m)
    res = run_bass_kernel_spmd(nc, in_maps, core_ids=list(range(8))).results
    f32 = np.float32
    keep = min(MAXWIN, S)
    y_prompt = np.stack([res[b]["yp"] for b in range(B)]).astype(f32, copy=False)
    wkv_p = np.stack([res[b]["wkvp"] for b in range(B)])[None].astype(f32, copy=False)
    shift_p = np.stack([res[b]["shp"] for b in range(B)])[None].astype(f32, copy=False)
    kwin_p = np.stack([res[b]["kwp"].reshape(keep, 8, 64) for b in range(B)])[None].astype(f32, copy=False)
    vwin_p = np.stack([res[b]["vwp"].reshape(keep, 8, 64) for b in range(B)])[None].astype(f32, copy=False)
    y_sample = np.concatenate([res[c]["ys"].reshape(L, 8, D) for c in range(8)]).astype(f32, copy=False)
    wkv_s = np.concatenate([res[c]["wkvs"].reshape(L, 8, 64, 64) for c in range(8)])[None].astype(f32, copy=False)
    shift_s = np.concatenate([res[c]["shs"] for c in range(8)])[None].astype(f32, copy=False)
    kwin_s = np.concatenate([res[c]["kws"].reshape(L, MAXWIN, 8, 64) for c in range(8)])[None].astype(f32, copy=False)
    vwin_s = np.concatenate([res[c]["vws"].reshape(L, MAXWIN, 8, 64) for c in range(8)])[None].astype(f32, copy=False)
    return (y_prompt, y_sample, wkv_p, shift_p, kwin_p, vwin_p, wkv_s, shift_s, kwin_s, vwin_s)
```
